# Optimizing a Trainium2 kernel written in Bass

```python
import math
import jax, jax.numpy as jnp
from jax import lax
import numpy as np

D_MODEL = 1024
BATCH = 2
SEQ = 16384
DEPTH = 2

GRID_W = 64
CTX_LEN = 256
BLK = 128
WINDOW = 128
ROPE_BASE = 10000.0
EPS = 1e-6
NEG = -1e30

A_HEADS = 8
A_KV = 2
A_DH = 64
B_GROUPS = 4
B_DG = 64
B_CHUNK = 128
C_HEADS = 4
C_DQ = 32
C_DV = 64

D_MIX = A_HEADS * A_DH + B_GROUPS * B_DG + C_HEADS * C_DV
OFF_AQ = 0
OFF_AK = OFF_AQ + A_HEADS * A_DH
OFF_AV = OFF_AK + A_KV * A_DH
OFF_BU = OFF_AV + A_KV * A_DH
OFF_BV = OFF_BU + B_GROUPS * B_DG
OFF_CQ = OFF_BV + B_GROUPS * B_DG
OFF_CK = OFF_CQ + C_HEADS * 2 * C_DQ
OFF_CV = OFF_CK + C_HEADS * 2 * C_DQ
D_IN = OFF_CV + C_HEADS * C_DV

MOE_GROUPS = 4
MOE_PER_GROUP = 4
N_EXPERTS = MOE_GROUPS * MOE_PER_GROUP
MOE_TOPK = 2
D_EXPERT = 512

kernel_name = 'hybrid_parallel_group_dit_block'


def rmsnorm(x, g):
    xf = x.astype(jnp.float32)
    y = xf * lax.rsqrt(jnp.mean(xf * xf, axis=-1, keepdims=True) + EPS)
    return (y * g.astype(jnp.float32)).astype(x.dtype)


def axial_rope(x, rows, cols):
    dh = x.shape[-1]
    n = dh // 4
    inv = ROPE_BASE ** (-jnp.arange(n, dtype=jnp.float32) / n)
    xf = x.astype(jnp.float32)

    def rot(xp, pos):
        ang = pos[:, None] * inv
        cs = jnp.cos(ang)[None, :, None, :]
        sn = jnp.sin(ang)[None, :, None, :]
        x1, x2 = xp[..., :n], xp[..., n:]
        return jnp.concatenate([x1 * cs - x2 * sn, x1 * sn + x2 * cs], axis=-1)

    out = jnp.concatenate([rot(xf[..., :2 * n], rows), rot(xf[..., 2 * n:], cols)], axis=-1)
    return out.astype(x.dtype)


def _sink_probs(scores, sink):
    m = sink
    for s in scores:
        m = jnp.maximum(m, jnp.max(s, axis=-1, keepdims=True))
    es = [jnp.exp(s - m) for s in scores]
    den = jnp.exp(sink - m)
    for e in es:
        den = den + jnp.sum(e, axis=-1, keepdims=True)
    return [e / den for e in es]


def window_sink_attn(q, k, v, kc, vc, sink):
    Bn, S, Hq, dh = q.shape
    Hkv = k.shape[2]
    G = Hq // Hkv
    nb = S // BLK
    scale = dh ** -0.5
    qb = q.reshape(Bn, nb, BLK, Hkv, G, dh)

    def band(t):
        tp = jnp.pad(t, ((0, 0), (BLK, BLK), (0, 0), (0, 0))).reshape(Bn, nb + 2, BLK, Hkv, dh)
        return jnp.concatenate([tp[:, :-2], tp[:, 1:-1], tp[:, 2:]], axis=2)

    kb, vb = band(k), band(v)
    s_loc = jnp.einsum('bnqhgd,bnkhd->bnhgqk', qb, kb).astype(jnp.float32) * scale
    bi = jnp.arange(nb)[:, None, None]
    qpos = bi * BLK + jnp.arange(BLK)[None, :, None]
    kpos = (bi - 1) * BLK + jnp.arange(3 * BLK)[None, None, :]
    mask = (jnp.abs(kpos - qpos) <= WINDOW) & (kpos >= 0) & (kpos < S)
    s_loc = jnp.where(mask[None, :, None, None], s_loc, NEG)
    s_ctx = jnp.einsum('bnqhgd,bchd->bnhgqc', qb, kc).astype(jnp.float32) * scale
    p_loc, p_ctx = _sink_probs([s_loc, s_ctx], sink.astype(jnp.float32).reshape(Hkv, G, 1, 1))
    o = (jnp.einsum('bnhgqk,bnkhd->bnqhgd', p_loc.astype(v.dtype), vb)
         + jnp.einsum('bnhgqc,bchd->bnqhgd', p_ctx.astype(v.dtype), vc))
    return o.reshape(Bn, S, Hq * dh)


def ctx_sink_attn(qc, kc, vc, sink):
    Bn, L, Hq, dh = qc.shape
    Hkv = kc.shape[2]
    G = Hq // Hkv
    qg = qc.reshape(Bn, L, Hkv, G, dh)
    s = jnp.einsum('bqhgd,bkhd->bhgqk', qg, kc).astype(jnp.float32) * dh ** -0.5
    (p,) = _sink_probs([s], sink.astype(jnp.float32).reshape(Hkv, G, 1, 1))
    o = jnp.einsum('bhgqk,bkhd->bqhgd', p.astype(vc.dtype), vc)
    return o.reshape(Bn, L, Hq * dh)


def chunk_gmlp(u, v, ws, bs, gtok):
    Bn, L, _ = u.shape
    nc = L // B_CHUNK
    u = jax.nn.gelu(u).reshape(Bn, nc, B_CHUNK, B_GROUPS, B_DG)
    vf = jax.nn.gelu(v).astype(jnp.float32).reshape(Bn, nc, B_CHUNK, B_GROUPS, B_DG)
    mu = jnp.mean(vf, axis=-1, keepdims=True)
    var = jnp.mean(jnp.square(vf - mu), axis=-1, keepdims=True)
    vn = ((vf - mu) * lax.rsqrt(var + EPS) * gtok.astype(jnp.float32)).astype(u.dtype)
    s = jnp.einsum('gpq,bnqgd->bnpgd', ws, vn) + bs.T[:, :, None]
    return (u * s).reshape(Bn, L, B_GROUPS * B_DG)


def _diff_core(q1, q2, k1, k2, v, lam):
    scale = C_DQ ** -0.5
    p1 = jax.nn.softmax(jnp.einsum('bqhd,bkhd->bhqk', q1, k1).astype(jnp.float32) * scale, axis=-1)
    p2 = jax.nn.softmax(jnp.einsum('bqhd,bkhd->bhqk', q2, k2).astype(jnp.float32) * scale, axis=-1)
    w = p1 - lam * p2
    return jnp.einsum('bhqk,bkhe->bqhe', w.astype(v.dtype), v)


def diff_attn(q1, q2, k1, k2, v, lam):
    Bn, Lq, H, dq = q1.shape
    nb = Lq // BLK
    qb1 = q1.reshape(Bn, nb, BLK, H, dq).swapaxes(0, 1)
    qb2 = q2.reshape(Bn, nb, BLK, H, dq).swapaxes(0, 1)
    out = lax.map(lambda qs: _diff_core(qs[0], qs[1], k1, k2, v, lam), (qb1, qb2))
    return out.swapaxes(0, 1).reshape(Bn, Lq, H, v.shape[-1])


def diff_out(o, gsub, lam_init):
    Bn, L, H, dv = o.shape
    return (rmsnorm(o, gsub) * (1.0 - lam_init)).reshape(Bn, L, H * dv)


def token_mixers(h, hc, w_in, w_out, sink, ws, bs, gtok, gsub, lam, lam_init, rows, cols, ctx_out):
    Bn, S, _ = h.shape
    Lc = hc.shape[1]
    p = h @ w_in

    def heads(lo, hi, n, d):
        return p[..., lo:hi].reshape(Bn, S, n, d)

    def cproj(lo, hi, n, d):
        return (hc @ w_in[:, lo:hi]).reshape(Bn, Lc, n, d)

    qa = axial_rope(heads(OFF_AQ, OFF_AK, A_HEADS, A_DH), rows, cols)
    ka = axial_rope(heads(OFF_AK, OFF_AV, A_KV, A_DH), rows, cols)
    va = heads(OFF_AV, OFF_BU, A_KV, A_DH)
    ka_c = cproj(OFF_AK, OFF_AV, A_KV, A_DH)
    va_c = cproj(OFF_AV, OFF_BU, A_KV, A_DH)
    ya = window_sink_attn(qa, ka, va, ka_c, va_c, sink)
    yb = chunk_gmlp(p[..., OFF_BU:OFF_BV], p[..., OFF_BV:OFF_CQ], ws, bs, gtok)
    qd = axial_rope(heads(OFF_CQ, OFF_CK, 2 * C_HEADS, C_DQ), rows, cols).reshape(Bn, S, C_HEADS, 2, C_DQ)
    kd = axial_rope(heads(OFF_CK, OFF_CV, 2 * C_HEADS, C_DQ), rows, cols).reshape(Bn, S, C_HEADS, 2, C_DQ)
    vd = heads(OFF_CV, D_IN, C_HEADS, C_DV)
    kd_c = cproj(OFF_CK, OFF_CV, 2 * C_HEADS, C_DQ).reshape(Bn, Lc, C_HEADS, 2, C_DQ)
    vd_c = cproj(OFF_CV, D_IN, C_HEADS, C_DV)
    k1 = jnp.concatenate([kd_c[..., 0, :], kd[..., 0, :]], axis=1)
    k2 = jnp.concatenate([kd_c[..., 1, :], kd[..., 1, :]], axis=1)
    vall = jnp.concatenate([vd_c, vd], axis=1)
    yc = diff_out(diff_attn(qd[..., 0, :], qd[..., 1, :], k1, k2, vall, lam), gsub, lam_init)
    y = jnp.concatenate([ya, yb, yc], axis=-1) @ w_out
    if not ctx_out:
        return y, None
    ya_c = ctx_sink_attn(cproj(OFF_AQ, OFF_AK, A_HEADS, A_DH), ka_c, va_c, sink)
    yb_c = chunk_gmlp(hc @ w_in[:, OFF_BU:OFF_BV], hc @ w_in[:, OFF_BV:OFF_CQ], ws, bs, gtok)
    qd_c = cproj(OFF_CQ, OFF_CK, 2 * C_HEADS, C_DQ).reshape(Bn, Lc, C_HEADS, 2, C_DQ)
    yc_c = diff_out(diff_attn(qd_c[..., 0, :], qd_c[..., 1, :], kd_c[..., 0, :], kd_c[..., 1, :], vd_c, lam), gsub, lam_init)
    y_c = jnp.concatenate([ya_c, yb_c, yc_c], axis=-1) @ w_out
    return y, y_c


def hier_moe(h, w_rg, b_rg, w_re, b_re, w_gate, w_up, w_down):
    Bn, L, D = h.shape
    t = h.reshape(-1, D)
    pg = jax.nn.softmax((t @ w_rg).astype(jnp.float32) + b_rg.astype(jnp.float32), axis=-1)
    ptop, gsel = lax.top_k(pg, 1)
    le = ((t @ w_re).astype(jnp.float32) + b_re.astype(jnp.float32)).reshape(-1, MOE_GROUPS, MOE_PER_GROUP)
    le_g = jnp.sum(le * jax.nn.one_hot(gsel[:, 0], MOE_GROUPS, dtype=jnp.float32)[:, :, None], axis=1)
    lv, li = lax.top_k(le_g, MOE_TOPK)
    w = jax.nn.softmax(lv, axis=-1) * ptop
    eid = gsel * MOE_PER_GROUP + li
    gates = jnp.sum(jax.nn.one_hot(eid, N_EXPERTS, dtype=jnp.float32) * w[..., None], axis=1)
    y = jnp.zeros(t.shape, jnp.float32)
    for e in range(N_EXPERTS):
        he = jax.nn.silu(t @ w_gate[e]) * (t @ w_up[e])
        y = y + gates[:, e:e + 1] * (he @ w_down[e]).astype(jnp.float32)
    return y.astype(h.dtype).reshape(Bn, L, D)


def setup_inputs(seed: int = 0) -> dict:
    key = jax.random.key(seed)
    ks = jax.random.split(key, 32)

    def nrm(k, shape, s):
        return jax.random.normal(k, shape, jnp.float32) * s

    D = D_MODEL
    return {
        'x': nrm(ks[0], (BATCH, SEQ, D), 1.0),
        'c': nrm(ks[1], (BATCH, D), 1.0),
        'ctx': nrm(ks[2], (BATCH, CTX_LEN, D), 1.0),
        'c_ctx': nrm(ks[3], (D,), 1.0),
        'w_ada': nrm(ks[4], (DEPTH, D, 6 * D), 0.2 * D ** -0.5),
        'b_ada': nrm(ks[5], (DEPTH, 6 * D), 0.01),
        'g_mix': 1.0 + nrm(ks[6], (DEPTH, D), 0.01),
        'w_in': nrm(ks[7], (DEPTH, D, D_IN), D ** -0.5),
        'sink': nrm(ks[8], (DEPTH, A_HEADS), 0.5),
        'ws_tok': nrm(ks[9], (DEPTH, B_GROUPS, B_CHUNK, B_CHUNK), B_CHUNK ** -0.5),
        'bs_tok': 1.0 + nrm(ks[10], (DEPTH, B_GROUPS, B_CHUNK), 0.02),
        'g_tok': 1.0 + nrm(ks[11], (DEPTH, B_GROUPS, B_DG), 0.01),
        'lam_q1': nrm(ks[12], (DEPTH, C_DQ), 0.1),
        'lam_k1': nrm(ks[13], (DEPTH, C_DQ), 0.1),
        'lam_q2': nrm(ks[14], (DEPTH, C_DQ), 0.1),
        'lam_k2': nrm(ks[15], (DEPTH, C_DQ), 0.1),
        'g_sub': 1.0 + nrm(ks[16], (DEPTH, C_DV), 0.01),
        'w_out': nrm(ks[17], (DEPTH, D_MIX, D), D_MIX ** -0.5),
        'g_ffn': 1.0 + nrm(ks[18], (DEPTH, D), 0.01),
        'w_rg': nrm(ks[19], (DEPTH, D, MOE_GROUPS), D ** -0.5),
        'b_rg': nrm(ks[20], (DEPTH, MOE_GROUPS), 0.01),
        'w_re': nrm(ks[21], (DEPTH, D, N_EXPERTS), D ** -0.5),
        'b_re': nrm(ks[22], (DEPTH, N_EXPERTS), 0.01),
        'w_gate': nrm(ks[23], (DEPTH, N_EXPERTS, D, D_EXPERT), D ** -0.5),
        'w_up': nrm(ks[24], (DEPTH, N_EXPERTS, D, D_EXPERT), D ** -0.5),
        'w_down': nrm(ks[25], (DEPTH, N_EXPERTS, D_EXPERT, D), D_EXPERT ** -0.5),
        'g_final': 1.0 + nrm(ks[26], (D,), 0.01),
    }


def reference(x, c, ctx, c_ctx, w_ada, b_ada, g_mix, w_in, sink, ws_tok, bs_tok, g_tok,
              lam_q1, lam_k1, lam_q2, lam_k2, g_sub, w_out, g_ffn, w_rg, b_rg, w_re, b_re,
              w_gate, w_up, w_down, g_final):
    S = x.shape[1]
    ROWS = S // GRID_W
    rows = jnp.repeat(jnp.arange(ROWS, dtype=jnp.float32), GRID_W)
    cols = jnp.broadcast_to(jnp.arange(GRID_W, dtype=jnp.float32)[None, :], (ROWS, GRID_W)).reshape(-1)
    xc = ctx
    for l in range(DEPTH):
        last = l == DEPTH - 1
        mod = jax.nn.silu(c) @ w_ada[l] + b_ada[l]
        modc = jax.nn.silu(c_ctx) @ w_ada[l] + b_ada[l]
        sh1, sc1, gt1, sh2, sc2, gt2 = jnp.split(mod[:, None, :], 6, axis=-1)
        csh1, csc1, cgt1, csh2, csc2, cgt2 = jnp.split(modc[None, None, :], 6, axis=-1)
        lam_init = 0.8 - 0.6 * math.exp(-0.3 * l)
        lam = (jnp.exp(jnp.sum(lam_q1[l].astype(jnp.float32) * lam_k1[l].astype(jnp.float32)))
               - jnp.exp(jnp.sum(lam_q2[l].astype(jnp.float32) * lam_k2[l].astype(jnp.float32))) + lam_init)
        h = rmsnorm(x, g_mix[l]) * (1.0 + sc1) + sh1
        hc = rmsnorm(xc, g_mix[l]) * (1.0 + csc1) + csh1
        y, y_c = token_mixers(h, hc, w_in[l], w_out[l], sink[l], ws_tok[l], bs_tok[l], g_tok[l],
                              g_sub[l], lam, lam_init, rows, cols, not last)
        x = x + gt1 * y
        h2 = rmsnorm(x, g_ffn[l]) * (1.0 + sc2) + sh2
        x = x + gt2 * hier_moe(h2, w_rg[l], b_rg[l], w_re[l], b_re[l], w_gate[l], w_up[l], w_down[l])
        if not last:
            xc = xc + cgt1 * y_c
            hc2 = rmsnorm(xc, g_ffn[l]) * (1.0 + csc2) + csh2
            xc = xc + cgt2 * hier_moe(hc2, w_rg[l], b_rg[l], w_re[l], b_re[l], w_gate[l], w_up[l], w_down[l])
    return rmsnorm(x, g_final)
```

```python
import math
from contextlib import ExitStack

import numpy as np
import concourse.bass as bass
import concourse.mybir as mybir
from concourse.bass_utils import run_bass_kernel_spmd

F32 = mybir.dt.float32
BF16 = mybir.dt.bfloat16
U8 = mybir.dt.uint8
AF = mybir.ActivationFunctionType
ALU = mybir.AluOpType
AX = mybir.AxisListType

D = 1024
DIN = 2048
EPS = 1e-6
GRID_W = 64
NEXP = 16
DEXP = 512
SCALE_A = 64 ** -0.5
SCALE_C = 32 ** -0.5


class Rec:
    __slots__ = ("q", "fn", "waits", "need", "val", "sem", "inc", "key")

    def __init__(self, q, fn):
        self.q, self.fn = q, fn
        self.waits = []
        self.need = False
        self.val = None
        self.sem = None
        self.inc = 1
        self.key = q.name


class Buf:
    __slots__ = ("name", "w", "r", "excl")

    def __init__(self, name="", excl=False):
        self.name = name
        self.w = None
        self.r = {}
        self.excl = excl


class Queue:
    def __init__(self, name, eng, sem):
        self.name, self.eng, self.sem = name, eng, sem
        self.recs = []
        self.dsems = []
        self.dlast = []
        self.dnext = 0


class FW:
    def __init__(self, nc, stack, n_dsem_sp=24, n_dsem_pool=12, n_cc=10):
        self.nc = nc
        self.Q = {}
        for name, eng in (("pe", nc.tensor), ("act", nc.scalar), ("dve", nc.vector), ("pool", nc.gpsimd), ("sp", nc.sync)):
            sem = stack.enter_context(nc.semaphore("q_" + name))
            self.Q[name] = Queue(name, eng, sem)
        for qn, n in (("sp", n_dsem_sp), ("pool", n_dsem_pool)):
            q = self.Q[qn]
            for i in range(n):
                q.dsems.append(stack.enter_context(nc.semaphore(f"d_{qn}{i}")))
                q.dlast.append(None)
        self.ccsems = [stack.enter_context(nc.semaphore(f"cc{i}")) for i in range(n_cc)]
        self.cclast = [None] * n_cc
        self.ccnext = 0
        self.all_dma = []

    def _deps(self, q, reads, writes):
        deps = []
        for b in reads:
            if b.w is not None:
                deps.append(b.w)
            if b.excl:
                deps.extend(x for x in b.r.values() if x.q is not q)
        for b in writes:
            if b.w is not None:
                deps.append(b.w)
            deps.extend(b.r.values())
        out = []
        seen = set()
        for d in deps:
            if id(d) in seen:
                continue
            seen.add(id(d))
            if d.q is q and q.name == "pe" and d.key == "pe":
                continue
            out.append(d)
        return out

    def _commit(self, rec, reads, writes):
        for d in rec.waits:
            d.need = True
        rec.q.recs.append(rec)
        for b in reads:
            b.r[rec.key] = rec
        for b in writes:
            b.w = rec
            b.r = {}

    def op(self, qn, fn, reads=(), writes=()):
        q = self.Q[qn]
        rec = Rec(q, fn)
        rec.waits = self._deps(q, reads, writes)
        self._commit(rec, reads, writes)
        return rec

    def dma(self, qn, out, in_, reads=(), writes=(), **kw):
        q = self.Q[qn]
        rec = Rec(q, lambda e: e.dma_start(out=out, in_=in_, **kw))
        i = q.dnext
        q.dnext = (q.dnext + 1) % len(q.dsems)
        rec.key = f"{qn}_d{i}"
        rec.sem = q.dsems[i]
        rec.inc = 16
        rec.need = True
        rec.waits = self._deps(q, reads, writes)
        if q.dlast[i] is not None:
            rec.waits.append(q.dlast[i])
        q.dlast[i] = rec
        self._commit(rec, reads, writes)
        self.all_dma.append(rec)
        return rec

    def cc(self, fn, reads=(), writes=()):
        q = self.Q["pool"]
        rec = Rec(q, fn)
        i = self.ccnext
        self.ccnext = (self.ccnext + 1) % len(self.ccsems)
        rec.key = f"cc{i}"
        rec.sem = self.ccsems[i]
        rec.inc = 1
        rec.need = True
        rec.waits = self._deps(q, reads, writes)
        if self.cclast[i] is not None:
            rec.waits.append(self.cclast[i])
        self.cclast[i] = rec
        self._commit(rec, reads, writes)
        return rec

    def barrier(self):
        lasts = []
        for q in self.Q.values():
            for r in reversed(q.recs):
                if r.key == q.name and r.fn is not None:
                    lasts.append(r)
                    break
            lasts.extend(x for x in q.dlast if x is not None)
        lasts.extend(x for x in self.cclast if x is not None)
        for q in self.Q.values():
            rec = Rec(q, None)
            rec.waits = [d for d in lasts if not (d.q is q and d.key == q.name)]
            for d in rec.waits:
                d.need = True
            q.recs.append(rec)

    def finish(self, block):
        for q in self.Q.values():
            cnt = 0
            dcnt = {}
            for r in q.recs:
                if r.fn is None:
                    continue
                if r.key != q.name:
                    dcnt[r.key] = dcnt.get(r.key, 0) + r.inc
                    r.val = dcnt[r.key]
                elif r.need:
                    cnt += 1
                    r.val = cnt
                    r.sem = q.sem
        stats = {}
        for q in self.Q.values():
            def run(eng, q=q):
                seen = {}
                nw = 0
                for r in q.recs:
                    for d in r.waits:
                        k = d.key
                        if seen.get(k, 0) >= d.val:
                            continue
                        eng.wait_ge(d.sem, d.val)
                        seen[k] = d.val
                        nw += 1
                    if r.fn is None:
                        continue
                    ins = r.fn(eng)
                    if r.need:
                        ins.then_inc(r.sem, r.inc)
                stats[q.name] = (len(q.recs), nw)
            getattr(block, {"pe": "tensor", "act": "scalar", "dve": "vector", "pool": "gpsimd", "sp": "sync"}[q.name])(run)
        return stats


class Arena:
    def __init__(self, handle, nbytes):
        self.h, self.n = handle, nbytes
        self.off = 0
        self.marks = []

    def alloc(self, shape, dtype, parts=128):
        esz = {F32: 4, BF16: 2, U8: 1}[dtype]
        n = int(np.prod(shape)) * esz
        n_al = (n + 63) // 64 * 64
        assert self.off + n_al <= self.n, f"SBUF arena overflow: {self.off}+{n_al} > {self.n}"
        ap = self.h[0:parts, self.off:self.off + n]
        self.off += n_al
        self.peak = max(getattr(self, 'peak', 0), self.off)
        if dtype != U8:
            ap = ap.bitcast(dtype)
        if len(shape) == 1:
            return ap
        names = [f"a{i}" for i in range(len(shape))]
        pat = "p (" + " ".join(names) + ") -> p " + " ".join(names)
        return ap.rearrange(pat, **{nm: s for nm, s in zip(names[:-1], shape[:-1])})

    def mark(self):
        self.marks.append(self.off)

    def release(self):
        print('arena scope peak', getattr(self, 'peak', 0), 'at release, off', self.off)
        self.off = self.marks.pop()


def bcast_rows(ap_row, n):
    return ap_row.partition_broadcast(n) if ap_row.shape[0] != 1 else ap_row.broadcast_to([n] + list(ap_row.shape[1:]))


def MM(out, lhsT, rhs, start=True, stop=True, **kw):
    return lambda e: e.matmul(out, lhsT=lhsT, rhs=rhs, start=start, stop=stop, **kw)


def TR(out, in_, ident):
    return lambda e: e.transpose(out, in_, ident)


def ACT(out, in_, func, **kw):
    return lambda e: e.activation(out=out, in_=in_, func=func, **kw)


def TT(out, a, b, op):
    return lambda e: e.tensor_tensor(out=out, in0=a, in1=b, op=op)


def TS(out, a, s1, op0, s2=None, op1=None, **kw):
    if op1 is None:
        return lambda e: e.tensor_scalar(out=out, in0=a, scalar1=s1, scalar2=None, op0=op0, **kw)
    return lambda e: e.tensor_scalar(out=out, in0=a, scalar1=s1, scalar2=s2, op0=op0, op1=op1, **kw)


def STT(out, a, s, b, op0, op1):
    return lambda e: e.scalar_tensor_tensor(out=out, in0=a, scalar=s, in1=b, op0=op0, op1=op1)


def CP(out, in_):
    return lambda e: e.tensor_copy(out=out, in_=in_)


def MEMSET(ap, v):
    return lambda e: e.memset(ap, v)


def RSUM(out, in_, axis=None):
    return lambda e: e.reduce_sum(out=out, in_=in_, axis=axis or AX.X)


def RMAX(out, in_, axis=None):
    return lambda e: e.reduce_max(out=out, in_=in_, axis=axis or AX.X)


class Cfg:
    def __init__(self, NLAT=32, RANKS=4, NB=2, DEPTH=2, stop_after=None):
        self.NLAT, self.RANKS, self.NB, self.DEPTH = NLAT, RANKS, NB, DEPTH
        self.NT = NLAT + 2
        self.NKC = 2 + RANKS * NLAT
        self.NCORES = NB * RANKS
        self.stop_after = stop_after


ARENA_BYTES = 207 * 1024


class StopBuild(Exception):
    pass


def build_program(cfg):
    NLAT, RANKS, NT, NKC, DEPTH = cfg.NLAT, cfg.RANKS, cfg.NT, cfg.NKC, cfg.DEPTH
    KCH = 8 if NLAT % 8 == 0 else (2 if NLAT % 2 == 0 else 1)
    nc = bass.Bass("TRN2", target_bir_lowering=False)
    dbg = set(getattr(cfg, "debug", ()) or ())
    I = {}

    def inp(name, shape):
        I[name] = nc.dram_tensor(name, shape, F32, kind="ExternalInput").ap()

    inp("xl", [NLAT * 128, D]); inp("ctx", [256, D]); inp("c2", [2, D])
    inp("w_ada", [DEPTH, D, 6 * D]); inp("b_ada2", [DEPTH, 2, 6 * D]); inp("g_mix", [DEPTH, D])
    inp("w_in", [DEPTH, D, DIN]); inp("sink", [DEPTH, 8]); inp("ws_tok", [DEPTH, 4, 128, 128])
    inp("bs_tok", [DEPTH, 4, 128]); inp("g_tok", [DEPTH, 256]); inp("lam4", [DEPTH, 128]); inp("g_sub", [DEPTH, 64])
    inp("w_out", [DEPTH, D, D]); inp("g_ffn", [DEPTH, D]); inp("w_r", [DEPTH, D, 20]); inp("b_r", [DEPTH, 20])
    inp("w_gate", [DEPTH, NEXP, D, DEXP]); inp("w_up", [DEPTH, NEXP, D, DEXP]); inp("w_down", [DEPTH, NEXP, DEXP, D])
    inp("g_final", [1, D]); inp("ident", [128, 128]); inp("masks", [128, 4 * 128]); inp("rope", [128, NT * 96])
    inp("sel", [128, 3 * RANKS])
    out_d = nc.dram_tensor("out", [NLAT * 128, D], F32, kind="ExternalOutput").ap()

    def scr(name, shape, dt):
        kind = "ExternalOutput" if name in dbg else "Internal"
        return nc.dram_tensor(name, shape, dt, kind=kind).ap()

    modd = scr("modd", [DEPTH, 2, 6 * D], F32)
    xbuf = scr("xbuf", [NT, 128, D], F32)
    xmid = scr("xmid", [NT, 128, D], F32)
    qta_d = scr("qta_d", [NT, 128, 512], BF16)
    qtc_d = scr("qtc_d", [NT, 128, 256], BF16)
    yb_d = scr("yb_d", [NT, 128, 256], BF16)
    h2t_d = scr("h2t_d", [128, 8, NT * 128], BF16)
    kctx_d = scr("kctx_d", [128, 2, 256], BF16)
    vctx_d = scr("vctx_d", [2, 128, 260], BF16)
    xk_s = scr("xk_s", [RANKS, 128, 2, NLAT * 128], BF16)
    xv_s = scr("xv_s", [RANKS, NLAT, 128, 260], BF16)
    xh_s = scr("xh_s", [RANKS, 128, 2 * 258], BF16)
    if RANKS > 1:
        xk_r = scr("xk_r", [RANKS, 128, 2, NLAT * 128], BF16)
        xv_r = scr("xv_r", [RANKS, NLAT, 128, 260], BF16)
        xh_r = scr("xh_r", [RANKS, 128, 2 * 258], BF16)
    else:
        xk_r, xv_r, xh_r = xk_s, xv_s, xh_s

    DB = {}

    def db(name, idx=0):
        k = (name, idx)
        if k not in DB:
            DB[k] = Buf(f"{name}{idx}")
        return DB[k]

    with ExitStack() as st:
        arena_h = st.enter_context(nc.sbuf_tensor("arena", [128, ARENA_BYTES], U8))
        ps = st.enter_context(nc.psum_tensor("ps", [128, 8, 512], F32))
        fw = FW(nc, st)
        A = Arena(arena_h, ARENA_BYTES)
        PS = [Buf(f"ps{i}", excl=True) for i in range(8)]
        psb = [ps[:, i, :].bitcast(BF16) for i in range(8)]

        class Ring:
            def __init__(self, n, shape, dt, parts=128):
                self.aps = [A.alloc(shape, dt, parts) for _ in range(n)]
                self.bufs = [Buf() for _ in range(n)]
                self.i = 0

            def next(self):
                k = self.i
                self.i = (self.i + 1) % len(self.aps)
                return self.aps[k], self.bufs[k]

        op, dma = fw.op, fw.dma

        ident_f = A.alloc([128], F32); Bidf = Buf()
        ident_b = A.alloc([128], BF16); Bidb = Buf()
        masks = A.alloc([4, 128], BF16); Bmask = Buf()
        sel = A.alloc([3 * RANKS], F32); Bsel = Buf()
        dma("sp", ident_f, I["ident"], writes=[Bidf])
        dma("pool", ident_b, I["ident"], writes=[Bidb])
        dma("pool", masks, I["masks"].rearrange("p (m q) -> p m q", m=4), writes=[Bmask])
        dma("sp", sel, I["sel"], writes=[Bsel])
        mneg = A.alloc([4, 512], BF16); Bmneg = Buf()
        for m_ in range(4):
            op("dve", TS(mneg[:, m_, :].rearrange("p (j q) -> p j q", j=4), masks[:, m_, :].unsqueeze(1).broadcast_to([128, 4, 128]),
                         -1.0, ALU.add, 30000.0, ALU.mult), reads=[Bmask], writes=[Bmneg])
        ones1 = A.alloc([1], F32); Bones = Buf()
        op("pool", MEMSET(ones1, 1.0), writes=[Bones])

        gates = A.alloc([NT, 16], F32)
        Bgate = [Buf() for _ in range(NT)]

        eps_t = A.alloc([1], F32); Beps = Buf()
        op("pool", MEMSET(eps_t, EPS), writes=[Beps])

        A.mark()
        c_raw = A.alloc([2, 8], F32); Bcraw = Buf()
        cS = A.alloc([8, 2], F32); BcS = Buf()
        dma("sp", c_raw, I["c2"].rearrange("r (p k) -> p r k", p=128), writes=[Bcraw])
        op("act", ACT(cS, c_raw.rearrange("p r k -> p k r"), AF.Silu), reads=[Bcraw], writes=[BcS])
        wa_ring = Ring(2, [8, 512], F32)
        bada = A.alloc([6 * D], F32, parts=2); Bbada = Buf()
        msb_ring = Ring(2, [512], F32, parts=2)
        import os as _os
        for l in range(DEPTH if not _os.environ.get("DBG_NOADA") else 0):
            dma("sp", bada, I["b_ada2"][l], writes=[Bbada])
            for cb in range(12):
                wa, Bwa = wa_ring.next()
                dma("sp", wa, I["w_ada"][l, :, cb * 512:(cb + 1) * 512].rearrange("(p k) n -> p k n", p=128), writes=[Bwa])
                for kc in range(8):
                    op("pe", MM(ps[0:2, 0, :], cS[:, kc, :], wa[:, kc, :], start=(kc == 0), stop=(kc == 7)),
                       reads=[BcS, Bwa], writes=[PS[0]])
                msb, Bmsb = msb_ring.next()
                op("dve", TT(msb, ps[0:2, 0, :], bada[:, cb * 512:(cb + 1) * 512], ALU.add), reads=[PS[0], Bbada], writes=[Bmsb])
                dma("sp", modd[l, :, cb * 512:(cb + 1) * 512], msb, reads=[Bmsb], writes=[db("modd", l)])
        A.release()
        fw.barrier()

        def load_mod(l, r, chunk, dst, buf, q="sp"):
            return dma(q, dst, modd[l, r:r + 1, chunk * D:(chunk + 1) * D].broadcast_to([128, D]),
                       reads=[db("modd", l)], writes=[buf])

        def load_gain(src_row, dst, buf, q="sp"):
            return dma(q, dst, src_row.broadcast_to([128] + [src_row.shape[1]]), writes=[buf])

        def xsrc(l, t):
            if l == 0:
                return (I["ctx"][t * 128:(t + 1) * 128, :], []) if t < 2 else (I["xl"][(t - 2) * 128:(t - 1) * 128, :], [])
            return xbuf[t], [db("xbuf", t)]

        def rstd_from_ss(ss, Bss, tmp, Btmp, rstd, Brstd, n):
            op("act", ACT(tmp, ss, AF.Ln, scale=1.0 / n, bias=eps_t), reads=[Bss, Beps], writes=[Btmp])
            op("act", ACT(rstd, tmp, AF.Exp, scale=-0.5), reads=[Btmp], writes=[Brstd])

        def ck(name):
            if cfg.stop_after == name:
                raise StopBuild()

        try:
          for l in range(DEPTH):
              last = (l == DEPTH - 1)
              if cfg.stop_after == "pre":
                  break
              lam_init = 0.8 - 0.6 * math.exp(-0.3 * l)
              A.mark()
              lam4b = A.alloc([4, 32], F32); Bl4 = Buf()
              dma("sp", lam4b, I["lam4"][l:l + 1, :].broadcast_to([128, 128]).rearrange("p (a b) -> p a b", a=4), writes=[Bl4])
              lsc = A.alloc([8], F32); Blsc = Buf()
              ljunk = A.alloc([32], F32); Blj = Buf()
              op("dve", TT(ljunk, lam4b[:, 0, :], lam4b[:, 1, :], ALU.mult), reads=[Bl4], writes=[Blj])
              op("dve", RSUM(lsc[:, 0:1], ljunk), reads=[Blj], writes=[Blsc])
              op("dve", TT(ljunk, lam4b[:, 2, :], lam4b[:, 3, :], ALU.mult), reads=[Bl4], writes=[Blj])
              op("dve", RSUM(lsc[:, 1:2], ljunk), reads=[Blj, Blsc], writes=[Blsc])
              op("act", ACT(lsc[:, 2:4], lsc[:, 0:2], AF.Exp), reads=[Blsc], writes=[Blsc])
              op("dve", TT(lsc[:, 4:5], lsc[:, 2:3], lsc[:, 3:4], ALU.subtract), reads=[Blsc], writes=[Blsc])
              neglam = lsc[:, 5:6]
              op("dve", TS(neglam, lsc[:, 4:5], -1.0, ALU.mult, -lam_init, ALU.add), reads=[Blsc], writes=[Blsc])
              esink = A.alloc([8], F32); Besink = Buf()
              dma("sp", esink, I["sink"][l:l + 1, :].broadcast_to([128, 8]), writes=[Besink])
              op("act", ACT(esink, esink, AF.Exp), reads=[Besink], writes=[Besink])
              gsub_s = A.alloc([64], F32); Bgsub = Buf()
              dma("sp", gsub_s, I["g_sub"][l:l + 1, :].broadcast_to([128, 64]), writes=[Bgsub])
              op("dve", TS(gsub_s, gsub_s, 1.0 - lam_init, ALU.mult), reads=[Bgsub], writes=[Bgsub])
              A.mark()
              KTA = A.alloc([(NT + 2) * 128], BF16)
              VA = A.alloc([NT + 2, 130], BF16)
              BKTA = [Buf() for _ in range(NT + 2)]
              BVA = [Buf() for _ in range(NT + 2)]
              op("pool", MEMSET(VA, 1.0), writes=BVA)
              op("pool", MEMSET(KTA[:, NT * 128:(NT + 2) * 128], 0.0), writes=BKTA[NT:NT + 2])

              A.mark()
              win = A.alloc([8, DIN], BF16); Bwin = Buf()
              for kc in range(8):
                  dma("pool", win[:, kc, :], I["w_in"][l, kc * 128:(kc + 1) * 128, :], writes=[Bwin])
              rope_ring = Ring(2, [96], F32)
              G1 = [A.alloc([D], F32) for _ in range(2)]; BG1 = [Buf(), Buf()]
              SH1 = [A.alloc([D], F32) for _ in range(2)]; BSH1 = [Buf(), Buf()]
              gtmp = A.alloc([D], F32); Bgtmp = Buf()
              for r in range(2):
                  load_mod(l, r, 1, G1[r], BG1[r])
                  load_gain(I["g_mix"][l:l + 1, :], gtmp, Bgtmp)
                  op("dve", STT(G1[r], G1[r], 1.0, gtmp, ALU.add, ALU.mult), reads=[BG1[r], Bgtmp], writes=[BG1[r]])
                  load_mod(l, r, 0, SH1[r], BSH1[r])
              ws_f = A.alloc([4, 128], F32); Bwsf = Buf()
              dma("sp", ws_f, I["ws_tok"][l].rearrange("g p q -> p g q"), writes=[Bwsf])
              wsT = A.alloc([4, 128], BF16); BwsT = Buf()
              for g in range(4):
                  op("pe", TR(ps[:, 7, g * 128:(g + 1) * 128], ws_f[:, g, :], ident_f), reads=[Bwsf, Bidf], writes=[PS[7]])
              op("dve", CP(wsT, ps[:, 7, :].rearrange("p (g q) -> p g q", g=4)), reads=[PS[7]], writes=[BwsT])
              bsT = A.alloc([4], F32); BbsT = Buf()
              dma("sp", bsT, I["bs_tok"][l].rearrange("g p -> p g"), writes=[BbsT], allow_slow_non_contiguous=True)
              gtokb = A.alloc([256], F32); Bgtok = Buf()
              dma("sp", gtokb, I["g_tok"][l:l + 1, :].broadcast_to([128, 256]), writes=[Bgtok])

              ck(('p1a', l))
              x_ring = Ring(2, [D], F32)
              junk_b = A.alloc([D], BF16); Bjunk = Buf()
              sc_ring = Ring(2, [8], F32)
              t1_ring = Ring(2, [D], F32)
              h_ring = Ring(2, [D], BF16)
              hT_ring = Ring(2, [8, 128], BF16)
              psb_ring = Ring(2, [DIN], F32)
              rt_ring = Ring(2, [4, 320], F32)
              qk_ring = Ring(2, [1152], BF16)
              qta_ring = Ring(2, [512], BF16)
              qtc_ring = Ring(2, [256], BF16)
              vt = A.alloc([4, 65], BF16); Bvt = Buf()
              op("pool", MEMSET(vt, 1.0), writes=[Bvt])
              NSTG = min(2, NLAT)
              stgK_ring = Ring(2, [RANKS, 2, NSTG * 128], BF16)
              stgV_ring = Ring(2, [RANKS, NSTG, 260], BF16)
              hal_st = A.alloc([RANKS, 2 * 258], BF16); Bhal = Buf()
              kct_ring = Ring(2, [256], BF16)
              gl_ring = Ring(2, [4, 512], F32)
              ln_ring = Ring(2, [16], F32)
              vn_ring = Ring(2, [256], BF16)
              yb_ring = Ring(2, [256], BF16)
              sq_ring = Ring(2, [256], F32)

              stg = {}

              def p1_tile(t):
                  r = 1 if t < 2 else 0
                  xt, Bx = x_ring.next()
                  src, sdeps = xsrc(l, t)
                  dma("sp", xt, src, reads=sdeps, writes=[Bx])
                  sc, Bsc = sc_ring.next()
                  t1, Bt1 = t1_ring.next()
                  op("dve", TT(t1, xt, xt, ALU.mult), reads=[Bx], writes=[Bt1])
                  op("dve", RSUM(sc[:, 0:1], t1), reads=[Bt1], writes=[Bsc])
                  rstd_from_ss(sc[:, 0:1], Bsc, sc[:, 1:2], Bsc, sc[:, 2:3], Bsc, D)
                  op("dve", STT(t1, xt, sc[:, 2:3], G1[r], ALU.mult, ALU.mult), reads=[Bx, Bsc, BG1[r]], writes=[Bt1])
                  h, Bh = h_ring.next()
                  op("dve", TT(h, t1, SH1[r], ALU.add), reads=[Bt1, BSH1[r]], writes=[Bh])
                  yield
                  for kc in range(8):
                      op("pe", TR(psb[0][:, kc * 128:(kc + 1) * 128], h[:, kc * 128:(kc + 1) * 128], ident_b), reads=[Bh, Bidb], writes=[PS[0]])
                  hT, BhT = hT_ring.next()
                  op("act", ACT(hT, psb[0].rearrange("p (k t) -> p k t", k=8), AF.Copy), reads=[PS[0]], writes=[BhT])
                  yield
                  for j in range(4):
                      for kc in range(8):
                          op("pe", MM(ps[:, 1 + j, :], hT[:, kc, :], win[:, kc, j * 512:(j + 1) * 512], start=(kc == 0), stop=(kc == 7)),
                             reads=[BhT, Bwin], writes=[PS[1 + j]])
                  ck(('p1b', l))
                  p_sb, Bp = psb_ring.next()
                  op("act", ACT(p_sb[:, 0:512], ps[:, 1, :], AF.Copy), reads=[PS[1]], writes=[Bp])
                  op("act", ACT(p_sb[:, 512:1024], ps[:, 2, :], AF.Copy), reads=[PS[2]], writes=[Bp])
                  op("dve", CP(p_sb[:, 1024:1536], ps[:, 3, :]), reads=[PS[3]], writes=[Bp])
                  op("dve", CP(p_sb[:, 1536:2048], ps[:, 4, :]), reads=[PS[4]], writes=[Bp])
                  yield
                  ropet, Brope = rope_ring.next()
                  dma("sp", ropet, I["rope"][:, t * 96:(t + 1) * 96], writes=[Brope])
                  qk, Bqk = qk_ring.next()
                  rt, Brt = rt_ring.next()
                  for (c0, nh, dd, tb0, o0) in ((0, 10, 16, 0, 0), (1280, 16, 8, 64, 640)):
                      w = nh * 4 * dd
                      xv = p_sb[:, c0:c0 + w].rearrange("p (h r t d) -> p h r t d", h=nh, r=2, t=2, d=dd)
                      ov = qk[:, o0:o0 + w].rearrange("p (h r t d) -> p h r t d", h=nh, r=2, t=2, d=dd)
                      cosv = ropet[:, tb0:tb0 + 2 * dd].rearrange("p (r d) -> p r d", r=2).unsqueeze(1).broadcast_to([128, nh, 2, dd])
                      sinv = ropet[:, tb0 + 2 * dd:tb0 + 4 * dd].rearrange("p (r d) -> p r d", r=2).unsqueeze(1).broadcast_to([128, nh, 2, dd])
                      tv = [rt[:, i, 0:nh * 2 * dd].rearrange("p (h r d) -> p h r d", h=nh, r=2, d=dd) for i in range(4)]
                      x1, x2 = xv[:, :, :, 0, :], xv[:, :, :, 1, :]
                      op("dve", TT(tv[0], x1, cosv, ALU.mult), reads=[Bp, Brope], writes=[Brt])
                      op("dve", TT(tv[1], x2, sinv, ALU.mult), reads=[Bp, Brope], writes=[Brt])
                      op("dve", TT(tv[2], x1, sinv, ALU.mult), reads=[Bp, Brope], writes=[Brt])
                      op("dve", TT(tv[3], x2, cosv, ALU.mult), reads=[Bp, Brope], writes=[Brt])
                      op("dve", TT(ov[:, :, :, 0, :], tv[0], tv[1], ALU.subtract), reads=[Brt], writes=[Bqk])
                      op("dve", TT(ov[:, :, :, 1, :], tv[2], tv[3], ALU.add), reads=[Brt], writes=[Bqk])
                  ck(('p1c', l))
                  op("pool", CP(VA[:, t, :].rearrange("p (g d) -> p g d", g=2)[:, :, 0:64],
                                p_sb[:, 640:768].rearrange("p (g d) -> p g d", g=2)), reads=[Bp], writes=[BVA[t]])
                  op("pool", CP(vt[:, :, 0:64], p_sb[:, 1792:2048].rearrange("p (h d) -> p h d", h=4)), reads=[Bp], writes=[Bvt])
                  if t < 2:
                      dma("sp", vctx_d[t], vt.rearrange("p h d -> p (h d)"), reads=[Bvt], writes=[db("vctx")])
                  else:
                      j = (t - 2) % NSTG
                      if j == 0:
                          stg["K"], stg["BK"] = stgK_ring.next()
                          stg["V"], stg["BV"] = stgV_ring.next()
                      stgK, BstK, stgV, BstV = stg["K"], stg["BK"], stg["V"], stg["BV"]
                      for rr in range(RANKS):
                          op("pool", TS(stgV[:, rr, j, :], vt.rearrange("p h d -> p (h d)"), sel[:, rr:rr + 1], ALU.mult, 1.0, ALU.mult),
                             reads=[Bvt, Bsel], writes=[BstV])
                  yield
                  ck(('p1d', l))
                  for jj in range(4):
                      op("pe", TR(psb[5][:, jj * 128:(jj + 1) * 128], qk[:, jj * 128:(jj + 1) * 128], ident_b),
                         reads=[Bqk, Bidb], writes=[PS[5]])
                  op("pe", TR(psb[5][:, 512:640], qk[:, 512:640], ident_b), reads=[Bqk, Bidb], writes=[PS[5]])
                  for c in range(4):
                      op("pe", TR(psb[6][:, c * 128:(c + 1) * 128], qk[:, 640 + c * 128:640 + (c + 1) * 128], ident_b),
                         reads=[Bqk, Bidb], writes=[PS[6]])
                  ck(('p1d1', l))
                  qta_t, Bqta = qta_ring.next()
                  op("dve", CP(qta_t, psb[5][:, 0:512]), reads=[PS[5]], writes=[Bqta])
                  dma("sp", qta_d[t], qta_t, reads=[Bqta], writes=[db("qta", t)])
                  ck(('p1d2', l))
                  op("act", ACT(KTA[:, t * 128:(t + 1) * 128], psb[5][:, 512:640], AF.Copy), reads=[PS[5]], writes=[BKTA[t]])
                  ck(('p1d3', l))
                  qtc_t, Bqtc = qtc_ring.next()
                  op("dve", CP(qtc_t, psb[6][:, 0:256]), reads=[PS[6]], writes=[Bqtc])
                  dma("sp", qtc_d[t], qtc_t, reads=[Bqtc], writes=[db("qtc", t)])
                  ck(('p1d4', l))
                  if t < 2:
                      kct, Bkct = kct_ring.next()
                      op("act", ACT(kct, psb[6][:, 256:512], AF.Copy), reads=[PS[6]], writes=[Bkct])
                      dma("sp", kctx_d[:, :, t * 128:(t + 1) * 128], kct.rearrange("p (c k) -> p c k", c=2), reads=[Bkct], writes=[db("kctx")])
                  else:
                      j = (t - 2) % NSTG
                      for rr in range(RANKS):
                          op("act", ACT(stgK[:, rr, :, j * 128:(j + 1) * 128], psb[6][:, 256:512].rearrange("p (c k) -> p c k", c=2), AF.Copy,
                                        scale=sel[:, rr:rr + 1]), reads=[PS[6], Bsel], writes=[BstK])
                      if j == NSTG - 1 or t == NT - 1:
                          g0 = (t - 2) - j
                          ng = j + 1
                          for rr in range(RANKS):
                              dma("sp", xk_s[rr, :, :, g0 * 128:(g0 + ng) * 128], stgK[:, rr, :, 0:ng * 128], reads=[BstK], writes=[db("xk_s", rr)])
                              dma("sp", xv_s[rr, g0:g0 + ng].rearrange("j p f -> p j f"), stgV[:, rr, 0:ng, :], reads=[BstV], writes=[db("xv_s", rr)])
                      if t == 2 or t == NT - 1:
                          w = 0 if t == 2 else 1
                          for rr in range(RANKS):
                              op("pool", TS(hal_st[:, rr, w * 258:w * 258 + 128], KTA[:, t * 128:(t + 1) * 128], sel[:, rr:rr + 1], ALU.mult, 1.0, ALU.mult),
                                 reads=[BKTA[t], Bsel], writes=[Bhal])
                              op("pool", TS(hal_st[:, rr, w * 258 + 128:(w + 1) * 258], VA[:, t, :], sel[:, rr:rr + 1], ALU.mult, 1.0, ALU.mult),
                                 reads=[BVA[t], Bsel], writes=[Bhal])
                  yield
                  ck(('p1e', l))
                  gl, Bgl = gl_ring.next()
                  xg, x2, th, gg = gl[:, 0, :], gl[:, 1, :], gl[:, 2, :], gl[:, 3, :]
                  op("dve", CP(xg, p_sb[:, 768:1280]), reads=[Bp], writes=[Bgl])
                  op("dve", TT(x2, xg, xg, ALU.mult), reads=[Bgl], writes=[Bgl])
                  op("dve", TS(x2, x2, 0.044715, ALU.mult, 1.0, ALU.add), reads=[Bgl], writes=[Bgl])
                  op("dve", TT(x2, x2, xg, ALU.mult), reads=[Bgl], writes=[Bgl])
                  op("act", ACT(th, x2, AF.Tanh, scale=math.sqrt(2.0 / math.pi)), reads=[Bgl], writes=[Bgl])
                  op("dve", TS(th, th, 0.5, ALU.mult, 0.5, ALU.add), reads=[Bgl], writes=[Bgl])
                  op("dve", TT(gg, th, xg, ALU.mult), reads=[Bgl], writes=[Bgl])
                  ug = gg[:, 0:256]
                  vg = gg[:, 256:512].rearrange("p (g d) -> p g d", g=4)
                  ln, Bln = ln_ring.next()
                  sq, Bsq = sq_ring.next()
                  op("dve", RSUM(ln[:, 0:4], vg), reads=[Bgl], writes=[Bln])
                  op("dve", TT(sq, gg[:, 256:512], gg[:, 256:512], ALU.mult), reads=[Bgl], writes=[Bsq])
                  op("dve", RSUM(ln[:, 4:8], sq.rearrange("p (g d) -> p g d", g=4)), reads=[Bsq, Bln], writes=[Bln])
                  op("dve", TS(ln[:, 0:4], ln[:, 0:4], 1.0 / 64, ALU.mult), reads=[Bln], writes=[Bln])
                  op("dve", TT(ln[:, 8:12], ln[:, 0:4], ln[:, 0:4], ALU.mult), reads=[Bln], writes=[Bln])
                  op("dve", STT(ln[:, 4:8], ln[:, 4:8], 1.0 / 64, ln[:, 8:12], ALU.mult, ALU.subtract), reads=[Bln], writes=[Bln])
                  op("act", ACT(ln[:, 8:12], ln[:, 4:8], AF.Ln, bias=eps_t), reads=[Bln, Beps], writes=[Bln])
                  op("act", ACT(ln[:, 12:16], ln[:, 8:12], AF.Exp, scale=-0.5), reads=[Bln], writes=[Bln])
                  for g in range(4):
                      op("dve", TS(sq[:, g * 64:(g + 1) * 64], vg[:, g, :], ln[:, g:g + 1], ALU.subtract, ln[:, 12 + g:13 + g], ALU.mult),
                         reads=[Bgl, Bln, Bsq], writes=[Bsq])
                  yield
                  vn, Bvn = vn_ring.next()
                  op("dve", TT(vn, sq, gtokb, ALU.mult), reads=[Bsq, Bgtok], writes=[Bvn])
                  for g in range(4):
                      op("pe", MM(ps[:, 7, g * 64:(g + 1) * 64], wsT[:, g, :], vn[:, g * 64:(g + 1) * 64], start=True, stop=True, skip_group_check=True),
                         reads=[BwsT, Bvn], writes=[PS[7]])
                  ybt, Byb = yb_ring.next()
                  for g in range(4):
                      op("dve", STT(ybt[:, g * 64:(g + 1) * 64], ps[:, 7, g * 64:(g + 1) * 64], bsT[:, g:g + 1], ug[:, g * 64:(g + 1) * 64], ALU.add, ALU.mult),
                         reads=[PS[7], BbsT, Bgl], writes=[Byb])
                  dma("sp", yb_d[t], ybt, reads=[Byb], writes=[db("yb", t)])

              def run_interleaved(gens, depth):
                  active = []
                  it = iter(gens)
                  done = False
                  while True:
                      while len(active) < depth and not done:
                          try:
                              active.append(next(it))
                          except StopIteration:
                              done = True
                      if not active:
                          break
                      for g_ in list(active):
                          try:
                              next(g_)
                          except StopIteration:
                              active.remove(g_)

              run_interleaved((p1_tile(t) for t in range(NT)), 2)
              ck(('p1f', l))
              for rr in range(RANKS):
                  dma("sp", xh_s[rr], hal_st[:, rr, :], reads=[Bhal], writes=[db("xh_s", rr)])
              A.release()
              fw.barrier()
              if cfg.stop_after == ("p1", l):
                  break

              if RANKS > 1:
                  groups = [[b * RANKS + r for r in range(RANKS)] for b in range(cfg.NB)]
                  fw.cc(lambda e: e.collective_compute("AllReduce", ALU.add, replica_groups=groups,
                                                       ins=[xh_s.rearrange("r p f -> (r p) f")], outs=[xh_r.rearrange("r p f -> (r p) f")]),
                        reads=[db("xh_s", rr) for rr in range(RANKS)], writes=[db("xh_r")])
                  for rr in range(RANKS):
                      fw.cc(lambda e, rr=rr: e.collective_compute("AllReduce", ALU.add, replica_groups=groups,
                                                                  ins=[xk_s[rr].rearrange("p c k -> p (c k)")],
                                                                  outs=[xk_r[rr].rearrange("p c k -> p (c k)")]),
                            reads=[db("xk_s", rr)], writes=[db("xk_r", rr)])
                      fw.cc(lambda e, rr=rr: e.collective_compute("AllReduce", ALU.add, replica_groups=groups,
                                                                  ins=[xv_s[rr].rearrange("j p f -> (j p) f")],
                                                                  outs=[xv_r[rr].rearrange("j p f -> (j p) f")]),
                            reads=[db("xv_s", rr)], writes=[db("xv_r", rr)])
                  kvr = lambda name, rr: db(name + "_r", rr)
              else:
                  kvr = lambda name, rr: db(name + "_s", rr)
              Bxh = db("xh_r") if RANKS > 1 else db("xh_s", 0)

              A.mark()
              hal = A.alloc([RANKS, 2 * 258], BF16); Bhl = Buf()
              dma("sp", hal, xh_r.rearrange("r p f -> p r f"), reads=[Bxh], writes=[Bhl])
              hacc = A.alloc([2, 258], F32); Bhacc = Buf()
              for w, so in ((0, 2 * RANKS), (1, RANKS)):
                  for rr in range(RANKS):
                      src = hal[:, rr, w * 258:(w + 1) * 258]
                      if rr == 0:
                          op("dve", TS(hacc[:, w, :], src, sel[:, so + rr:so + rr + 1], ALU.mult), reads=[Bhl, Bsel], writes=[Bhacc])
                      else:
                          op("dve", STT(hacc[:, w, :], src, sel[:, so + rr:so + rr + 1], hacc[:, w, :], ALU.mult, ALU.add),
                             reads=[Bhl, Bsel, Bhacc], writes=[Bhacc])
              op("dve", CP(KTA[:, NT * 128:(NT + 1) * 128], hacc[:, 1, 0:128]), reads=[Bhacc], writes=[BKTA[NT]])
              op("dve", CP(VA[:, NT, :], hacc[:, 1, 128:258]), reads=[Bhacc], writes=[BVA[NT]])
              op("dve", CP(KTA[:, (NT + 1) * 128:(NT + 2) * 128], hacc[:, 0, 0:128]), reads=[Bhacc], writes=[BKTA[NT + 1]])
              op("dve", CP(VA[:, NT + 1, :], hacc[:, 0, 128:258]), reads=[Bhacc], writes=[BVA[NT + 1]])

              wout = A.alloc([8, D], BF16); Bwout = Buf()
              for kc in range(8):
                  dma("pool", wout[:, kc, :], I["w_out"][l, kc * 128:(kc + 1) * 128, :], writes=[Bwout])
              wr = A.alloc([8, 20], F32); Bwr = Buf()
              dma("sp", wr, I["w_r"][l].rearrange("(k p) n -> p k n", p=128), writes=[Bwr])
              brb = A.alloc([20], F32); Bbr = Buf()
              dma("sp", brb, I["b_r"][l:l + 1, :].broadcast_to([128, 20]), writes=[Bbr])
              GT1 = A.alloc([D], F32); BGT1 = Buf()
              G2 = A.alloc([D], F32); BG2 = Buf()
              SH2 = A.alloc([D], F32); BSH2 = Buf()
              gtmp2 = A.alloc([D], F32); Bgtmp2 = Buf()

              def load_p2_mods(r):
                  load_mod(l, r, 2, GT1, BGT1)
                  load_mod(l, r, 4, G2, BG2)
                  load_gain(I["g_ffn"][l:l + 1, :], gtmp2, Bgtmp2)
                  op("dve", STT(G2, G2, 1.0, gtmp2, ALU.add, ALU.mult), reads=[BG2, Bgtmp2], writes=[BG2])
                  load_mod(l, r, 3, SH2, BSH2)

              kc_ring = Ring(3, [2, KCH * 128], BF16)
              vc_ring = Ring(3, [KCH, 260], BF16)
              qb_ring = Ring(2, [2, 256], BF16)
              E_aps = [[A.alloc([1024], BF16) for _ in range(2)] for _ in range(2)]
              E_buf = [[Buf() for _ in range(2)] for _ in range(2)]
              usb = A.alloc([2, 512], F32); Busb = Buf()
              accS = [A.alloc([1024], F32) for _ in range(2)]; BaccS = [Buf(), Buf()]
              epair = [A.alloc([1024], BF16) for _ in range(2)]; Bepair = [Buf(), Buf()]
              uasb = A.alloc([2, 512], F32); Buasb = Buf()
              qa_ring = Ring(2, [512], BF16)
              eab_ring = Ring(2, [5, 512], BF16)
              fsc_ring = Ring(4, [48], F32)
              osb_ring = Ring(2, [256], F32)
              ojk = A.alloc([256], F32); Bojk = Buf()
              ymix_ring = Ring(2, [D], BF16)
              ymT_ring = Ring(2, [8, 128], BF16)
              x2_ring = Ring(2, [D], F32)
              yt_ring = Ring(2, [D], F32)
              xm_ring = Ring(2, [D], F32)
              h2_ring = Ring(2, [D], F32)
              h2Tf_ring = Ring(2, [8, 128], F32)
              h2Tb_ring = Ring(2, [8, 128], BF16)
              rsc_ring = Ring(2, [96], F32)
              junk2 = A.alloc([D], BF16); Bjunk2 = Buf()

              qblocks = []
              if not last:
                  qblocks.append(([0, 1], 1, "ctx"))
              for i in range(0, NLAT, 2):
                  qblocks.append(([2 + i, 3 + i] if i + 1 < NLAT else [2 + i], 0, "lat"))
              cur_r = None
              pending = []
              pend_ptr = [0]

              def step_pending():
                  if not pending:
                      return
                  k = pend_ptr[0] % len(pending)
                  try:
                      next(pending[k])
                      pend_ptr[0] += 1
                  except StopIteration:
                      pending.pop(k)

              def flush_pending():
                  while pending:
                      step_pending()

              for (tiles, r, kind) in qblocks:
                  if r != cur_r:
                      load_p2_mods(r)
                      cur_r = r
                  nq = len(tiles) * 128
                  chunks = [(kctx_d, vctx_d.rearrange("j p f -> p j f"), 2, [db("kctx"), db("vctx")])]
                  if kind == "lat":
                      for rr in range(RANKS):
                          for c0 in range(0, NLAT, KCH):
                              chunks.append((xk_r[rr, :, :, c0 * 128:(c0 + KCH) * 128], xv_r[rr, c0:c0 + KCH].rearrange("j p f -> p j f"), KCH,
                                             [kvr("xk", rr), kvr("xv", rr)]))
                  Qb, BQb = qb_ring.next()
                  for j, t in enumerate(tiles):
                      dma("sp", Qb[:, :, j * 128:(j + 1) * 128], qtc_d[t].rearrange("p (c k) -> p c k", c=2), reads=[db("qtc", t)], writes=[BQb])
                  ktl = []
                  loaded = {}

                  def load_chunk(ci):
                      ksrc, vsrc, n, deps = chunks[ci]
                      kc_t, Bkc = kc_ring.next()
                      vc_t, Bvc = vc_ring.next()
                      dma("sp", kc_t[:, :, 0:n * 128], ksrc, reads=deps, writes=[Bkc])
                      dma("sp", vc_t[:, 0:n, :], vsrc, reads=deps, writes=[Bvc])
                      loaded[ci] = (kc_t, Bkc, vc_t, Bvc)

                  for ci, ch in enumerate(chunks):
                      for kk in range(ch[2]):
                          ktl.append((ci, kk))
                  nk = len(ktl)
                  load_chunk(0)
                  if len(chunks) > 1:
                      load_chunk(1)

                  def qk(i, X):
                      ci, kk = ktl[i]
                      kc_t, Bkc, _, _ = loaded[ci]
                      for c in range(2):
                          for gg in range(2):
                              g = 2 * X + gg
                              op("pe", MM(ps[:, 2 * X + gg, c * 256:c * 256 + nq], kc_t[32 * g:32 * g + 32, c, kk * 128:(kk + 1) * 128],
                                          Qb[32 * g:32 * g + 32, c, 0:nq], tile_position=(32 * g, 0)),
                                 reads=[Bkc, BQb], writes=[PS[2 * X], PS[2 * X + 1]])

                  def ex(i, X):
                      Et, Eb = E_aps[X][i % 2], E_buf[X][i % 2]
                      op("act", ACT(Et.rearrange("p (c g q) -> p g c q", c=2, g=2)[:, :, :, 0:nq],
                                    ps[:, 2 * X:2 * X + 2, :].rearrange("p g (c q) -> p g c q", c=2)[:, :, :, 0:nq], AF.Exp, scale=SCALE_C),
                         reads=[PS[2 * X], PS[2 * X + 1]], writes=[Eb])

                  def pv(i, X):
                      ci, kk = ktl[i]
                      _, _, vc_t, Bvc = loaded[ci]
                      Et, Eb = E_aps[X][i % 2], E_buf[X][i % 2]
                      for c in range(2):
                          hh = 2 * c + X
                          rhs = Et[:, c * 512:(c + 1) * 512].rearrange("p (g q) -> p g q", g=2)[:, :, 0:nq]
                          o = ps[64 * c:64 * c + 64, 4 + X, :].rearrange("p (g q) -> p g q", g=2)[:, :, 0:nq]
                          op("pe", MM(o, vc_t[:, kk, hh * 65:hh * 65 + 64], rhs, start=(i == 0), stop=(i == nk - 1), tile_position=(0, 64 * c),
                                      skip_group_check=True), reads=[Bvc, Eb], writes=[PS[4 + X]])
                      if i % 2 == 1:
                          Ep = E_aps[X][(i - 1) % 2]
                          Epb = E_buf[X][(i - 1) % 2]
                          if i == 1:
                              op("dve", TT(accS[X], Ep, Et, ALU.add), reads=[Epb, Eb], writes=[BaccS[X]])
                          else:
                              op("dve", TT(epair[X], Ep, Et, ALU.add), reads=[Epb, Eb], writes=[Bepair[X]])
                              op("dve", TT(accS[X], accS[X], epair[X], ALU.add), reads=[Bepair[X], BaccS[X]], writes=[BaccS[X]])
                      elif i == nk - 1:
                          if i == 0:
                              op("dve", CP(accS[X], Et), reads=[Eb], writes=[BaccS[X]])
                          else:
                              op("dve", TT(accS[X], accS[X], Et, ALU.add), reads=[Eb, BaccS[X]], writes=[BaccS[X]])

                  adv = max(1, nk // 22)
                  qk(0, 0)
                  qk(0, 1)
                  for i in range(nk):
                      ci, kk = ktl[i]
                      if kk == 0 and ci + 2 < len(chunks) and (ci + 2) not in loaded:
                          load_chunk(ci + 2)
                      for X in range(2):
                          ex(i, X)
                          if i + 1 < nk:
                              qk(i + 1, X)
                          pv(i, X)
                      if pending and i % adv == adv // 2:
                          step_pending()
                  flush_pending()

                  op("act", ACT(usb[:, 0:2, :], ps[:, 4:6, :], AF.Copy), reads=[PS[4], PS[5]], writes=[Busb])
                  for j, t in enumerate(tiles):
                      for X in range(2):
                          for c in range(2):
                              for gg in range(2):
                                  hm = (2 * c + X) * 2 + gg
                                  col = j * 8 + hm
                                  op("pe", MM(ps[:, 6, col:col + 1], accS[X][:, (c * 2 + gg) * 256 + j * 128:(c * 2 + gg) * 256 + (j + 1) * 128], ones1,
                                              start=True, stop=True, skip_group_check=True), reads=[BaccS[X], Bones], writes=[PS[6]])
                  for j, t in enumerate(tiles):
                      for X in range(2):
                          for m in range(2):
                              op("pe", TR(ps[:, j, (X * 2 + m) * 128:(X * 2 + m + 1) * 128], usb[:, X, m * 256 + j * 128:m * 256 + (j + 1) * 128], ident_f),
                                 reads=[Busb, Bidf], writes=[PS[j]])
                  ymix_l = []
                  for j, t in enumerate(tiles):
                      PU = [PS[j]]
                      fs, Bfs = fsc_ring.next()
                      rcp = fs[:, 0:8]
                      op("dve", lambda e, rcp=rcp, j=j: e.reciprocal(out=rcp, in_=ps[:, 6, j * 8:(j + 1) * 8]), reads=[PS[6]], writes=[Bfs])
                      nrl = fs[:, 8:12]
                      op("dve", TS(nrl, rcp.rearrange("p (h m) -> p h m", m=2)[:, :, 1], neglam, ALU.mult), reads=[Bfs, Blsc], writes=[Bfs])
                      osb, Bosb = osb_ring.next()
                      for hh in range(4):
                          X_, c_ = hh % 2, hh // 2
                          u1 = ps[:, j, ((X_ * 2 + 0) * 2 + c_) * 64:((X_ * 2 + 0) * 2 + c_) * 64 + 64]
                          u2 = ps[:, j, ((X_ * 2 + 1) * 2 + c_) * 64:((X_ * 2 + 1) * 2 + c_) * 64 + 64]
                          op("dve", TS(osb[:, hh * 64:(hh + 1) * 64], u1, rcp[:, 2 * hh:2 * hh + 1], ALU.mult), reads=PU + [Bfs], writes=[Bosb])
                          op("dve", STT(osb[:, hh * 64:(hh + 1) * 64], u2, nrl[:, hh:hh + 1], osb[:, hh * 64:(hh + 1) * 64], ALU.mult, ALU.add),
                             reads=PU + [Bfs, Bosb], writes=[Bosb])
                      op("dve", TT(ojk, osb, osb, ALU.mult), reads=[Bosb], writes=[Bojk])
                      op("dve", RSUM(fs[:, 12:16], ojk.rearrange("p (h d) -> p h d", h=4)), reads=[Bojk, Bfs], writes=[Bfs])
                      op("act", ACT(fs[:, 16:20], fs[:, 12:16], AF.Ln, scale=1.0 / 64, bias=eps_t), reads=[Bfs, Beps], writes=[Bfs])
                      op("act", ACT(fs[:, 20:24], fs[:, 16:20], AF.Exp, scale=-0.5), reads=[Bfs], writes=[Bfs])
                      ymix, Bym = ymix_ring.next()
                      for hh in range(4):
                          op("dve", STT(ymix[:, 768 + hh * 64:768 + (hh + 1) * 64], osb[:, hh * 64:(hh + 1) * 64], fs[:, 20 + hh:21 + hh], gsub_s,
                                        ALU.mult, ALU.mult), reads=[Bosb, Bfs, Bgsub], writes=[Bym])
                      dma("sp", ymix[:, 512:768], yb_d[t], reads=[db("yb", t)], writes=[Bym])
                      ymix_l.append((ymix, Bym))
                  def tail_tile(j, t):
                      ymix, Bym = ymix_l[j]
                      fs, Bfs = fsc_ring.next()
                      qa, Bqa = qa_ring.next()
                      dma("sp", qa, qta_d[t], reads=[db("qta", t)], writes=[Bqa])
                      if kind == "ctx":
                          slots = [(0, None), (1, None)]
                      else:
                          pslot = (t - 1, 0) if t > 2 else (NT, 2)
                          nslot = (t + 1, 1) if t < NT - 1 else (NT + 1, 3)
                          slots = [(0, None), (1, None), pslot, (t, None), nslot]
                      ns = len(slots)
                      for g in range(2):
                          for k_i, (slot, mk) in enumerate(slots):
                              op("pe", MM(ps[:, k_i, :], KTA[64 * g:64 * g + 64, slot * 128:(slot + 1) * 128], qa[64 * g:64 * g + 64, :],
                                          start=True, stop=(mk is None), tile_position=(64 * g, 0)), reads=[BKTA[slot], Bqa], writes=[PS[k_i]])
                              if mk is not None:
                                  op("pe", MM(ps[:, k_i, :], ident_b, mneg[:, mk, :], start=False, stop=True), reads=[Bidb, Bmneg], writes=[PS[k_i]])
                          eab, Beab = eab_ring.next()
                          op("act", ACT(eab[:, 0:ns, :], ps[:, 0:ns, :], AF.Exp, scale=SCALE_A), reads=PS[0:ns], writes=[Beab])
                          for k_i, (slot, mk) in enumerate(slots):
                              op("pe", MM(ps[0:65, 6 + g, :], VA[:, slot, g * 65:(g + 1) * 65], eab[:, k_i, :], start=(k_i == 0), stop=(k_i == ns - 1)),
                                 reads=[BVA[slot], Beab], writes=[PS[6 + g]])
                      op("act", ACT(uasb[0:65, :, :], ps[0:65, 6:8, :], AF.Copy), reads=[PS[6], PS[7]], writes=[Buasb])
                      for g in range(2):
                          for jh in range(4):
                              op("pe", TR(ps[:, 4 + g, jh * 65:(jh + 1) * 65], uasb[0:65, g, jh * 128:(jh + 1) * 128], ident_f[0:65, 0:65]),
                                 reads=[Buasb, Bidf], writes=[PS[4 + g]])
                      UAT = ps[:, 4:6, 0:260].rearrange("p g (j d) -> p g j d", j=4)
                      den = fs[:, 24:32]
                      op("dve", TT(den.rearrange("p (g j) -> p g j", g=2), UAT[:, :, :, 64], esink.rearrange("p (g j) -> p g j", g=2), ALU.add),
                         reads=[PS[4], PS[5], Besink, Bfs], writes=[Bfs])
                      op("dve", lambda e, den=den: e.reciprocal(out=den, in_=den), reads=[Bfs], writes=[Bfs])
                      for g in range(2):
                          op("dve", TT(ymix[:, g * 256:(g + 1) * 256].rearrange("p (j d) -> p j d", j=4), UAT[:, g, :, 0:64],
                                       den[:, g * 4:(g + 1) * 4].unsqueeze(2).broadcast_to([128, 4, 64]), ALU.mult),
                             reads=[PS[4 + g], Bfs], writes=[Bym])
                      yield

                  def tail2(j, t, ymix, Bym):
                      for kc in range(8):
                          op("pe", TR(psb[6][:, kc * 128:(kc + 1) * 128], ymix[:, kc * 128:(kc + 1) * 128], ident_b), reads=[Bym, Bidb], writes=[PS[6]])
                      ymT, BymT = ymT_ring.next()
                      op("act", ACT(ymT, psb[6].rearrange("p (k t) -> p k t", k=8), AF.Copy), reads=[PS[6]], writes=[BymT])
                      yield
                      x2t, Bx2 = x2_ring.next()
                      src, sdeps = xsrc(l, t)
                      dma("sp", x2t, src, reads=sdeps, writes=[Bx2])
                      ytm, Byt = yt_ring.next()
                      for hf in range(2):
                          for kc in range(8):
                              op("pe", MM(ps[:, 7, :], ymT[:, kc, :], wout[:, kc, hf * 512:(hf + 1) * 512], start=(kc == 0), stop=(kc == 7)),
                                 reads=[BymT, Bwout], writes=[PS[7]])
                          op("dve", TT(ytm[:, hf * 512:(hf + 1) * 512], ps[:, 7, :], GT1[:, hf * 512:(hf + 1) * 512], ALU.mult),
                             reads=[PS[7], BGT1], writes=[Byt])
                          yield
                      xm, Bxm = xm_ring.next()
                      op("dve", TT(xm, ytm, x2t, ALU.add), reads=[Byt, Bx2], writes=[Bxm])
                      dma("sp", xmid[t], xm, reads=[Bxm], writes=[db("xmid", t)])
                      yield
                      rs, Brs = rsc_ring.next()
                      op("dve", TT(ytm, xm, xm, ALU.mult), reads=[Bxm], writes=[Byt])
                      op("dve", RSUM(rs[:, 0:1], ytm), reads=[Byt], writes=[Brs])
                      rstd_from_ss(rs[:, 0:1], Brs, rs[:, 1:2], Brs, rs[:, 2:3], Brs, D)
                      op("dve", STT(ytm, xm, rs[:, 2:3], G2, ALU.mult, ALU.mult), reads=[Bxm, Brs, BG2], writes=[Byt])
                      h2, Bh2 = h2_ring.next()
                      op("dve", TT(h2, ytm, SH2, ALU.add), reads=[Byt, BSH2], writes=[Bh2])
                      h2Tf, Bh2Tf = h2Tf_ring.next()
                      h2Tb, Bh2Tb = h2Tb_ring.next()
                      for hb in range(2):
                          yield
                          for kq in range(4):
                              kc = 4 * hb + kq
                              op("pe", TR(ps[:, 6, kq * 128:(kq + 1) * 128], h2[:, kc * 128:(kc + 1) * 128], ident_f), reads=[Bh2, Bidf], writes=[PS[6]])
                          h2ps = ps[:, 6, :].rearrange("p (k t) -> p k t", k=4)
                          op("act", ACT(h2Tf[:, 4 * hb:4 * hb + 4, :], h2ps, AF.Copy), reads=[PS[6]], writes=[Bh2Tf])
                          op("dve", CP(h2Tb[:, 4 * hb:4 * hb + 4, :], h2ps), reads=[PS[6]], writes=[Bh2Tb])
                      dma("sp", h2t_d[:, :, t * 128:(t + 1) * 128], h2Tb, reads=[Bh2Tb], writes=[db("h2t", t)])
                      yield
                      for kc in range(8):
                          op("pe", MM(ps[:, 7, 0:20], h2Tf[:, kc, :], wr[:, kc, :], start=(kc == 0), stop=(kc == 7)), reads=[Bh2Tf, Bwr], writes=[PS[7]])
                      lg = rs[:, 4:24]
                      op("dve", TT(lg, ps[:, 7, 0:20], brb, ALU.add), reads=[PS[7], Bbr, Brs], writes=[Brs])
                      yield
                      lgG = lg[:, 0:4]
                      le = lg[:, 4:20].rearrange("p (g j) -> p g j", g=4)
                      mx = rs[:, 24:25]
                      op("dve", RMAX(mx, lgG), reads=[Brs], writes=[Brs])
                      shf = rs[:, 25:29]
                      op("dve", TS(shf, lgG, mx, ALU.subtract), reads=[Brs], writes=[Brs])
                      exg = rs[:, 29:33]
                      sume = rs[:, 33:34]
                      op("act", ACT(exg, shf, AF.Exp), reads=[Brs], writes=[Brs])
                      op("dve", RSUM(sume, exg), reads=[Brs], writes=[Brs])
                      ptop = rs[:, 34:35]
                      op("dve", lambda e, ptop=ptop, sume=sume: e.reciprocal(out=ptop, in_=sume), reads=[Brs], writes=[Brs])
                      oh = rs[:, 35:39]
                      op("dve", TS(oh, lgG, mx, ALU.is_equal), reads=[Brs], writes=[Brs])
                      tmp16 = rs[:, 40:56]
                      op("dve", TT(tmp16.rearrange("p (g j) -> p g j", g=4), le, oh.unsqueeze(2).broadcast_to([128, 4, 4]), ALU.mult), reads=[Brs], writes=[Brs])
                      leg = rs[:, 56:60]
                      op("dve", RSUM(leg, tmp16.rearrange("p (g j) -> p j g", g=4)), reads=[Brs], writes=[Brs])
                      m1 = rs[:, 60:61]
                      op("dve", RMAX(m1, leg), reads=[Brs], writes=[Brs])
                      mk1 = rs[:, 61:65]
                      op("dve", TS(mk1, leg, m1, ALU.is_equal), reads=[Brs], writes=[Brs])
                      le2 = rs[:, 65:69]
                      op("dve", STT(le2, mk1, -1e30, leg, ALU.mult, ALU.add), reads=[Brs], writes=[Brs])
                      m2 = rs[:, 69:70]
                      op("dve", RMAX(m2, le2), reads=[Brs], writes=[Brs])
                      mk2 = rs[:, 70:74]
                      op("dve", TS(mk2, le2, m2, ALU.is_equal), reads=[Brs], writes=[Brs])
                      d21 = rs[:, 74:75]
                      op("dve", TT(d21, m2, m1, ALU.subtract), reads=[Brs], writes=[Brs])
                      e21 = rs[:, 75:76]
                      op("act", ACT(e21, d21, AF.Exp), reads=[Brs], writes=[Brs])
                      w1 = rs[:, 76:77]
                      op("dve", TS(w1, e21, 1.0, ALU.add), reads=[Brs], writes=[Brs])
                      op("dve", lambda e, w1=w1: e.reciprocal(out=w1, in_=w1), reads=[Brs], writes=[Brs])
                      op("dve", TT(w1, w1, ptop, ALU.mult), reads=[Brs], writes=[Brs])
                      w2 = rs[:, 77:78]
                      op("dve", TT(w2, w1, e21, ALU.mult), reads=[Brs], writes=[Brs])
                      gj = rs[:, 78:82]
                      op("dve", TS(gj, mk1, w1, ALU.mult), reads=[Brs], writes=[Brs])
                      op("dve", STT(gj, mk2, w2, gj, ALU.mult, ALU.add), reads=[Brs], writes=[Brs])
                      op("dve", TT(gates[:, t, :].rearrange("p (g j) -> p g j", g=4), oh.unsqueeze(2).broadcast_to([128, 4, 4]),
                                   gj.unsqueeze(1).broadcast_to([128, 4, 4]), ALU.mult), reads=[Brs], writes=[Bgate[t]])

                  run_interleaved((tail_tile(j, t) for j, t in enumerate(tiles)), 2)
                  if kind == "ctx":
                      run_interleaved((tail2(j, t, *ymix_l[j]) for j, t in enumerate(tiles)), 2)
                  else:
                      pending.extend([tail2(j, t, *ymix_l[j]) for j, t in enumerate(tiles)])
              flush_pending()
              A.release()
              A.release()
              fw.barrier()
              if cfg.stop_after == ("p2", l):
                  break

              A.mark()
              p3tiles = list(range(NT)) if not last else list(range(2, NT))
              if len(p3tiles) <= 9:
                  sblocks = [p3tiles]
              else:
                  hsz = (len(p3tiles) + 1) // 2
                  sblocks = [p3tiles[:hsz], p3tiles[hsz:]]
              SBT = max(len(x) for x in sblocks)
              GT2 = [A.alloc([D], F32) for _ in range(2)]; BGT2 = [Buf(), Buf()]
              for r in ((0,) if last else (0, 1)):
                  load_mod(l, r, 5, GT2[r], BGT2[r])
              if last:
                  gfin = A.alloc([D], F32); Bgfin = Buf()
                  load_gain(I["g_final"], gfin, Bgfin)
              h2sb = A.alloc([8, SBT * 128], BF16); Bh2sb = Buf()
              yacc = A.alloc([SBT, D], F32); Byacc = [Buf() for _ in range(SBT)]
              wg_ring = Ring(2, [8, DEXP], BF16)
              wu_ring = Ring(2, [8, DEXP], BF16)
              wd_ring = Ring(2, [4, D], BF16)
              sg_ring = Ring(2, [512], F32)
              he_ring = Ring(2, [4, 512], BF16)
              xm3_ring = Ring(2, [D], F32)
              fo_ring = Ring(2, [D], F32)
              f3_ring = Ring(2, [8], F32)
              junk3 = A.alloc([D], BF16); Bjunk3 = Buf()
              ycnt = 0
              for tl in sblocks:
                  ntok = len(tl) * 128
                  dma("sp", h2sb[:, :, 0:ntok], h2t_d[:, :, tl[0] * 128:(tl[-1] + 1) * 128], reads=[db("h2t", t) for t in tl], writes=[Bh2sb])
                  for e_ in range(NEXP):
                      wg, Bwg = wg_ring.next()
                      wu, Bwu = wu_ring.next()
                      wd, Bwd = wd_ring.next()
                      dma("pool", wg, I["w_gate"][l, e_].rearrange("(k p) n -> p k n", p=128), writes=[Bwg])
                      dma("pool", wu, I["w_up"][l, e_].rearrange("(k p) n -> p k n", p=128), writes=[Bwu])
                      dma("pool", wd, I["w_down"][l, e_].rearrange("(k p) n -> p k n", p=128), writes=[Bwd])
                      for b0 in range(0, ntok, 512):
                          n = min(512, ntok - b0)
                          he, Bhe = he_ring.next()
                          for dc in range(4):
                              gb, ub = dc % 2, 2 + dc % 2
                              for kc in range(8):
                                  op("pe", MM(ps[:, gb, 0:n], wg[:, kc, dc * 128:(dc + 1) * 128], h2sb[:, kc, b0:b0 + n], start=(kc == 0), stop=(kc == 7)),
                                     reads=[Bwg, Bh2sb], writes=[PS[gb]])
                              for kc in range(8):
                                  op("pe", MM(ps[:, ub, 0:n], wu[:, kc, dc * 128:(dc + 1) * 128], h2sb[:, kc, b0:b0 + n], start=(kc == 0), stop=(kc == 7)),
                                     reads=[Bwu, Bh2sb], writes=[PS[ub]])
                              sg, Bsg = sg_ring.next()
                              op("act", ACT(sg[:, 0:n], ps[:, gb, 0:n], AF.Silu), reads=[PS[gb]], writes=[Bsg])
                              op("dve", TT(he[:, dc, 0:n], sg[:, 0:n], ps[:, ub, 0:n], ALU.mult), reads=[Bsg, PS[ub]], writes=[Bhe])
                          for j in range(n // 128):
                              ti = b0 // 128 + j
                              t = tl[ti]
                              for hf in range(2):
                                  yb_ = 4 + (ycnt % 4)
                                  ycnt += 1
                                  for dc in range(4):
                                      op("pe", MM(ps[:, yb_, :], he[:, dc, j * 128:(j + 1) * 128], wd[:, dc, hf * 512:(hf + 1) * 512], start=(dc == 0), stop=(dc == 3)),
                                         reads=[Bhe, Bwd], writes=[PS[yb_]])
                                  ya_ = yacc[:, ti, hf * 512:(hf + 1) * 512]
                                  if e_ == 0:
                                      op("dve", TS(ya_, ps[:, yb_, :], gates[:, t, e_:e_ + 1], ALU.mult), reads=[PS[yb_], Bgate[t]], writes=[Byacc[ti]])
                                  else:
                                      op("dve", STT(ya_, ps[:, yb_, :], gates[:, t, e_:e_ + 1], ya_, ALU.mult, ALU.add),
                                         reads=[PS[yb_], Bgate[t], Byacc[ti]], writes=[Byacc[ti]])
                  for ti, t in enumerate(tl):
                      r = 1 if t < 2 else 0
                      xm3, Bxm3 = xm3_ring.next()
                      dma("sp", xm3, xmid[t], reads=[db("xmid", t)], writes=[Bxm3])
                      op("dve", TT(yacc[:, ti, :], yacc[:, ti, :], GT2[r], ALU.mult), reads=[Byacc[ti], BGT2[r]], writes=[Byacc[ti]])
                      xo, Bxo = xm3, Bxm3
                      op("dve", TT(xo, yacc[:, ti, :], xm3, ALU.add), reads=[Byacc[ti], Bxm3], writes=[Bxo])
                      if not last:
                          dma("sp", xbuf[t], xo, reads=[Bxo], writes=[db("xbuf", t)])
                      else:
                          f3, Bf3 = f3_ring.next()
                          fo, Bfo = fo_ring.next()
                          op("dve", TT(fo, xo, xo, ALU.mult), reads=[Bxo], writes=[Bfo])
                          op("dve", RSUM(f3[:, 0:1], fo), reads=[Bfo], writes=[Bf3])
                          rstd_from_ss(f3[:, 0:1], Bf3, f3[:, 1:2], Bf3, f3[:, 2:3], Bf3, D)
                          op("dve", STT(fo, xo, f3[:, 2:3], gfin, ALU.mult, ALU.mult), reads=[Bxo, Bf3, Bgfin], writes=[Bfo])
                          dma("sp", out_d[(t - 2) * 128:(t - 1) * 128, :], fo, reads=[Bfo], writes=[db("out", t)])
              A.release()
              A.release()
              fw.barrier()

        except StopBuild:
            pass
        fw.barrier()
        blk = st.enter_context(nc.Block())
        stats = fw.finish(blk)
    return nc, stats


def _rope_table(cfg, r):
    NT, NLAT = cfg.NT, cfg.NLAT
    tab = np.zeros((128, NT, 96), np.float32)
    tab[:, :, 0:32] = 1.0
    tab[:, :, 64:80] = 1.0
    invA = (np.float32(10000.0) ** (-np.arange(16, dtype=np.float32) / np.float32(16))).astype(np.float32)
    invC = (np.float32(10000.0) ** (-np.arange(8, dtype=np.float32) / np.float32(8))).astype(np.float32)
    for t in range(2, NT):
        tok = r * NLAT * 128 + (t - 2) * 128 + np.arange(128)
        rows = (tok // GRID_W).astype(np.float32)
        cols = (tok % GRID_W).astype(np.float32)
        for pi, pos in enumerate((rows, cols)):
            angA = (pos[:, None] * invA[None, :]).astype(np.float32)
            angC = (pos[:, None] * invC[None, :]).astype(np.float32)
            tab[:, t, pi * 16:(pi + 1) * 16] = np.cos(angA)
            tab[:, t, 32 + pi * 16:32 + (pi + 1) * 16] = np.sin(angA)
            tab[:, t, 64 + pi * 8:64 + (pi + 1) * 8] = np.cos(angC)
            tab[:, t, 80 + pi * 8:80 + (pi + 1) * 8] = np.sin(angC)
    return tab.reshape(128, NT * 96)


_WIN_PERM = np.arange(DIN)
for _g in range(2):
    for _j in range(4):
        _WIN_PERM[(_j * 2 + _g) * 64:(_j * 2 + _g + 1) * 64] = np.arange((_g * 4 + _j) * 64, (_g * 4 + _j + 1) * 64)


def make_in_maps(cfg, inputs):
    NLAT, RANKS, NB = cfg.NLAT, cfg.RANKS, cfg.NB
    f = lambda a: np.ascontiguousarray(np.asarray(a, dtype=np.float32))
    x, c, ctx, c_ctx = f(inputs["x"]), f(inputs["c"]), f(inputs["ctx"]), f(inputs["c_ctx"])
    dp = cfg.DEPTH
    shared = {
        "w_ada": f(inputs["w_ada"]),
        "b_ada2": f(np.repeat(f(inputs["b_ada"])[:, None, :], 2, axis=1)),
        "g_mix": f(inputs["g_mix"]), "w_in": f(f(inputs["w_in"])[:, :, _WIN_PERM]), "sink": f(inputs["sink"]),
        "ws_tok": f(inputs["ws_tok"]), "bs_tok": f(inputs["bs_tok"]), "g_tok": f(inputs["g_tok"]).reshape(dp, 256),
        "lam4": f(np.stack([f(inputs["lam_q1"]), f(inputs["lam_k1"]), f(inputs["lam_q2"]), f(inputs["lam_k2"])], axis=1)).reshape(dp, 128),
        "g_sub": f(inputs["g_sub"]), "w_out": f(inputs["w_out"]), "g_ffn": f(inputs["g_ffn"]),
        "w_r": f(np.concatenate([f(inputs["w_rg"]), f(inputs["w_re"])], axis=-1)),
        "b_r": f(np.concatenate([f(inputs["b_rg"]), f(inputs["b_re"])], axis=-1)),
        "w_gate": f(inputs["w_gate"]), "w_up": f(inputs["w_up"]), "w_down": f(inputs["w_down"]),
        "g_final": f(inputs["g_final"]).reshape(1, D),
        "ident": np.eye(128, dtype=np.float32),
    }
    kk = np.arange(128)[:, None]
    qq = np.arange(128)[None, :]
    mL = (qq <= kk).astype(np.float32)
    mU = (kk <= qq).astype(np.float32)
    maps = []
    for b in range(NB):
        for r in range(RANKS):
            m = dict(shared)
            m["xl"] = f(x[b, r * NLAT * 128:(r + 1) * NLAT * 128, :])
            m["ctx"] = f(ctx[b])
            m["c2"] = f(np.stack([c[b], c_ctx], axis=0))
            mk = np.stack([mL, mU, mL if r > 0 else np.zeros_like(mL), mU if r < RANKS - 1 else np.zeros_like(mU)], axis=1)
            m["masks"] = f(mk.reshape(128, 4 * 128))
            m["rope"] = _rope_table(cfg, r)
            s = np.zeros((3, RANKS), np.float32)
            s[0, r] = 1.0
            if r > 0:
                s[1, r - 1] = 1.0
            if r < RANKS - 1:
                s[2, r + 1] = 1.0
            m["sel"] = f(np.broadcast_to(s.reshape(1, 3 * RANKS), (128, 3 * RANKS)))
            maps.append(m)
    return maps


_CACHE = {}


def kernel(**inputs):
    x = np.asarray(inputs["x"])
    NB, S, _ = x.shape
    RANKS = 8 // NB
    NLAT = S // (RANKS * 128)
    cfg = Cfg(NLAT=NLAT, RANKS=RANKS, NB=NB, DEPTH=int(np.asarray(inputs["w_in"]).shape[0]))
    key = (NLAT, RANKS, NB, cfg.DEPTH)
    if key not in _CACHE:
        _CACHE[key] = build_program(cfg)[0]
    nc = _CACHE[key]
    maps = make_in_maps(cfg, inputs)
    res = run_bass_kernel_spmd(nc, maps, core_ids=list(range(cfg.NCORES)))
    out = np.zeros((NB, S, D), np.float32)
    for b in range(NB):
        for r in range(RANKS):
            out[b, r * NLAT * 128:(r + 1) * NLAT * 128, :] = np.asarray(res.results[b * RANKS + r]["out"])
    return out
```

```python
import math
from contextlib import ExitStack

import numpy as np
import concourse.bass as bass
import concourse.mybir as mybir
from concourse.bass_utils import run_bass_kernel_spmd

F32 = mybir.dt.float32
BF16 = mybir.dt.bfloat16
U8 = mybir.dt.uint8
AF = mybir.ActivationFunctionType
ALU = mybir.AluOpType
AX = mybir.AxisListType

D = 1024
DIN = 2048
EPS = 1e-6
GRID_W = 64
NEXP = 16
DEXP = 512
SCALE_A = 64 ** -0.5
SCALE_C = 32 ** -0.5


class Rec:
    __slots__ = ("q", "fn", "waits", "need", "val", "sem", "inc", "key")

    def __init__(self, q, fn):
        self.q, self.fn = q, fn
        self.waits = []
        self.need = False
        self.val = None
        self.sem = None
        self.inc = 1
        self.key = q.name


class Buf:
    __slots__ = ("name", "w", "r", "excl")

    def __init__(self, name="", excl=False):
        self.name = name
        self.w = None
        self.r = {}
        self.excl = excl


class Queue:
    def __init__(self, name, eng, sem):
        self.name, self.eng, self.sem = name, eng, sem
        self.recs = []
        self.dsems = []
        self.dlast = []
        self.dnext = 0


class FW:
    def __init__(self, nc, stack, n_dsem_sp=24, n_dsem_pool=12, n_cc=10):
        self.nc = nc
        self.Q = {}
        for name, eng in (("pe", nc.tensor), ("act", nc.scalar), ("dve", nc.vector), ("pool", nc.gpsimd), ("sp", nc.sync)):
            sem = stack.enter_context(nc.semaphore("q_" + name))
            self.Q[name] = Queue(name, eng, sem)
        for qn, n in (("sp", n_dsem_sp), ("pool", n_dsem_pool)):
            q = self.Q[qn]
            for i in range(n):
                q.dsems.append(stack.enter_context(nc.semaphore(f"d_{qn}{i}")))
                q.dlast.append(None)
        self.ccsems = [stack.enter_context(nc.semaphore(f"cc{i}")) for i in range(n_cc)]
        self.cclast = [None] * n_cc
        self.ccnext = 0
        self.all_dma = []

    def _deps(self, q, reads, writes):
        deps = []
        for b in reads:
            if b.w is not None:
                deps.append(b.w)
            if b.excl:
                deps.extend(x for x in b.r.values() if x.q is not q)
        for b in writes:
            if b.w is not None:
                deps.append(b.w)
            deps.extend(b.r.values())
        out = []
        seen = set()
        for d in deps:
            if id(d) in seen:
                continue
            seen.add(id(d))
            if d.q is q and q.name == "pe" and d.key == "pe":
                continue
            out.append(d)
        return out

    def _commit(self, rec, reads, writes):
        for d in rec.waits:
            d.need = True
        rec.q.recs.append(rec)
        for b in reads:
            b.r[rec.key] = rec
        for b in writes:
            b.w = rec
            b.r = {}

    def op(self, qn, fn, reads=(), writes=()):
        q = self.Q[qn]
        rec = Rec(q, fn)
        rec.waits = self._deps(q, reads, writes)
        self._commit(rec, reads, writes)
        return rec

    def dma(self, qn, out, in_, reads=(), writes=(), **kw):
        q = self.Q[qn]
        rec = Rec(q, lambda e: e.dma_start(out=out, in_=in_, **kw))
        i = q.dnext
        q.dnext = (q.dnext + 1) % len(q.dsems)
        rec.key = f"{qn}_d{i}"
        rec.sem = q.dsems[i]
        rec.inc = 16
        rec.need = True
        rec.waits = self._deps(q, reads, writes)
        if q.dlast[i] is not None:
            rec.waits.append(q.dlast[i])
        q.dlast[i] = rec
        self._commit(rec, reads, writes)
        self.all_dma.append(rec)
        return rec

    def cc(self, fn, reads=(), writes=()):
        q = self.Q["pool"]
        rec = Rec(q, fn)
        i = self.ccnext
        self.ccnext = (self.ccnext + 1) % len(self.ccsems)
        rec.key = f"cc{i}"
        rec.sem = self.ccsems[i]
        rec.inc = 1
        rec.need = True
        rec.waits = self._deps(q, reads, writes)
        if self.cclast[i] is not None:
            rec.waits.append(self.cclast[i])
        self.cclast[i] = rec
        self._commit(rec, reads, writes)
        return rec

    def barrier(self):
        lasts = []
        for q in self.Q.values():
            for r in reversed(q.recs):
                if r.key == q.name and r.fn is not None:
                    lasts.append(r)
                    break
            lasts.extend(x for x in q.dlast if x is not None)
        lasts.extend(x for x in self.cclast if x is not None)
        for q in self.Q.values():
            rec = Rec(q, None)
            rec.waits = [d for d in lasts if not (d.q is q and d.key == q.name)]
            for d in rec.waits:
                d.need = True
            q.recs.append(rec)

    def finish(self, block):
        for q in self.Q.values():
            cnt = 0
            dcnt = {}
            for r in q.recs:
                if r.fn is None:
                    continue
                if r.key != q.name:
                    dcnt[r.key] = dcnt.get(r.key, 0) + r.inc
                    r.val = dcnt[r.key]
                elif r.need:
                    cnt += 1
                    r.val = cnt
                    r.sem = q.sem
        stats = {}
        for q in self.Q.values():
            def run(eng, q=q):
                seen = {}
                nw = 0
                for r in q.recs:
                    for d in r.waits:
                        k = d.key
                        if seen.get(k, 0) >= d.val:
                            continue
                        eng.wait_ge(d.sem, d.val)
                        seen[k] = d.val
                        nw += 1
                    if r.fn is None:
                        continue
                    ins = r.fn(eng)
                    if r.need:
                        ins.then_inc(r.sem, r.inc)
                stats[q.name] = (len(q.recs), nw)
            getattr(block, {"pe": "tensor", "act": "scalar", "dve": "vector", "pool": "gpsimd", "sp": "sync"}[q.name])(run)
        return stats


class Arena:
    def __init__(self, handle, nbytes):
        self.h, self.n = handle, nbytes
        self.off = 0
        self.marks = []

    def alloc(self, shape, dtype, parts=128):
        esz = {F32: 4, BF16: 2, U8: 1}[dtype]
        n = int(np.prod(shape)) * esz
        n_al = (n + 63) // 64 * 64
        assert self.off + n_al <= self.n, f"SBUF arena overflow: {self.off}+{n_al} > {self.n}"
        ap = self.h[0:parts, self.off:self.off + n]
        self.off += n_al
        self.peak = max(getattr(self, 'peak', 0), self.off)
        if dtype != U8:
            ap = ap.bitcast(dtype)
        if len(shape) == 1:
            return ap
        names = [f"a{i}" for i in range(len(shape))]
        pat = "p (" + " ".join(names) + ") -> p " + " ".join(names)
        return ap.rearrange(pat, **{nm: s for nm, s in zip(names[:-1], shape[:-1])})

    def mark(self):
        self.marks.append(self.off)

    def release(self):
        print('arena scope peak', getattr(self, 'peak', 0), 'at release, off', self.off)
        self.off = self.marks.pop()


def bcast_rows(ap_row, n):
    return ap_row.partition_broadcast(n) if ap_row.shape[0] != 1 else ap_row.broadcast_to([n] + list(ap_row.shape[1:]))


def MM(out, lhsT, rhs, start=True, stop=True, **kw):
    return lambda e: e.matmul(out, lhsT=lhsT, rhs=rhs, start=start, stop=stop, **kw)


def TR(out, in_, ident):
    return lambda e: e.transpose(out, in_, ident)


def ACT(out, in_, func, **kw):
    return lambda e: e.activation(out=out, in_=in_, func=func, **kw)


def TT(out, a, b, op):
    return lambda e: e.tensor_tensor(out=out, in0=a, in1=b, op=op)


def TS(out, a, s1, op0, s2=None, op1=None, **kw):
    if op1 is None:
        return lambda e: e.tensor_scalar(out=out, in0=a, scalar1=s1, scalar2=None, op0=op0, **kw)
    return lambda e: e.tensor_scalar(out=out, in0=a, scalar1=s1, scalar2=s2, op0=op0, op1=op1, **kw)


def STT(out, a, s, b, op0, op1):
    return lambda e: e.scalar_tensor_tensor(out=out, in0=a, scalar=s, in1=b, op0=op0, op1=op1)


def CP(out, in_):
    return lambda e: e.tensor_copy(out=out, in_=in_)


def MEMSET(ap, v):
    return lambda e: e.memset(ap, v)


def RSUM(out, in_, axis=None):
    return lambda e: e.reduce_sum(out=out, in_=in_, axis=axis or AX.X)


def RMAX(out, in_, axis=None):
    return lambda e: e.reduce_max(out=out, in_=in_, axis=axis or AX.X)


class Cfg:
    def __init__(self, NLAT=32, RANKS=4, NB=2, DEPTH=2, stop_after=None):
        self.NLAT, self.RANKS, self.NB, self.DEPTH = NLAT, RANKS, NB, DEPTH
        self.NT = NLAT + 2
        self.NKC = 2 + RANKS * NLAT
        self.NCORES = NB * RANKS
        self.stop_after = stop_after


ARENA_BYTES = 207 * 1024


class StopBuild(Exception):
    pass


def build_program(cfg):
    NLAT, RANKS, NT, NKC, DEPTH = cfg.NLAT, cfg.RANKS, cfg.NT, cfg.NKC, cfg.DEPTH
    KCH = 8 if NLAT % 8 == 0 else (2 if NLAT % 2 == 0 else 1)
    nc = bass.Bass("TRN2", target_bir_lowering=False)
    dbg = set(getattr(cfg, "debug", ()) or ())
    I = {}

    def inp(name, shape):
        I[name] = nc.dram_tensor(name, shape, F32, kind="ExternalInput").ap()

    inp("xl", [NLAT * 128, D]); inp("ctx", [256, D]); inp("c2", [2, D])
    inp("w_ada", [DEPTH, D, 6 * D]); inp("b_ada2", [DEPTH, 2, 6 * D]); inp("g_mix", [DEPTH, D])
    inp("w_in", [DEPTH, D, DIN]); inp("sink", [DEPTH, 8]); inp("ws_tok", [DEPTH, 4, 128, 128])
    inp("bs_tok", [DEPTH, 4, 128]); inp("g_tok", [DEPTH, 256]); inp("lam4", [DEPTH, 128]); inp("g_sub", [DEPTH, 64])
    inp("w_out", [DEPTH, D, D]); inp("g_ffn", [DEPTH, D]); inp("w_r", [DEPTH, D, 20]); inp("b_r", [DEPTH, 20])
    inp("w_gate", [DEPTH, NEXP, D, DEXP]); inp("w_up", [DEPTH, NEXP, D, DEXP]); inp("w_down", [DEPTH, NEXP, DEXP, D])
    inp("g_final", [1, D]); inp("ident", [128, 128]); inp("masks", [128, 4 * 128]); inp("rope", [128, NT * 96])
    inp("sel", [128, 3 * RANKS])
    out_d = nc.dram_tensor("out", [NLAT * 128, D], F32, kind="ExternalOutput").ap()

    def scr(name, shape, dt):
        kind = "ExternalOutput" if name in dbg else "Internal"
        return nc.dram_tensor(name, shape, dt, kind=kind).ap()

    modd = scr("modd", [DEPTH, 2, 6 * D], F32)
    xbuf = scr("xbuf", [NT, 128, D], F32)
    xmid = scr("xmid", [NT, 128, D], F32)
    qta_d = scr("qta_d", [NT, 128, 512], BF16)
    qtc_d = scr("qtc_d", [NT, 128, 256], BF16)
    yb_d = scr("yb_d", [NT, 128, 256], BF16)
    h2t_d = scr("h2t_d", [128, 8, NT * 128], BF16)
    kctx_d = scr("kctx_d", [128, 2, 256], BF16)
    vctx_d = scr("vctx_d", [2, 128, 260], BF16)
    xk_s = scr("xk_s", [RANKS, 128, 2, NLAT * 128], BF16)
    xv_s = scr("xv_s", [RANKS, NLAT, 128, 260], BF16)
    xh_s = scr("xh_s", [RANKS, 128, 2 * 258], BF16)
    if RANKS > 1:
        xk_r = scr("xk_r", [RANKS, 128, 2, NLAT * 128], BF16)
        xv_r = scr("xv_r", [RANKS, NLAT, 128, 260], BF16)
        xh_r = scr("xh_r", [RANKS, 128, 2 * 258], BF16)
    else:
        xk_r, xv_r, xh_r = xk_s, xv_s, xh_s

    DB = {}

    def db(name, idx=0):
        k = (name, idx)
        if k not in DB:
            DB[k] = Buf(f"{name}{idx}")
        return DB[k]

    with ExitStack() as st:
        arena_h = st.enter_context(nc.sbuf_tensor("arena", [128, ARENA_BYTES], U8))
        ps = st.enter_context(nc.psum_tensor("ps", [128, 8, 512], F32))
        fw = FW(nc, st)
        A = Arena(arena_h, ARENA_BYTES)
        PS = [Buf(f"ps{i}", excl=True) for i in range(8)]
        psb = [ps[:, i, :].bitcast(BF16) for i in range(8)]

        class Ring:
            def __init__(self, n, shape, dt, parts=128):
                self.aps = [A.alloc(shape, dt, parts) for _ in range(n)]
                self.bufs = [Buf() for _ in range(n)]
                self.i = 0

            def next(self):
                k = self.i
                self.i = (self.i + 1) % len(self.aps)
                return self.aps[k], self.bufs[k]

        op, dma = fw.op, fw.dma

        ident_f = A.alloc([128], F32); Bidf = Buf()
        ident_b = A.alloc([128], BF16); Bidb = Buf()
        masks = A.alloc([4, 128], BF16); Bmask = Buf()
        sel = A.alloc([3 * RANKS], F32); Bsel = Buf()
        dma("sp", ident_f, I["ident"], writes=[Bidf])
        dma("pool", ident_b, I["ident"], writes=[Bidb])
        dma("pool", masks, I["masks"].rearrange("p (m q) -> p m q", m=4), writes=[Bmask])
        dma("sp", sel, I["sel"], writes=[Bsel])
        mneg = A.alloc([4, 512], BF16); Bmneg = Buf()
        for m_ in range(4):
            op("dve", TS(mneg[:, m_, :].rearrange("p (j q) -> p j q", j=4), masks[:, m_, :].unsqueeze(1).broadcast_to([128, 4, 128]),
                         -1.0, ALU.add, 30000.0, ALU.mult), reads=[Bmask], writes=[Bmneg])
        ones1 = A.alloc([1], F32); Bones = Buf()
        op("pool", MEMSET(ones1, 1.0), writes=[Bones])

        gates = A.alloc([NT, 16], F32)
        Bgate = [Buf() for _ in range(NT)]

        eps_t = A.alloc([1], F32); Beps = Buf()
        op("pool", MEMSET(eps_t, EPS), writes=[Beps])

        A.mark()
        c_raw = A.alloc([2, 8], F32); Bcraw = Buf()
        cS = A.alloc([8, 2], F32); BcS = Buf()
        dma("sp", c_raw, I["c2"].rearrange("r (p k) -> p r k", p=128), writes=[Bcraw])
        op("act", ACT(cS, c_raw.rearrange("p r k -> p k r"), AF.Silu), reads=[Bcraw], writes=[BcS])
        wa_ring = Ring(2, [8, 512], F32)
        bada = A.alloc([6 * D], F32, parts=2); Bbada = Buf()
        msb_ring = Ring(2, [512], F32, parts=2)
        import os as _os
        for l in range(DEPTH if not _os.environ.get("DBG_NOADA") else 0):
            dma("sp", bada, I["b_ada2"][l], writes=[Bbada])
            for cb in range(12):
                wa, Bwa = wa_ring.next()
                dma("sp", wa, I["w_ada"][l, :, cb * 512:(cb + 1) * 512].rearrange("(p k) n -> p k n", p=128), writes=[Bwa])
                for kc in range(8):
                    op("pe", MM(ps[0:2, 0, :], cS[:, kc, :], wa[:, kc, :], start=(kc == 0), stop=(kc == 7)),
                       reads=[BcS, Bwa], writes=[PS[0]])
                msb, Bmsb = msb_ring.next()
                op("dve", TT(msb, ps[0:2, 0, :], bada[:, cb * 512:(cb + 1) * 512], ALU.add), reads=[PS[0], Bbada], writes=[Bmsb])
                dma("sp", modd[l, :, cb * 512:(cb + 1) * 512], msb, reads=[Bmsb], writes=[db("modd", l)])
        A.release()
        fw.barrier()

        def load_mod(l, r, chunk, dst, buf, q="sp"):
            return dma(q, dst, modd[l, r:r + 1, chunk * D:(chunk + 1) * D].broadcast_to([128, D]),
                       reads=[db("modd", l)], writes=[buf])

        def load_gain(src_row, dst, buf, q="sp"):
            return dma(q, dst, src_row.broadcast_to([128] + [src_row.shape[1]]), writes=[buf])

        def xsrc(l, t):
            if l == 0:
                return (I["ctx"][t * 128:(t + 1) * 128, :], []) if t < 2 else (I["xl"][(t - 2) * 128:(t - 1) * 128, :], [])
            return xbuf[t], [db("xbuf", t)]

        def rstd_from_ss(ss, Bss, tmp, Btmp, rstd, Brstd, n):
            op("act", ACT(tmp, ss, AF.Ln, scale=1.0 / n, bias=eps_t), reads=[Bss, Beps], writes=[Btmp])
            op("act", ACT(rstd, tmp, AF.Exp, scale=-0.5), reads=[Btmp], writes=[Brstd])

        def ck(name):
            if cfg.stop_after == name:
                raise StopBuild()

        try:
          for l in range(DEPTH):
              last = (l == DEPTH - 1)
              if cfg.stop_after == "pre":
                  break
              lam_init = 0.8 - 0.6 * math.exp(-0.3 * l)
              A.mark()
              lam4b = A.alloc([4, 32], F32); Bl4 = Buf()
              dma("sp", lam4b, I["lam4"][l:l + 1, :].broadcast_to([128, 128]).rearrange("p (a b) -> p a b", a=4), writes=[Bl4])
              lsc = A.alloc([8], F32); Blsc = Buf()
              ljunk = A.alloc([32], F32); Blj = Buf()
              op("dve", TT(ljunk, lam4b[:, 0, :], lam4b[:, 1, :], ALU.mult), reads=[Bl4], writes=[Blj])
              op("dve", RSUM(lsc[:, 0:1], ljunk), reads=[Blj], writes=[Blsc])
              op("dve", TT(ljunk, lam4b[:, 2, :], lam4b[:, 3, :], ALU.mult), reads=[Bl4], writes=[Blj])
              op("dve", RSUM(lsc[:, 1:2], ljunk), reads=[Blj, Blsc], writes=[Blsc])
              op("act", ACT(lsc[:, 2:4], lsc[:, 0:2], AF.Exp), reads=[Blsc], writes=[Blsc])
              op("dve", TT(lsc[:, 4:5], lsc[:, 2:3], lsc[:, 3:4], ALU.subtract), reads=[Blsc], writes=[Blsc])
              neglam = lsc[:, 5:6]
              op("dve", TS(neglam, lsc[:, 4:5], -1.0, ALU.mult, -lam_init, ALU.add), reads=[Blsc], writes=[Blsc])
              esink = A.alloc([8], F32); Besink = Buf()
              dma("sp", esink, I["sink"][l:l + 1, :].broadcast_to([128, 8]), writes=[Besink])
              op("act", ACT(esink, esink, AF.Exp), reads=[Besink], writes=[Besink])
              gsub_s = A.alloc([64], F32); Bgsub = Buf()
              dma("sp", gsub_s, I["g_sub"][l:l + 1, :].broadcast_to([128, 64]), writes=[Bgsub])
              op("dve", TS(gsub_s, gsub_s, 1.0 - lam_init, ALU.mult), reads=[Bgsub], writes=[Bgsub])
              A.mark()
              KTA = A.alloc([(NT + 2) * 128], BF16)
              VA = A.alloc([NT + 2, 130], BF16)
              BKTA = [Buf() for _ in range(NT + 2)]
              BVA = [Buf() for _ in range(NT + 2)]
              op("pool", MEMSET(VA, 1.0), writes=BVA)
              op("pool", MEMSET(KTA[:, NT * 128:(NT + 2) * 128], 0.0), writes=BKTA[NT:NT + 2])

              A.mark()
              win = A.alloc([8, DIN], BF16); Bwin = Buf()
              for kc in range(8):
                  dma("pool", win[:, kc, :], I["w_in"][l, kc * 128:(kc + 1) * 128, :], writes=[Bwin])
              rope_ring = Ring(2, [96], F32)
              G1 = [A.alloc([D], F32) for _ in range(2)]; BG1 = [Buf(), Buf()]
              SH1 = [A.alloc([D], F32) for _ in range(2)]; BSH1 = [Buf(), Buf()]
              gtmp = A.alloc([D], F32); Bgtmp = Buf()
              for r in range(2):
                  load_mod(l, r, 1, G1[r], BG1[r])
                  load_gain(I["g_mix"][l:l + 1, :], gtmp, Bgtmp)
                  op("dve", STT(G1[r], G1[r], 1.0, gtmp, ALU.add, ALU.mult), reads=[BG1[r], Bgtmp], writes=[BG1[r]])
                  load_mod(l, r, 0, SH1[r], BSH1[r])
              ws_f = A.alloc([4, 128], F32); Bwsf = Buf()
              dma("sp", ws_f, I["ws_tok"][l].rearrange("g p q -> p g q"), writes=[Bwsf])
              wsT = A.alloc([4, 128], BF16); BwsT = Buf()
              for g in range(4):
                  op("pe", TR(ps[:, 7, g * 128:(g + 1) * 128], ws_f[:, g, :], ident_f), reads=[Bwsf, Bidf], writes=[PS[7]])
              op("dve", CP(wsT, ps[:, 7, :].rearrange("p (g q) -> p g q", g=4)), reads=[PS[7]], writes=[BwsT])
              bsT = A.alloc([4], F32); BbsT = Buf()
              dma("sp", bsT, I["bs_tok"][l].rearrange("g p -> p g"), writes=[BbsT], allow_slow_non_contiguous=True)
              gtokb = A.alloc([256], F32); Bgtok = Buf()
              dma("sp", gtokb, I["g_tok"][l:l + 1, :].broadcast_to([128, 256]), writes=[Bgtok])

              ck(('p1a', l))
              x_ring = Ring(2, [D], F32)
              junk_b = A.alloc([D], BF16); Bjunk = Buf()
              sc_ring = Ring(2, [8], F32)
              t1_ring = Ring(2, [D], F32)
              h_ring = Ring(2, [D], BF16)
              hT_ring = Ring(2, [8, 128], BF16)
              psb_ring = Ring(2, [DIN], F32)
              rt_ring = Ring(2, [4, 320], F32)
              qk_ring = Ring(2, [1152], BF16)
              qta_ring = Ring(2, [512], BF16)
              qtc_ring = Ring(2, [256], BF16)
              vt = A.alloc([4, 65], BF16); Bvt = Buf()
              op("pool", MEMSET(vt, 1.0), writes=[Bvt])
              NSTG = min(2, NLAT)
              stgK_ring = Ring(2, [RANKS, 2, NSTG * 128], BF16)
              stgV_ring = Ring(2, [RANKS, NSTG, 260], BF16)
              hal_st = A.alloc([RANKS, 2 * 258], BF16); Bhal = Buf()
              kct_ring = Ring(2, [256], BF16)
              gl_ring = Ring(2, [4, 512], F32)
              ln_ring = Ring(2, [16], F32)
              vn_ring = Ring(2, [256], BF16)
              yb_ring = Ring(2, [256], BF16)
              sq_ring = Ring(2, [256], F32)

              stg = {}

              def p1_tile(t):
                  r = 1 if t < 2 else 0
                  xt, Bx = x_ring.next()
                  src, sdeps = xsrc(l, t)
                  dma("sp", xt, src, reads=sdeps, writes=[Bx])
                  sc, Bsc = sc_ring.next()
                  t1, Bt1 = t1_ring.next()
                  op("dve", TT(t1, xt, xt, ALU.mult), reads=[Bx], writes=[Bt1])
                  op("dve", RSUM(sc[:, 0:1], t1), reads=[Bt1], writes=[Bsc])
                  rstd_from_ss(sc[:, 0:1], Bsc, sc[:, 1:2], Bsc, sc[:, 2:3], Bsc, D)
                  op("dve", STT(t1, xt, sc[:, 2:3], G1[r], ALU.mult, ALU.mult), reads=[Bx, Bsc, BG1[r]], writes=[Bt1])
                  h, Bh = h_ring.next()
                  op("dve", TT(h, t1, SH1[r], ALU.add), reads=[Bt1, BSH1[r]], writes=[Bh])
                  yield
                  for kc in range(8):
                      op("pe", TR(psb[0][:, kc * 128:(kc + 1) * 128], h[:, kc * 128:(kc + 1) * 128], ident_b), reads=[Bh, Bidb], writes=[PS[0]])
                  hT, BhT = hT_ring.next()
                  op("act", ACT(hT, psb[0].rearrange("p (k t) -> p k t", k=8), AF.Copy), reads=[PS[0]], writes=[BhT])
                  yield
                  for j in range(4):
                      for kc in range(8):
                          op("pe", MM(ps[:, 1 + j, :], hT[:, kc, :], win[:, kc, j * 512:(j + 1) * 512], start=(kc == 0), stop=(kc == 7)),
                             reads=[BhT, Bwin], writes=[PS[1 + j]])
                  ck(('p1b', l))
                  p_sb, Bp = psb_ring.next()
                  op("act", ACT(p_sb[:, 0:512], ps[:, 1, :], AF.Copy), reads=[PS[1]], writes=[Bp])
                  op("act", ACT(p_sb[:, 512:1024], ps[:, 2, :], AF.Copy), reads=[PS[2]], writes=[Bp])
                  op("dve", CP(p_sb[:, 1024:1536], ps[:, 3, :]), reads=[PS[3]], writes=[Bp])
                  op("dve", CP(p_sb[:, 1536:2048], ps[:, 4, :]), reads=[PS[4]], writes=[Bp])
                  yield
                  ropet, Brope = rope_ring.next()
                  dma("sp", ropet, I["rope"][:, t * 96:(t + 1) * 96], writes=[Brope])
                  qk, Bqk = qk_ring.next()
                  rt, Brt = rt_ring.next()
                  for (c0, nh, dd, tb0, o0) in ((0, 10, 16, 0, 0), (1280, 16, 8, 64, 640)):
                      w = nh * 4 * dd
                      xv = p_sb[:, c0:c0 + w].rearrange("p (h r t d) -> p h r t d", h=nh, r=2, t=2, d=dd)
                      ov = qk[:, o0:o0 + w].rearrange("p (h r t d) -> p h r t d", h=nh, r=2, t=2, d=dd)
                      cosv = ropet[:, tb0:tb0 + 2 * dd].rearrange("p (r d) -> p r d", r=2).unsqueeze(1).broadcast_to([128, nh, 2, dd])
                      sinv = ropet[:, tb0 + 2 * dd:tb0 + 4 * dd].rearrange("p (r d) -> p r d", r=2).unsqueeze(1).broadcast_to([128, nh, 2, dd])
                      tv = [rt[:, i, 0:nh * 2 * dd].rearrange("p (h r d) -> p h r d", h=nh, r=2, d=dd) for i in range(4)]
                      x1, x2 = xv[:, :, :, 0, :], xv[:, :, :, 1, :]
                      op("dve", TT(tv[0], x1, cosv, ALU.mult), reads=[Bp, Brope], writes=[Brt])
                      op("dve", TT(tv[1], x2, sinv, ALU.mult), reads=[Bp, Brope], writes=[Brt])
                      op("dve", TT(tv[2], x1, sinv, ALU.mult), reads=[Bp, Brope], writes=[Brt])
                      op("dve", TT(tv[3], x2, cosv, ALU.mult), reads=[Bp, Brope], writes=[Brt])
                      op("dve", TT(ov[:, :, :, 0, :], tv[0], tv[1], ALU.subtract), reads=[Brt], writes=[Bqk])
                      op("dve", TT(ov[:, :, :, 1, :], tv[2], tv[3], ALU.add), reads=[Brt], writes=[Bqk])
                  ck(('p1c', l))
                  op("pool", CP(VA[:, t, :].rearrange("p (g d) -> p g d", g=2)[:, :, 0:64],
                                p_sb[:, 640:768].rearrange("p (g d) -> p g d", g=2)), reads=[Bp], writes=[BVA[t]])
                  op("pool", CP(vt[:, :, 0:64], p_sb[:, 1792:2048].rearrange("p (h d) -> p h d", h=4)), reads=[Bp], writes=[Bvt])
                  if t < 2:
                      dma("sp", vctx_d[t], vt.rearrange("p h d -> p (h d)"), reads=[Bvt], writes=[db("vctx")])
                  else:
                      j = (t - 2) % NSTG
                      if j == 0:
                          stg["K"], stg["BK"] = stgK_ring.next()
                          stg["V"], stg["BV"] = stgV_ring.next()
                      stgK, BstK, stgV, BstV = stg["K"], stg["BK"], stg["V"], stg["BV"]
                      for rr in range(RANKS):
                          op("pool", TS(stgV[:, rr, j, :], vt.rearrange("p h d -> p (h d)"), sel[:, rr:rr + 1], ALU.mult, 1.0, ALU.mult),
                             reads=[Bvt, Bsel], writes=[BstV])
                  yield
                  ck(('p1d', l))
                  for jj in range(4):
                      op("pe", TR(psb[5][:, jj * 128:(jj + 1) * 128], qk[:, jj * 128:(jj + 1) * 128], ident_b),
                         reads=[Bqk, Bidb], writes=[PS[5]])
                  op("pe", TR(psb[5][:, 512:640], qk[:, 512:640], ident_b), reads=[Bqk, Bidb], writes=[PS[5]])
                  for c in range(4):
                      op("pe", TR(psb[6][:, c * 128:(c + 1) * 128], qk[:, 640 + c * 128:640 + (c + 1) * 128], ident_b),
                         reads=[Bqk, Bidb], writes=[PS[6]])
                  ck(('p1d1', l))
                  qta_t, Bqta = qta_ring.next()
                  op("dve", CP(qta_t, psb[5][:, 0:512]), reads=[PS[5]], writes=[Bqta])
                  dma("sp", qta_d[t], qta_t, reads=[Bqta], writes=[db("qta", t)])
                  ck(('p1d2', l))
                  op("act", ACT(KTA[:, t * 128:(t + 1) * 128], psb[5][:, 512:640], AF.Copy), reads=[PS[5]], writes=[BKTA[t]])
                  ck(('p1d3', l))
                  qtc_t, Bqtc = qtc_ring.next()
                  op("dve", CP(qtc_t, psb[6][:, 0:256]), reads=[PS[6]], writes=[Bqtc])
                  dma("sp", qtc_d[t], qtc_t, reads=[Bqtc], writes=[db("qtc", t)])
                  ck(('p1d4', l))
                  if t < 2:
                      kct, Bkct = kct_ring.next()
                      op("act", ACT(kct, psb[6][:, 256:512], AF.Copy), reads=[PS[6]], writes=[Bkct])
                      dma("sp", kctx_d[:, :, t * 128:(t + 1) * 128], kct.rearrange("p (c k) -> p c k", c=2), reads=[Bkct], writes=[db("kctx")])
                  else:
                      j = (t - 2) % NSTG
                      for rr in range(RANKS):
                          op("act", ACT(stgK[:, rr, :, j * 128:(j + 1) * 128], psb[6][:, 256:512].rearrange("p (c k) -> p c k", c=2), AF.Copy,
                                        scale=sel[:, rr:rr + 1]), reads=[PS[6], Bsel], writes=[BstK])
                      if j == NSTG - 1 or t == NT - 1:
                          g0 = (t - 2) - j
                          ng = j + 1
                          for rr in range(RANKS):
                              dma("sp", xk_s[rr, :, :, g0 * 128:(g0 + ng) * 128], stgK[:, rr, :, 0:ng * 128], reads=[BstK], writes=[db("xk_s", rr)])
                              dma("sp", xv_s[rr, g0:g0 + ng].rearrange("j p f -> p j f"), stgV[:, rr, 0:ng, :], reads=[BstV], writes=[db("xv_s", rr)])
                      if t == 2 or t == NT - 1:
                          w = 0 if t == 2 else 1
                          for rr in range(RANKS):
                              op("pool", TS(hal_st[:, rr, w * 258:w * 258 + 128], KTA[:, t * 128:(t + 1) * 128], sel[:, rr:rr + 1], ALU.mult, 1.0, ALU.mult),
                                 reads=[BKTA[t], Bsel], writes=[Bhal])
                              op("pool", TS(hal_st[:, rr, w * 258 + 128:(w + 1) * 258], VA[:, t, :], sel[:, rr:rr + 1], ALU.mult, 1.0, ALU.mult),
                                 reads=[BVA[t], Bsel], writes=[Bhal])
                  yield
                  ck(('p1e', l))
                  gl, Bgl = gl_ring.next()
                  xg, x2, th, gg = gl[:, 0, :], gl[:, 1, :], gl[:, 2, :], gl[:, 3, :]
                  op("dve", CP(xg, p_sb[:, 768:1280]), reads=[Bp], writes=[Bgl])
                  op("dve", TT(x2, xg, xg, ALU.mult), reads=[Bgl], writes=[Bgl])
                  op("dve", TS(x2, x2, 0.044715, ALU.mult, 1.0, ALU.add), reads=[Bgl], writes=[Bgl])
                  op("dve", TT(x2, x2, xg, ALU.mult), reads=[Bgl], writes=[Bgl])
                  op("act", ACT(th, x2, AF.Tanh, scale=math.sqrt(2.0 / math.pi)), reads=[Bgl], writes=[Bgl])
                  op("dve", TS(th, th, 0.5, ALU.mult, 0.5, ALU.add), reads=[Bgl], writes=[Bgl])
                  op("dve", TT(gg, th, xg, ALU.mult), reads=[Bgl], writes=[Bgl])
                  ug = gg[:, 0:256]
                  vg = gg[:, 256:512].rearrange("p (g d) -> p g d", g=4)
                  ln, Bln = ln_ring.next()
                  sq, Bsq = sq_ring.next()
                  op("dve", RSUM(ln[:, 0:4], vg), reads=[Bgl], writes=[Bln])
                  op("dve", TT(sq, gg[:, 256:512], gg[:, 256:512], ALU.mult), reads=[Bgl], writes=[Bsq])
                  op("dve", RSUM(ln[:, 4:8], sq.rearrange("p (g d) -> p g d", g=4)), reads=[Bsq, Bln], writes=[Bln])
                  op("dve", TS(ln[:, 0:4], ln[:, 0:4], 1.0 / 64, ALU.mult), reads=[Bln], writes=[Bln])
                  op("dve", TT(ln[:, 8:12], ln[:, 0:4], ln[:, 0:4], ALU.mult), reads=[Bln], writes=[Bln])
                  op("dve", STT(ln[:, 4:8], ln[:, 4:8], 1.0 / 64, ln[:, 8:12], ALU.mult, ALU.subtract), reads=[Bln], writes=[Bln])
                  op("act", ACT(ln[:, 8:12], ln[:, 4:8], AF.Ln, bias=eps_t), reads=[Bln, Beps], writes=[Bln])
                  op("act", ACT(ln[:, 12:16], ln[:, 8:12], AF.Exp, scale=-0.5), reads=[Bln], writes=[Bln])
                  for g in range(4):
                      op("dve", TS(sq[:, g * 64:(g + 1) * 64], vg[:, g, :], ln[:, g:g + 1], ALU.subtract, ln[:, 12 + g:13 + g], ALU.mult),
                         reads=[Bgl, Bln, Bsq], writes=[Bsq])
                  yield
                  vn, Bvn = vn_ring.next()
                  op("dve", TT(vn, sq, gtokb, ALU.mult), reads=[Bsq, Bgtok], writes=[Bvn])
                  for g in range(4):
                      op("pe", MM(ps[:, 7, g * 64:(g + 1) * 64], wsT[:, g, :], vn[:, g * 64:(g + 1) * 64], start=True, stop=True, skip_group_check=True),
                         reads=[BwsT, Bvn], writes=[PS[7]])
                  ybt, Byb = yb_ring.next()
                  for g in range(4):
                      op("dve", STT(ybt[:, g * 64:(g + 1) * 64], ps[:, 7, g * 64:(g + 1) * 64], bsT[:, g:g + 1], ug[:, g * 64:(g + 1) * 64], ALU.add, ALU.mult),
                         reads=[PS[7], BbsT, Bgl], writes=[Byb])
                  dma("sp", yb_d[t], ybt, reads=[Byb], writes=[db("yb", t)])

              def run_interleaved(gens, depth):
                  active = []
                  it = iter(gens)
                  done = False
                  while True:
                      while len(active) < depth and not done:
                          try:
                              active.append(next(it))
                          except StopIteration:
                              done = True
                      if not active:
                          break
                      for g_ in list(active):
                          try:
                              next(g_)
                          except StopIteration:
                              active.remove(g_)

              run_interleaved((p1_tile(t) for t in range(NT)), 2)
              ck(('p1f', l))
              for rr in range(RANKS):
                  dma("sp", xh_s[rr], hal_st[:, rr, :], reads=[Bhal], writes=[db("xh_s", rr)])
              A.release()
              fw.barrier()
              if cfg.stop_after == ("p1", l):
                  break

              if RANKS > 1:
                  groups = [[b * RANKS + r for r in range(RANKS)] for b in range(cfg.NB)]
                  fw.cc(lambda e: e.collective_compute("AllReduce", ALU.add, replica_groups=groups,
                                                       ins=[xh_s.rearrange("r p f -> (r p) f")], outs=[xh_r.rearrange("r p f -> (r p) f")]),
                        reads=[db("xh_s", rr) for rr in range(RANKS)], writes=[db("xh_r")])
                  for rr in range(RANKS):
                      fw.cc(lambda e, rr=rr: e.collective_compute("AllReduce", ALU.add, replica_groups=groups,
                                                                  ins=[xk_s[rr].rearrange("p c k -> p (c k)")],
                                                                  outs=[xk_r[rr].rearrange("p c k -> p (c k)")]),
                            reads=[db("xk_s", rr)], writes=[db("xk_r", rr)])
                      fw.cc(lambda e, rr=rr: e.collective_compute("AllReduce", ALU.add, replica_groups=groups,
                                                                  ins=[xv_s[rr].rearrange("j p f -> (j p) f")],
                                                                  outs=[xv_r[rr].rearrange("j p f -> (j p) f")]),
                            reads=[db("xv_s", rr)], writes=[db("xv_r", rr)])
                  kvr = lambda name, rr: db(name + "_r", rr)
              else:
                  kvr = lambda name, rr: db(name + "_s", rr)
              Bxh = db("xh_r") if RANKS > 1 else db("xh_s", 0)

              A.mark()
              hal = A.alloc([RANKS, 2 * 258], BF16); Bhl = Buf()
              dma("sp", hal, xh_r.rearrange("r p f -> p r f"), reads=[Bxh], writes=[Bhl])
              hacc = A.alloc([2, 258], F32); Bhacc = Buf()
              for w, so in ((0, 2 * RANKS), (1, RANKS)):
                  for rr in range(RANKS):
                      src = hal[:, rr, w * 258:(w + 1) * 258]
                      if rr == 0:
                          op("dve", TS(hacc[:, w, :], src, sel[:, so + rr:so + rr + 1], ALU.mult), reads=[Bhl, Bsel], writes=[Bhacc])
                      else:
                          op("dve", STT(hacc[:, w, :], src, sel[:, so + rr:so + rr + 1], hacc[:, w, :], ALU.mult, ALU.add),
                             reads=[Bhl, Bsel, Bhacc], writes=[Bhacc])
              op("dve", CP(KTA[:, NT * 128:(NT + 1) * 128], hacc[:, 1, 0:128]), reads=[Bhacc], writes=[BKTA[NT]])
              op("dve", CP(VA[:, NT, :], hacc[:, 1, 128:258]), reads=[Bhacc], writes=[BVA[NT]])
              op("dve", CP(KTA[:, (NT + 1) * 128:(NT + 2) * 128], hacc[:, 0, 0:128]), reads=[Bhacc], writes=[BKTA[NT + 1]])
              op("dve", CP(VA[:, NT + 1, :], hacc[:, 0, 128:258]), reads=[Bhacc], writes=[BVA[NT + 1]])

              wout = A.alloc([8, D], BF16); Bwout = Buf()
              for kc in range(8):
                  dma("pool", wout[:, kc, :], I["w_out"][l, kc * 128:(kc + 1) * 128, :], writes=[Bwout])
              wr = A.alloc([8, 20], F32); Bwr = Buf()
              dma("sp", wr, I["w_r"][l].rearrange("(k p) n -> p k n", p=128), writes=[Bwr])
              brb = A.alloc([20], F32); Bbr = Buf()
              dma("sp", brb, I["b_r"][l:l + 1, :].broadcast_to([128, 20]), writes=[Bbr])
              GT1 = A.alloc([D], F32); BGT1 = Buf()
              G2 = A.alloc([D], F32); BG2 = Buf()
              SH2 = A.alloc([D], F32); BSH2 = Buf()
              gtmp2 = A.alloc([D], F32); Bgtmp2 = Buf()

              def load_p2_mods(r):
                  load_mod(l, r, 2, GT1, BGT1)
                  load_mod(l, r, 4, G2, BG2)
                  load_gain(I["g_ffn"][l:l + 1, :], gtmp2, Bgtmp2)
                  op("dve", STT(G2, G2, 1.0, gtmp2, ALU.add, ALU.mult), reads=[BG2, Bgtmp2], writes=[BG2])
                  load_mod(l, r, 3, SH2, BSH2)

              kc_ring = Ring(3, [2, KCH * 128], BF16)
              vc_ring = Ring(3, [KCH, 260], BF16)
              qb_ring = Ring(2, [2, 256], BF16)
              E_aps = [[A.alloc([1024], BF16) for _ in range(2)] for _ in range(2)]
              E_buf = [[Buf() for _ in range(2)] for _ in range(2)]
              usb = A.alloc([2, 512], F32); Busb = Buf()
              accS = [A.alloc([1024], F32) for _ in range(2)]; BaccS = [Buf(), Buf()]
              epair = [A.alloc([1024], BF16) for _ in range(2)]; Bepair = [Buf(), Buf()]
              uasb = A.alloc([2, 512], F32); Buasb = Buf()
              qa_ring = Ring(2, [512], BF16)
              eab_ring = Ring(2, [5, 512], BF16)
              fsc_ring = Ring(4, [48], F32)
              osb_ring = Ring(2, [256], F32)
              ojk = A.alloc([256], F32); Bojk = Buf()
              ymix_ring = Ring(2, [D], BF16)
              ymT_ring = Ring(2, [8, 128], BF16)
              x2_ring = Ring(2, [D], F32)
              yt_ring = Ring(2, [D], F32)
              xm_ring = Ring(2, [D], F32)
              h2_ring = Ring(2, [D], F32)
              h2Tf_ring = Ring(2, [8, 128], F32)
              h2Tb_ring = Ring(2, [8, 128], BF16)
              rsc_ring = Ring(2, [96], F32)
              junk2 = A.alloc([D], BF16); Bjunk2 = Buf()

              qblocks = []
              if not last:
                  qblocks.append(([0, 1], 1, "ctx"))
              for i in range(0, NLAT, 2):
                  qblocks.append(([2 + i, 3 + i] if i + 1 < NLAT else [2 + i], 0, "lat"))
              cur_r = None
              pending = []
              pend_ptr = [0]

              def step_pending():
                  if not pending:
                      return
                  k = pend_ptr[0] % len(pending)
                  try:
                      next(pending[k])
                      pend_ptr[0] += 1
                  except StopIteration:
                      pending.pop(k)

              def flush_pending():
                  while pending:
                      step_pending()

              for (tiles, r, kind) in qblocks:
                  if r != cur_r:
                      load_p2_mods(r)
                      cur_r = r
                  nq = len(tiles) * 128
                  chunks = [(kctx_d, vctx_d.rearrange("j p f -> p j f"), 2, [db("kctx"), db("vctx")])]
                  if kind == "lat":
                      for rr in range(RANKS):
                          for c0 in range(0, NLAT, KCH):
                              chunks.append((xk_r[rr, :, :, c0 * 128:(c0 + KCH) * 128], xv_r[rr, c0:c0 + KCH].rearrange("j p f -> p j f"), KCH,
                                             [kvr("xk", rr), kvr("xv", rr)]))
                  Qb, BQb = qb_ring.next()
                  for j, t in enumerate(tiles):
                      dma("sp", Qb[:, :, j * 128:(j + 1) * 128], qtc_d[t].rearrange("p (c k) -> p c k", c=2), reads=[db("qtc", t)], writes=[BQb])
                  ktl = []
                  loaded = {}

                  def load_chunk(ci):
                      ksrc, vsrc, n, deps = chunks[ci]
                      kc_t, Bkc = kc_ring.next()
                      vc_t, Bvc = vc_ring.next()
                      dma("sp", kc_t[:, :, 0:n * 128], ksrc, reads=deps, writes=[Bkc])
                      dma("sp", vc_t[:, 0:n, :], vsrc, reads=deps, writes=[Bvc])
                      loaded[ci] = (kc_t, Bkc, vc_t, Bvc)

                  for ci, ch in enumerate(chunks):
                      for kk in range(ch[2]):
                          ktl.append((ci, kk))
                  nk = len(ktl)
                  load_chunk(0)
                  if len(chunks) > 1:
                      load_chunk(1)

                  def qk(i, X):
                      ci, kk = ktl[i]
                      kc_t, Bkc, _, _ = loaded[ci]
                      for c in range(2):
                          for gg in range(2):
                              g = 2 * X + gg
                              op("pe", MM(ps[:, 2 * X + gg, c * 256:c * 256 + nq], kc_t[32 * g:32 * g + 32, c, kk * 128:(kk + 1) * 128],
                                          Qb[32 * g:32 * g + 32, c, 0:nq], tile_position=(32 * g, 0)),
                                 reads=[Bkc, BQb], writes=[PS[2 * X], PS[2 * X + 1]])

                  def ex(i, X):
                      Et, Eb = E_aps[X][i % 2], E_buf[X][i % 2]
                      op("act", ACT(Et.rearrange("p (c g q) -> p g c q", c=2, g=2)[:, :, :, 0:nq],
                                    ps[:, 2 * X:2 * X + 2, :].rearrange("p g (c q) -> p g c q", c=2)[:, :, :, 0:nq], AF.Exp, scale=SCALE_C),
                         reads=[PS[2 * X], PS[2 * X + 1]], writes=[Eb])

                  def pv(i, X):
                      ci, kk = ktl[i]
                      _, _, vc_t, Bvc = loaded[ci]
                      Et, Eb = E_aps[X][i % 2], E_buf[X][i % 2]
                      for c in range(2):
                          hh = 2 * c + X
                          rhs = Et[:, c * 512:(c + 1) * 512].rearrange("p (g q) -> p g q", g=2)[:, :, 0:nq]
                          o = ps[64 * c:64 * c + 64, 4 + X, :].rearrange("p (g q) -> p g q", g=2)[:, :, 0:nq]
                          op("pe", MM(o, vc_t[:, kk, hh * 65:hh * 65 + 64], rhs, start=(i == 0), stop=(i == nk - 1), tile_position=(0, 64 * c),
                                      skip_group_check=True), reads=[Bvc, Eb], writes=[PS[4 + X]])
                      if i % 2 == 1:
                          Ep = E_aps[X][(i - 1) % 2]
                          Epb = E_buf[X][(i - 1) % 2]
                          if i == 1:
                              op("dve", TT(accS[X], Ep, Et, ALU.add), reads=[Epb, Eb], writes=[BaccS[X]])
                          else:
                              op("dve", TT(epair[X], Ep, Et, ALU.add), reads=[Epb, Eb], writes=[Bepair[X]])
                              op("dve", TT(accS[X], accS[X], epair[X], ALU.add), reads=[Bepair[X], BaccS[X]], writes=[BaccS[X]])
                      elif i == nk - 1:
                          if i == 0:
                              op("dve", CP(accS[X], Et), reads=[Eb], writes=[BaccS[X]])
                          else:
                              op("dve", TT(accS[X], accS[X], Et, ALU.add), reads=[Eb, BaccS[X]], writes=[BaccS[X]])

                  adv = max(1, nk // 60)
                  qk(0, 0)
                  qk(0, 1)
                  for i in range(nk):
                      ci, kk = ktl[i]
                      if kk == 0 and ci + 2 < len(chunks) and (ci + 2) not in loaded:
                          load_chunk(ci + 2)
                      for X in range(2):
                          ex(i, X)
                          if i + 1 < nk:
                              qk(i + 1, X)
                          pv(i, X)
                      if pending and i % adv == adv // 2:
                          step_pending()
                  flush_pending()

                  op("act", ACT(usb[:, 0:2, :], ps[:, 4:6, :], AF.Copy), reads=[PS[4], PS[5]], writes=[Busb])
                  for j, t in enumerate(tiles):
                      for X in range(2):
                          for c in range(2):
                              for gg in range(2):
                                  hm = (2 * c + X) * 2 + gg
                                  col = j * 8 + hm
                                  op("pe", MM(ps[:, 6, col:col + 1], accS[X][:, (c * 2 + gg) * 256 + j * 128:(c * 2 + gg) * 256 + (j + 1) * 128], ones1,
                                              start=True, stop=True, skip_group_check=True), reads=[BaccS[X], Bones], writes=[PS[6]])
                  for j, t in enumerate(tiles):
                      for X in range(2):
                          for m in range(2):
                              op("pe", TR(ps[:, j, (X * 2 + m) * 128:(X * 2 + m + 1) * 128], usb[:, X, m * 256 + j * 128:m * 256 + (j + 1) * 128], ident_f),
                                 reads=[Busb, Bidf], writes=[PS[j]])
                  ymix_l = []
                  for j, t in enumerate(tiles):
                      PU = [PS[j]]
                      fs, Bfs = fsc_ring.next()
                      rcp = fs[:, 0:8]
                      op("dve", lambda e, rcp=rcp, j=j: e.reciprocal(out=rcp, in_=ps[:, 6, j * 8:(j + 1) * 8]), reads=[PS[6]], writes=[Bfs])
                      nrl = fs[:, 8:12]
                      op("dve", TS(nrl, rcp.rearrange("p (h m) -> p h m", m=2)[:, :, 1], neglam, ALU.mult), reads=[Bfs, Blsc], writes=[Bfs])
                      osb, Bosb = osb_ring.next()
                      for hh in range(4):
                          X_, c_ = hh % 2, hh // 2
                          u1 = ps[:, j, ((X_ * 2 + 0) * 2 + c_) * 64:((X_ * 2 + 0) * 2 + c_) * 64 + 64]
                          u2 = ps[:, j, ((X_ * 2 + 1) * 2 + c_) * 64:((X_ * 2 + 1) * 2 + c_) * 64 + 64]
                          op("dve", TS(osb[:, hh * 64:(hh + 1) * 64], u1, rcp[:, 2 * hh:2 * hh + 1], ALU.mult), reads=PU + [Bfs], writes=[Bosb])
                          op("dve", STT(osb[:, hh * 64:(hh + 1) * 64], u2, nrl[:, hh:hh + 1], osb[:, hh * 64:(hh + 1) * 64], ALU.mult, ALU.add),
                             reads=PU + [Bfs, Bosb], writes=[Bosb])
                      op("dve", TT(ojk, osb, osb, ALU.mult), reads=[Bosb], writes=[Bojk])
                      op("dve", RSUM(fs[:, 12:16], ojk.rearrange("p (h d) -> p h d", h=4)), reads=[Bojk, Bfs], writes=[Bfs])
                      op("act", ACT(fs[:, 16:20], fs[:, 12:16], AF.Ln, scale=1.0 / 64, bias=eps_t), reads=[Bfs, Beps], writes=[Bfs])
                      op("act", ACT(fs[:, 20:24], fs[:, 16:20], AF.Exp, scale=-0.5), reads=[Bfs], writes=[Bfs])
                      ymix, Bym = ymix_ring.next()
                      for hh in range(4):
                          op("dve", STT(ymix[:, 768 + hh * 64:768 + (hh + 1) * 64], osb[:, hh * 64:(hh + 1) * 64], fs[:, 20 + hh:21 + hh], gsub_s,
                                        ALU.mult, ALU.mult), reads=[Bosb, Bfs, Bgsub], writes=[Bym])
                      dma("sp", ymix[:, 512:768], yb_d[t], reads=[db("yb", t)], writes=[Bym])
                      ymix_l.append((ymix, Bym))
                  def tail_tile(j, t):
                      ymix, Bym = ymix_l[j]
                      fs, Bfs = fsc_ring.next()
                      qa, Bqa = qa_ring.next()
                      dma("sp", qa, qta_d[t], reads=[db("qta", t)], writes=[Bqa])
                      if kind == "ctx":
                          slots = [(0, None), (1, None)]
                      else:
                          pslot = (t - 1, 0) if t > 2 else (NT, 2)
                          nslot = (t + 1, 1) if t < NT - 1 else (NT + 1, 3)
                          slots = [(0, None), (1, None), pslot, (t, None), nslot]
                      ns = len(slots)
                      for g in range(2):
                          for k_i, (slot, mk) in enumerate(slots):
                              op("pe", MM(ps[:, k_i, :], KTA[64 * g:64 * g + 64, slot * 128:(slot + 1) * 128], qa[64 * g:64 * g + 64, :],
                                          start=True, stop=(mk is None), tile_position=(64 * g, 0)), reads=[BKTA[slot], Bqa], writes=[PS[k_i]])
                              if mk is not None:
                                  op("pe", MM(ps[:, k_i, :], ident_b, mneg[:, mk, :], start=False, stop=True), reads=[Bidb, Bmneg], writes=[PS[k_i]])
                          eab, Beab = eab_ring.next()
                          op("act", ACT(eab[:, 0:ns, :], ps[:, 0:ns, :], AF.Exp, scale=SCALE_A), reads=PS[0:ns], writes=[Beab])
                          for k_i, (slot, mk) in enumerate(slots):
                              op("pe", MM(ps[0:65, 6 + g, :], VA[:, slot, g * 65:(g + 1) * 65], eab[:, k_i, :], start=(k_i == 0), stop=(k_i == ns - 1)),
                                 reads=[BVA[slot], Beab], writes=[PS[6 + g]])
                      op("act", ACT(uasb[0:65, :, :], ps[0:65, 6:8, :], AF.Copy), reads=[PS[6], PS[7]], writes=[Buasb])
                      for g in range(2):
                          for jh in range(4):
                              op("pe", TR(ps[:, 4 + g, jh * 65:(jh + 1) * 65], uasb[0:65, g, jh * 128:(jh + 1) * 128], ident_f[0:65, 0:65]),
                                 reads=[Buasb, Bidf], writes=[PS[4 + g]])
                      UAT = ps[:, 4:6, 0:260].rearrange("p g (j d) -> p g j d", j=4)
                      den = fs[:, 24:32]
                      op("dve", TT(den.rearrange("p (g j) -> p g j", g=2), UAT[:, :, :, 64], esink.rearrange("p (g j) -> p g j", g=2), ALU.add),
                         reads=[PS[4], PS[5], Besink, Bfs], writes=[Bfs])
                      op("dve", lambda e, den=den: e.reciprocal(out=den, in_=den), reads=[Bfs], writes=[Bfs])
                      for g in range(2):
                          op("dve", TT(ymix[:, g * 256:(g + 1) * 256].rearrange("p (j d) -> p j d", j=4), UAT[:, g, :, 0:64],
                                       den[:, g * 4:(g + 1) * 4].unsqueeze(2).broadcast_to([128, 4, 64]), ALU.mult),
                             reads=[PS[4 + g], Bfs], writes=[Bym])
                      yield

                  def tail2(j, t, ymix, Bym):
                      pb = 6 + j
                      P_ = PS[pb]
                      for kc in range(8):
                          op("pe", TR(psb[pb][:, kc * 128:(kc + 1) * 128], ymix[:, kc * 128:(kc + 1) * 128], ident_b), reads=[Bym, Bidb], writes=[P_])
                      ymT, BymT = ymT_ring.next()
                      x2t, Bx2 = x2_ring.next()
                      src, sdeps = xsrc(l, t)
                      dma("sp", x2t, src, reads=sdeps, writes=[Bx2])
                      yield
                      op("dve", CP(ymT, psb[pb].rearrange("p (k t) -> p k t", k=8)), reads=[P_], writes=[BymT])
                      yield
                      ytm, Byt = yt_ring.next()
                      for hf in range(2):
                          for kc in range(8):
                              op("pe", MM(ps[:, pb, :], ymT[:, kc, :], wout[:, kc, hf * 512:(hf + 1) * 512], start=(kc == 0), stop=(kc == 7)),
                                 reads=[BymT, Bwout], writes=[P_])
                          yield
                          op("dve", TT(ytm[:, hf * 512:(hf + 1) * 512], ps[:, pb, :], GT1[:, hf * 512:(hf + 1) * 512], ALU.mult),
                             reads=[P_, BGT1], writes=[Byt])
                          yield
                      xm, Bxm = xm_ring.next()
                      op("dve", TT(xm, ytm, x2t, ALU.add), reads=[Byt, Bx2], writes=[Bxm])
                      dma("sp", xmid[t], xm, reads=[Bxm], writes=[db("xmid", t)])
                      rs, Brs = rsc_ring.next()
                      op("dve", TT(ytm, xm, xm, ALU.mult), reads=[Bxm], writes=[Byt])
                      op("dve", RSUM(rs[:, 0:1], ytm), reads=[Byt], writes=[Brs])
                      yield
                      rstd_from_ss(rs[:, 0:1], Brs, rs[:, 1:2], Brs, rs[:, 2:3], Brs, D)
                      yield
                      op("dve", STT(ytm, xm, rs[:, 2:3], G2, ALU.mult, ALU.mult), reads=[Bxm, Brs, BG2], writes=[Byt])
                      h2, Bh2 = h2_ring.next()
                      op("dve", TT(h2, ytm, SH2, ALU.add), reads=[Byt, BSH2], writes=[Bh2])
                      yield
                      h2Tf, Bh2Tf = h2Tf_ring.next()
                      h2Tb, Bh2Tb = h2Tb_ring.next()
                      for hb in range(2):
                          for kq in range(4):
                              kc = 4 * hb + kq
                              op("pe", TR(ps[:, pb, kq * 128:(kq + 1) * 128], h2[:, kc * 128:(kc + 1) * 128], ident_f), reads=[Bh2, Bidf], writes=[P_])
                          yield
                          h2ps = ps[:, pb, :].rearrange("p (k t) -> p k t", k=4)
                          op("dve", CP(h2Tf[:, 4 * hb:4 * hb + 4, :], h2ps), reads=[P_], writes=[Bh2Tf])
                          op("dve", CP(h2Tb[:, 4 * hb:4 * hb + 4, :], h2ps), reads=[P_], writes=[Bh2Tb])
                          yield
                      dma("sp", h2t_d[:, :, t * 128:(t + 1) * 128], h2Tb, reads=[Bh2Tb], writes=[db("h2t", t)])
                      for kc in range(8):
                          op("pe", MM(ps[:, pb, 0:20], h2Tf[:, kc, :], wr[:, kc, :], start=(kc == 0), stop=(kc == 7)), reads=[Bh2Tf, Bwr], writes=[P_])
                      yield
                      lg = rs[:, 4:24]
                      op("dve", TT(lg, ps[:, pb, 0:20], brb, ALU.add), reads=[P_, Bbr, Brs], writes=[Brs])
                      lgG = lg[:, 0:4]
                      le = lg[:, 4:20].rearrange("p (g j) -> p g j", g=4)
                      mx = rs[:, 24:25]
                      op("dve", RMAX(mx, lgG), reads=[Brs], writes=[Brs])
                      shf = rs[:, 25:29]
                      op("dve", TS(shf, lgG, mx, ALU.subtract), reads=[Brs], writes=[Brs])
                      yield
                      exg = rs[:, 29:33]
                      sume = rs[:, 33:34]
                      op("act", ACT(exg, shf, AF.Exp), reads=[Brs], writes=[Brs])
                      yield
                      op("dve", RSUM(sume, exg), reads=[Brs], writes=[Brs])
                      ptop = rs[:, 34:35]
                      op("dve", lambda e, ptop=ptop, sume=sume: e.reciprocal(out=ptop, in_=sume), reads=[Brs], writes=[Brs])
                      oh = rs[:, 35:39]
                      op("dve", TS(oh, lgG, mx, ALU.is_equal), reads=[Brs], writes=[Brs])
                      tmp16 = rs[:, 40:56]
                      op("dve", TT(tmp16.rearrange("p (g j) -> p g j", g=4), le, oh.unsqueeze(2).broadcast_to([128, 4, 4]), ALU.mult), reads=[Brs], writes=[Brs])
                      yield
                      leg = rs[:, 56:60]
                      op("dve", RSUM(leg, tmp16.rearrange("p (g j) -> p j g", g=4)), reads=[Brs], writes=[Brs])
                      m1 = rs[:, 60:61]
                      op("dve", RMAX(m1, leg), reads=[Brs], writes=[Brs])
                      mk1 = rs[:, 61:65]
                      op("dve", TS(mk1, leg, m1, ALU.is_equal), reads=[Brs], writes=[Brs])
                      le2 = rs[:, 65:69]
                      op("dve", STT(le2, mk1, -1e30, leg, ALU.mult, ALU.add), reads=[Brs], writes=[Brs])
                      yield
                      m2 = rs[:, 69:70]
                      op("dve", RMAX(m2, le2), reads=[Brs], writes=[Brs])
                      mk2 = rs[:, 70:74]
                      op("dve", TS(mk2, le2, m2, ALU.is_equal), reads=[Brs], writes=[Brs])
                      d21 = rs[:, 74:75]
                      op("dve", TT(d21, m2, m1, ALU.subtract), reads=[Brs], writes=[Brs])
                      yield
                      e21 = rs[:, 75:76]
                      op("act", ACT(e21, d21, AF.Exp), reads=[Brs], writes=[Brs])
                      yield
                      w1 = rs[:, 76:77]
                      op("dve", TS(w1, e21, 1.0, ALU.add), reads=[Brs], writes=[Brs])
                      op("dve", lambda e, w1=w1: e.reciprocal(out=w1, in_=w1), reads=[Brs], writes=[Brs])
                      op("dve", TT(w1, w1, ptop, ALU.mult), reads=[Brs], writes=[Brs])
                      w2 = rs[:, 77:78]
                      op("dve", TT(w2, w1, e21, ALU.mult), reads=[Brs], writes=[Brs])
                      yield
                      gj = rs[:, 78:82]
                      op("dve", TS(gj, mk1, w1, ALU.mult), reads=[Brs], writes=[Brs])
                      op("dve", STT(gj, mk2, w2, gj, ALU.mult, ALU.add), reads=[Brs], writes=[Brs])
                      op("dve", TT(gates[:, t, :].rearrange("p (g j) -> p g j", g=4), oh.unsqueeze(2).broadcast_to([128, 4, 4]),
                                   gj.unsqueeze(1).broadcast_to([128, 4, 4]), ALU.mult), reads=[Brs], writes=[Bgate[t]])

                  run_interleaved((tail_tile(j, t) for j, t in enumerate(tiles)), 2)
                  if kind == "ctx":
                      run_interleaved((tail2(j, t, *ymix_l[j]) for j, t in enumerate(tiles)), 2)
                  else:
                      pending.extend([tail2(j, t, *ymix_l[j]) for j, t in enumerate(tiles)])
              flush_pending()
              A.release()
              A.release()
              fw.barrier()
              if cfg.stop_after == ("p2", l):
                  break

              A.mark()
              p3tiles = list(range(NT)) if not last else list(range(2, NT))
              if len(p3tiles) <= 9:
                  sblocks = [p3tiles]
              else:
                  hsz = (len(p3tiles) + 1) // 2
                  sblocks = [p3tiles[:hsz], p3tiles[hsz:]]
              SBT = max(len(x) for x in sblocks)
              GT2 = [A.alloc([D], F32) for _ in range(2)]; BGT2 = [Buf(), Buf()]
              for r in ((0,) if last else (0, 1)):
                  load_mod(l, r, 5, GT2[r], BGT2[r])
              if last:
                  gfin = A.alloc([D], F32); Bgfin = Buf()
                  load_gain(I["g_final"], gfin, Bgfin)
              h2sb = A.alloc([8, SBT * 128], BF16); Bh2sb = Buf()
              yacc = A.alloc([SBT, D], F32); Byacc = [Buf() for _ in range(SBT)]
              wg_ring = Ring(2, [8, DEXP], BF16)
              wu_ring = Ring(2, [8, DEXP], BF16)
              wd_ring = Ring(2, [4, D], BF16)
              sg_ring = Ring(2, [512], F32)
              he_ring = Ring(2, [4, 512], BF16)
              xm3_ring = Ring(2, [D], F32)
              fo_ring = Ring(2, [D], F32)
              f3_ring = Ring(2, [8], F32)
              junk3 = A.alloc([D], BF16); Bjunk3 = Buf()
              ycnt = 0
              for tl in sblocks:
                  ntok = len(tl) * 128
                  dma("sp", h2sb[:, :, 0:ntok], h2t_d[:, :, tl[0] * 128:(tl[-1] + 1) * 128], reads=[db("h2t", t) for t in tl], writes=[Bh2sb])
                  for e_ in range(NEXP):
                      wg, Bwg = wg_ring.next()
                      wu, Bwu = wu_ring.next()
                      wd, Bwd = wd_ring.next()
                      dma("pool", wg, I["w_gate"][l, e_].rearrange("(k p) n -> p k n", p=128), writes=[Bwg])
                      dma("pool", wu, I["w_up"][l, e_].rearrange("(k p) n -> p k n", p=128), writes=[Bwu])
                      dma("pool", wd, I["w_down"][l, e_].rearrange("(k p) n -> p k n", p=128), writes=[Bwd])
                      for b0 in range(0, ntok, 512):
                          n = min(512, ntok - b0)
                          he, Bhe = he_ring.next()
                          for dc in range(4):
                              gb, ub = dc % 2, 2 + dc % 2
                              for kc in range(8):
                                  op("pe", MM(ps[:, gb, 0:n], wg[:, kc, dc * 128:(dc + 1) * 128], h2sb[:, kc, b0:b0 + n], start=(kc == 0), stop=(kc == 7)),
                                     reads=[Bwg, Bh2sb], writes=[PS[gb]])
                              for kc in range(8):
                                  op("pe", MM(ps[:, ub, 0:n], wu[:, kc, dc * 128:(dc + 1) * 128], h2sb[:, kc, b0:b0 + n], start=(kc == 0), stop=(kc == 7)),
                                     reads=[Bwu, Bh2sb], writes=[PS[ub]])
                              sg, Bsg = sg_ring.next()
                              op("act", ACT(sg[:, 0:n], ps[:, gb, 0:n], AF.Silu), reads=[PS[gb]], writes=[Bsg])
                              op("dve", TT(he[:, dc, 0:n], sg[:, 0:n], ps[:, ub, 0:n], ALU.mult), reads=[Bsg, PS[ub]], writes=[Bhe])
                          for j in range(n // 128):
                              ti = b0 // 128 + j
                              t = tl[ti]
                              for hf in range(2):
                                  yb_ = 4 + (ycnt % 4)
                                  ycnt += 1
                                  for dc in range(4):
                                      op("pe", MM(ps[:, yb_, :], he[:, dc, j * 128:(j + 1) * 128], wd[:, dc, hf * 512:(hf + 1) * 512], start=(dc == 0), stop=(dc == 3)),
                                         reads=[Bhe, Bwd], writes=[PS[yb_]])
                                  ya_ = yacc[:, ti, hf * 512:(hf + 1) * 512]
                                  if e_ == 0:
                                      op("dve", TS(ya_, ps[:, yb_, :], gates[:, t, e_:e_ + 1], ALU.mult), reads=[PS[yb_], Bgate[t]], writes=[Byacc[ti]])
                                  else:
                                      op("dve", STT(ya_, ps[:, yb_, :], gates[:, t, e_:e_ + 1], ya_, ALU.mult, ALU.add),
                                         reads=[PS[yb_], Bgate[t], Byacc[ti]], writes=[Byacc[ti]])
                  for ti, t in enumerate(tl):
                      r = 1 if t < 2 else 0
                      xm3, Bxm3 = xm3_ring.next()
                      dma("sp", xm3, xmid[t], reads=[db("xmid", t)], writes=[Bxm3])
                      op("dve", TT(yacc[:, ti, :], yacc[:, ti, :], GT2[r], ALU.mult), reads=[Byacc[ti], BGT2[r]], writes=[Byacc[ti]])
                      xo, Bxo = xm3, Bxm3
                      op("dve", TT(xo, yacc[:, ti, :], xm3, ALU.add), reads=[Byacc[ti], Bxm3], writes=[Bxo])
                      if not last:
                          dma("sp", xbuf[t], xo, reads=[Bxo], writes=[db("xbuf", t)])
                      else:
                          f3, Bf3 = f3_ring.next()
                          fo, Bfo = fo_ring.next()
                          op("dve", TT(fo, xo, xo, ALU.mult), reads=[Bxo], writes=[Bfo])
                          op("dve", RSUM(f3[:, 0:1], fo), reads=[Bfo], writes=[Bf3])
                          rstd_from_ss(f3[:, 0:1], Bf3, f3[:, 1:2], Bf3, f3[:, 2:3], Bf3, D)
                          op("dve", STT(fo, xo, f3[:, 2:3], gfin, ALU.mult, ALU.mult), reads=[Bxo, Bf3, Bgfin], writes=[Bfo])
                          dma("sp", out_d[(t - 2) * 128:(t - 1) * 128, :], fo, reads=[Bfo], writes=[db("out", t)])
              A.release()
              A.release()
              fw.barrier()

        except StopBuild:
            pass
        fw.barrier()
        blk = st.enter_context(nc.Block())
        stats = fw.finish(blk)
    return nc, stats


def _rope_table(cfg, r):
    NT, NLAT = cfg.NT, cfg.NLAT
    tab = np.zeros((128, NT, 96), np.float32)
    tab[:, :, 0:32] = 1.0
    tab[:, :, 64:80] = 1.0
    invA = (np.float32(10000.0) ** (-np.arange(16, dtype=np.float32) / np.float32(16))).astype(np.float32)
    invC = (np.float32(10000.0) ** (-np.arange(8, dtype=np.float32) / np.float32(8))).astype(np.float32)
    for t in range(2, NT):
        tok = r * NLAT * 128 + (t - 2) * 128 + np.arange(128)
        rows = (tok // GRID_W).astype(np.float32)
        cols = (tok % GRID_W).astype(np.float32)
        for pi, pos in enumerate((rows, cols)):
            angA = (pos[:, None] * invA[None, :]).astype(np.float32)
            angC = (pos[:, None] * invC[None, :]).astype(np.float32)
            tab[:, t, pi * 16:(pi + 1) * 16] = np.cos(angA)
            tab[:, t, 32 + pi * 16:32 + (pi + 1) * 16] = np.sin(angA)
            tab[:, t, 64 + pi * 8:64 + (pi + 1) * 8] = np.cos(angC)
            tab[:, t, 80 + pi * 8:80 + (pi + 1) * 8] = np.sin(angC)
    return tab.reshape(128, NT * 96)


_WIN_PERM = np.arange(DIN)
for _g in range(2):
    for _j in range(4):
        _WIN_PERM[(_j * 2 + _g) * 64:(_j * 2 + _g + 1) * 64] = np.arange((_g * 4 + _j) * 64, (_g * 4 + _j + 1) * 64)


def make_in_maps(cfg, inputs):
    NLAT, RANKS, NB = cfg.NLAT, cfg.RANKS, cfg.NB
    f = lambda a: np.ascontiguousarray(np.asarray(a, dtype=np.float32))
    x, c, ctx, c_ctx = f(inputs["x"]), f(inputs["c"]), f(inputs["ctx"]), f(inputs["c_ctx"])
    dp = cfg.DEPTH
    shared = {
        "w_ada": f(inputs["w_ada"]),
        "b_ada2": f(np.repeat(f(inputs["b_ada"])[:, None, :], 2, axis=1)),
        "g_mix": f(inputs["g_mix"]), "w_in": f(f(inputs["w_in"])[:, :, _WIN_PERM]), "sink": f(inputs["sink"]),
        "ws_tok": f(inputs["ws_tok"]), "bs_tok": f(inputs["bs_tok"]), "g_tok": f(inputs["g_tok"]).reshape(dp, 256),
        "lam4": f(np.stack([f(inputs["lam_q1"]), f(inputs["lam_k1"]), f(inputs["lam_q2"]), f(inputs["lam_k2"])], axis=1)).reshape(dp, 128),
        "g_sub": f(inputs["g_sub"]), "w_out": f(inputs["w_out"]), "g_ffn": f(inputs["g_ffn"]),
        "w_r": f(np.concatenate([f(inputs["w_rg"]), f(inputs["w_re"])], axis=-1)),
        "b_r": f(np.concatenate([f(inputs["b_rg"]), f(inputs["b_re"])], axis=-1)),
        "w_gate": f(inputs["w_gate"]), "w_up": f(inputs["w_up"]), "w_down": f(inputs["w_down"]),
        "g_final": f(inputs["g_final"]).reshape(1, D),
        "ident": np.eye(128, dtype=np.float32),
    }
    kk = np.arange(128)[:, None]
    qq = np.arange(128)[None, :]
    mL = (qq <= kk).astype(np.float32)
    mU = (kk <= qq).astype(np.float32)
    maps = []
    for b in range(NB):
        for r in range(RANKS):
            m = dict(shared)
            m["xl"] = f(x[b, r * NLAT * 128:(r + 1) * NLAT * 128, :])
            m["ctx"] = f(ctx[b])
            m["c2"] = f(np.stack([c[b], c_ctx], axis=0))
            mk = np.stack([mL, mU, mL if r > 0 else np.zeros_like(mL), mU if r < RANKS - 1 else np.zeros_like(mU)], axis=1)
            m["masks"] = f(mk.reshape(128, 4 * 128))
            m["rope"] = _rope_table(cfg, r)
            s = np.zeros((3, RANKS), np.float32)
            s[0, r] = 1.0
            if r > 0:
                s[1, r - 1] = 1.0
            if r < RANKS - 1:
                s[2, r + 1] = 1.0
            m["sel"] = f(np.broadcast_to(s.reshape(1, 3 * RANKS), (128, 3 * RANKS)))
            maps.append(m)
    return maps


_CACHE = {}


def kernel(**inputs):
    x = np.asarray(inputs["x"])
    NB, S, _ = x.shape
    RANKS = 8 // NB
    NLAT = S // (RANKS * 128)
    cfg = Cfg(NLAT=NLAT, RANKS=RANKS, NB=NB, DEPTH=int(np.asarray(inputs["w_in"]).shape[0]))
    key = (NLAT, RANKS, NB, cfg.DEPTH)
    if key not in _CACHE:
        _CACHE[key] = build_program(cfg)[0]
    nc = _CACHE[key]
    maps = make_in_maps(cfg, inputs)
    res = run_bass_kernel_spmd(nc, maps, core_ids=list(range(cfg.NCORES)))
    out = np.zeros((NB, S, D), np.float32)
    for b in range(NB):
        for r in range(RANKS):
            out[b, r * NLAT * 128:(r + 1) * NLAT * 128, :] = np.asarray(res.results[b * RANKS + r]["out"])
    return out
```

```python
import math
from contextlib import ExitStack

import numpy as np
import concourse.bass as bass
import concourse.mybir as mybir
from concourse.bass_utils import run_bass_kernel_spmd

F32 = mybir.dt.float32
BF16 = mybir.dt.bfloat16
U8 = mybir.dt.uint8
AF = mybir.ActivationFunctionType
ALU = mybir.AluOpType
AX = mybir.AxisListType

D = 1024
DIN = 2048
EPS = 1e-6
GRID_W = 64
NEXP = 16
DEXP = 512
SCALE_A = 64 ** -0.5
SCALE_C = 32 ** -0.5


class Rec:
    __slots__ = ("q", "fn", "waits", "need", "val", "sem", "inc", "key")

    def __init__(self, q, fn):
        self.q, self.fn = q, fn
        self.waits = []
        self.need = False
        self.val = None
        self.sem = None
        self.inc = 1
        self.key = q.name


class Buf:
    __slots__ = ("name", "w", "r", "excl")

    def __init__(self, name="", excl=False):
        self.name = name
        self.w = None
        self.r = {}
        self.excl = excl


class Queue:
    def __init__(self, name, eng, sem):
        self.name, self.eng, self.sem = name, eng, sem
        self.recs = []
        self.dsems = []
        self.dlast = []
        self.dnext = 0


class FW:
    def __init__(self, nc, stack, n_dsem_sp=24, n_dsem_pool=12, n_cc=10):
        self.nc = nc
        self.Q = {}
        for name, eng in (("pe", nc.tensor), ("act", nc.scalar), ("dve", nc.vector), ("pool", nc.gpsimd), ("sp", nc.sync)):
            sem = stack.enter_context(nc.semaphore("q_" + name))
            self.Q[name] = Queue(name, eng, sem)
        for qn, n in (("sp", n_dsem_sp), ("pool", n_dsem_pool)):
            q = self.Q[qn]
            for i in range(n):
                q.dsems.append(stack.enter_context(nc.semaphore(f"d_{qn}{i}")))
                q.dlast.append(None)
        self.ccsems = [stack.enter_context(nc.semaphore(f"cc{i}")) for i in range(n_cc)]
        self.cclast = [None] * n_cc
        self.ccnext = 0
        self.all_dma = []

    def _deps(self, q, reads, writes):
        deps = []
        for b in reads:
            if b.w is not None:
                deps.append(b.w)
            if b.excl:
                deps.extend(x for x in b.r.values() if x.q is not q)
        for b in writes:
            if b.w is not None:
                deps.append(b.w)
            deps.extend(b.r.values())
        out = []
        seen = set()
        for d in deps:
            if id(d) in seen:
                continue
            seen.add(id(d))
            if d.q is q and q.name == "pe" and d.key == "pe":
                continue
            out.append(d)
        return out

    def _commit(self, rec, reads, writes):
        for d in rec.waits:
            d.need = True
        rec.q.recs.append(rec)
        for b in reads:
            b.r[rec.key] = rec
        for b in writes:
            b.w = rec
            b.r = {}

    def op(self, qn, fn, reads=(), writes=()):
        q = self.Q[qn]
        rec = Rec(q, fn)
        rec.waits = self._deps(q, reads, writes)
        self._commit(rec, reads, writes)
        return rec

    def dma(self, qn, out, in_, reads=(), writes=(), **kw):
        q = self.Q[qn]
        rec = Rec(q, lambda e: e.dma_start(out=out, in_=in_, **kw))
        i = q.dnext
        q.dnext = (q.dnext + 1) % len(q.dsems)
        rec.key = f"{qn}_d{i}"
        rec.sem = q.dsems[i]
        rec.inc = 16
        rec.need = True
        rec.waits = self._deps(q, reads, writes)
        if q.dlast[i] is not None:
            rec.waits.append(q.dlast[i])
        q.dlast[i] = rec
        self._commit(rec, reads, writes)
        self.all_dma.append(rec)
        return rec

    def cc(self, fn, reads=(), writes=()):
        q = self.Q["pool"]
        rec = Rec(q, fn)
        i = self.ccnext
        self.ccnext = (self.ccnext + 1) % len(self.ccsems)
        rec.key = f"cc{i}"
        rec.sem = self.ccsems[i]
        rec.inc = 1
        rec.need = True
        rec.waits = self._deps(q, reads, writes)
        if self.cclast[i] is not None:
            rec.waits.append(self.cclast[i])
        self.cclast[i] = rec
        self._commit(rec, reads, writes)
        return rec

    def barrier(self):
        lasts = []
        for q in self.Q.values():
            for r in reversed(q.recs):
                if r.key == q.name and r.fn is not None:
                    lasts.append(r)
                    break
            lasts.extend(x for x in q.dlast if x is not None)
        lasts.extend(x for x in self.cclast if x is not None)
        for q in self.Q.values():
            rec = Rec(q, None)
            rec.waits = [d for d in lasts if not (d.q is q and d.key == q.name)]
            for d in rec.waits:
                d.need = True
            q.recs.append(rec)

    def finish(self, block):
        for q in self.Q.values():
            cnt = 0
            dcnt = {}
            for r in q.recs:
                if r.fn is None:
                    continue
                if r.key != q.name:
                    dcnt[r.key] = dcnt.get(r.key, 0) + r.inc
                    r.val = dcnt[r.key]
                elif r.need:
                    cnt += 1
                    r.val = cnt
                    r.sem = q.sem
        stats = {}
        for q in self.Q.values():
            def run(eng, q=q):
                seen = {}
                nw = 0
                for r in q.recs:
                    for d in r.waits:
                        k = d.key
                        if seen.get(k, 0) >= d.val:
                            continue
                        eng.wait_ge(d.sem, d.val)
                        seen[k] = d.val
                        nw += 1
                    if r.fn is None:
                        continue
                    ins = r.fn(eng)
                    if r.need:
                        ins.then_inc(r.sem, r.inc)
                stats[q.name] = (len(q.recs), nw)
            getattr(block, {"pe": "tensor", "act": "scalar", "dve": "vector", "pool": "gpsimd", "sp": "sync"}[q.name])(run)
        return stats


class Arena:
    def __init__(self, handle, nbytes):
        self.h, self.n = handle, nbytes
        self.off = 0
        self.marks = []

    def alloc(self, shape, dtype, parts=128):
        esz = {F32: 4, BF16: 2, U8: 1}[dtype]
        n = int(np.prod(shape)) * esz
        n_al = (n + 63) // 64 * 64
        assert self.off + n_al <= self.n, f"SBUF arena overflow: {self.off}+{n_al} > {self.n}"
        ap = self.h[0:parts, self.off:self.off + n]
        self.off += n_al
        self.peak = max(getattr(self, 'peak', 0), self.off)
        if dtype != U8:
            ap = ap.bitcast(dtype)
        if len(shape) == 1:
            return ap
        names = [f"a{i}" for i in range(len(shape))]
        pat = "p (" + " ".join(names) + ") -> p " + " ".join(names)
        return ap.rearrange(pat, **{nm: s for nm, s in zip(names[:-1], shape[:-1])})

    def mark(self):
        self.marks.append(self.off)

    def release(self):
        print('arena scope peak', getattr(self, 'peak', 0), 'at release, off', self.off)
        self.off = self.marks.pop()


def bcast_rows(ap_row, n):
    return ap_row.partition_broadcast(n) if ap_row.shape[0] != 1 else ap_row.broadcast_to([n] + list(ap_row.shape[1:]))


def MM(out, lhsT, rhs, start=True, stop=True, **kw):
    return lambda e: e.matmul(out, lhsT=lhsT, rhs=rhs, start=start, stop=stop, **kw)


def TR(out, in_, ident):
    return lambda e: e.transpose(out, in_, ident)


def ACT(out, in_, func, **kw):
    return lambda e: e.activation(out=out, in_=in_, func=func, **kw)


def TT(out, a, b, op):
    return lambda e: e.tensor_tensor(out=out, in0=a, in1=b, op=op)


def TS(out, a, s1, op0, s2=None, op1=None, **kw):
    if op1 is None:
        return lambda e: e.tensor_scalar(out=out, in0=a, scalar1=s1, scalar2=None, op0=op0, **kw)
    return lambda e: e.tensor_scalar(out=out, in0=a, scalar1=s1, scalar2=s2, op0=op0, op1=op1, **kw)


def STT(out, a, s, b, op0, op1):
    return lambda e: e.scalar_tensor_tensor(out=out, in0=a, scalar=s, in1=b, op0=op0, op1=op1)


def CP(out, in_):
    return lambda e: e.tensor_copy(out=out, in_=in_)


def MEMSET(ap, v):
    return lambda e: e.memset(ap, v)


def RSUM(out, in_, axis=None):
    return lambda e: e.reduce_sum(out=out, in_=in_, axis=axis or AX.X)


def RMAX(out, in_, axis=None):
    return lambda e: e.reduce_max(out=out, in_=in_, axis=axis or AX.X)


class Cfg:
    def __init__(self, NLAT=32, RANKS=4, NB=2, DEPTH=2, stop_after=None):
        self.NLAT, self.RANKS, self.NB, self.DEPTH = NLAT, RANKS, NB, DEPTH
        self.NT = NLAT + 2
        self.NKC = 2 + RANKS * NLAT
        self.NCORES = NB * RANKS
        self.stop_after = stop_after


ARENA_BYTES = 207 * 1024


class StopBuild(Exception):
    pass


def build_program(cfg):
    NLAT, RANKS, NT, NKC, DEPTH = cfg.NLAT, cfg.RANKS, cfg.NT, cfg.NKC, cfg.DEPTH
    KCH = 8 if NLAT % 8 == 0 else (2 if NLAT % 2 == 0 else 1)
    nc = bass.Bass("TRN2", target_bir_lowering=False)
    dbg = set(getattr(cfg, "debug", ()) or ())
    I = {}

    def inp(name, shape):
        I[name] = nc.dram_tensor(name, shape, F32, kind="ExternalInput").ap()

    inp("xl", [NLAT * 128, D]); inp("ctx", [256, D]); inp("c2", [2, D])
    inp("w_ada", [DEPTH, D, 6 * D]); inp("b_ada2", [DEPTH, 2, 6 * D]); inp("g_mix", [DEPTH, D])
    inp("w_in", [DEPTH, D, DIN]); inp("sink", [DEPTH, 8]); inp("ws_tok", [DEPTH, 4, 128, 128])
    inp("bs_tok", [DEPTH, 4, 128]); inp("g_tok", [DEPTH, 256]); inp("lam4", [DEPTH, 128]); inp("g_sub", [DEPTH, 64])
    inp("w_out", [DEPTH, D, D]); inp("g_ffn", [DEPTH, D]); inp("w_r", [DEPTH, D, 20]); inp("b_r", [DEPTH, 20])
    inp("w_gate", [DEPTH, NEXP, D, DEXP]); inp("w_up", [DEPTH, NEXP, D, DEXP]); inp("w_down", [DEPTH, NEXP, DEXP, D])
    inp("g_final", [1, D]); inp("ident", [128, 128]); inp("masks", [128, 4 * 128]); inp("rope", [128, NT * 96])
    inp("sel", [128, 3 * RANKS])
    out_d = nc.dram_tensor("out", [NLAT * 128, D], F32, kind="ExternalOutput").ap()

    def scr(name, shape, dt):
        kind = "ExternalOutput" if name in dbg else "Internal"
        return nc.dram_tensor(name, shape, dt, kind=kind).ap()

    modd = scr("modd", [DEPTH, 2, 6 * D], F32)
    xbuf = scr("xbuf", [NT, 128, D], F32)
    xmid = scr("xmid", [NT, 128, D], F32)
    qta_d = scr("qta_d", [NT, 128, 512], BF16)
    qtc_d = scr("qtc_d", [NT, 128, 256], BF16)
    yb_d = scr("yb_d", [NT, 128, 256], BF16)
    h2t_d = scr("h2t_d", [128, 8, NT * 128], BF16)
    kctx_d = scr("kctx_d", [128, 2, 256], BF16)
    vctx_d = scr("vctx_d", [2, 128, 260], BF16)
    xk_s = scr("xk_s", [RANKS, 128, 2, NLAT * 128], BF16)
    xv_s = scr("xv_s", [RANKS, NLAT, 128, 260], BF16)
    xh_s = scr("xh_s", [RANKS, 128, 2 * 258], BF16)
    if RANKS > 1:
        xk_r = scr("xk_r", [RANKS, 128, 2, NLAT * 128], BF16)
        xv_r = scr("xv_r", [RANKS, NLAT, 128, 260], BF16)
        xh_r = scr("xh_r", [RANKS, 128, 2 * 258], BF16)
    else:
        xk_r, xv_r, xh_r = xk_s, xv_s, xh_s

    DB = {}

    def db(name, idx=0):
        k = (name, idx)
        if k not in DB:
            DB[k] = Buf(f"{name}{idx}")
        return DB[k]

    with ExitStack() as st:
        arena_h = st.enter_context(nc.sbuf_tensor("arena", [128, ARENA_BYTES], U8))
        ps = st.enter_context(nc.psum_tensor("ps", [128, 8, 512], F32))
        fw = FW(nc, st)
        A = Arena(arena_h, ARENA_BYTES)
        PS = [Buf(f"ps{i}", excl=True) for i in range(8)]
        psb = [ps[:, i, :].bitcast(BF16) for i in range(8)]

        class Ring:
            def __init__(self, n, shape, dt, parts=128):
                self.aps = [A.alloc(shape, dt, parts) for _ in range(n)]
                self.bufs = [Buf() for _ in range(n)]
                self.i = 0

            def next(self):
                k = self.i
                self.i = (self.i + 1) % len(self.aps)
                return self.aps[k], self.bufs[k]

        op, dma = fw.op, fw.dma

        ident_f = A.alloc([128], F32); Bidf = Buf()
        ident_b = A.alloc([128], BF16); Bidb = Buf()
        masks = A.alloc([4, 128], BF16); Bmask = Buf()
        sel = A.alloc([3 * RANKS], F32); Bsel = Buf()
        dma("sp", ident_f, I["ident"], writes=[Bidf])
        dma("pool", ident_b, I["ident"], writes=[Bidb])
        dma("pool", masks, I["masks"].rearrange("p (m q) -> p m q", m=4), writes=[Bmask])
        dma("sp", sel, I["sel"], writes=[Bsel])
        mneg = A.alloc([4, 512], BF16); Bmneg = Buf()
        for m_ in range(4):
            op("dve", TS(mneg[:, m_, :].rearrange("p (j q) -> p j q", j=4), masks[:, m_, :].unsqueeze(1).broadcast_to([128, 4, 128]),
                         -1.0, ALU.add, 30000.0, ALU.mult), reads=[Bmask], writes=[Bmneg])
        ones1 = A.alloc([1], F32); Bones = Buf()
        op("pool", MEMSET(ones1, 1.0), writes=[Bones])

        gates = A.alloc([NT, 16], F32)
        Bgate = [Buf() for _ in range(NT)]

        eps_t = A.alloc([1], F32); Beps = Buf()
        op("pool", MEMSET(eps_t, EPS), writes=[Beps])

        A.mark()
        c_raw = A.alloc([2, 8], F32); Bcraw = Buf()
        cS = A.alloc([8, 2], F32); BcS = Buf()
        dma("sp", c_raw, I["c2"].rearrange("r (p k) -> p r k", p=128), writes=[Bcraw])
        op("act", ACT(cS, c_raw.rearrange("p r k -> p k r"), AF.Silu), reads=[Bcraw], writes=[BcS])
        wa_ring = Ring(2, [8, 512], F32)
        bada = A.alloc([6 * D], F32, parts=2); Bbada = Buf()
        msb_ring = Ring(2, [512], F32, parts=2)
        import os as _os
        for l in range(DEPTH if not _os.environ.get("DBG_NOADA") else 0):
            dma("sp", bada, I["b_ada2"][l], writes=[Bbada])
            for cb in range(12):
                wa, Bwa = wa_ring.next()
                dma("sp", wa, I["w_ada"][l, :, cb * 512:(cb + 1) * 512].rearrange("(p k) n -> p k n", p=128), writes=[Bwa])
                for kc in range(8):
                    op("pe", MM(ps[0:2, 0, :], cS[:, kc, :], wa[:, kc, :], start=(kc == 0), stop=(kc == 7)),
                       reads=[BcS, Bwa], writes=[PS[0]])
                msb, Bmsb = msb_ring.next()
                op("dve", TT(msb, ps[0:2, 0, :], bada[:, cb * 512:(cb + 1) * 512], ALU.add), reads=[PS[0], Bbada], writes=[Bmsb])
                dma("sp", modd[l, :, cb * 512:(cb + 1) * 512], msb, reads=[Bmsb], writes=[db("modd", l)])
        A.release()
        fw.barrier()

        def load_mod(l, r, chunk, dst, buf, q="sp"):
            return dma(q, dst, modd[l, r:r + 1, chunk * D:(chunk + 1) * D].broadcast_to([128, D]),
                       reads=[db("modd", l)], writes=[buf])

        def load_gain(src_row, dst, buf, q="sp"):
            return dma(q, dst, src_row.broadcast_to([128] + [src_row.shape[1]]), writes=[buf])

        def xsrc(l, t):
            if l == 0:
                return (I["ctx"][t * 128:(t + 1) * 128, :], []) if t < 2 else (I["xl"][(t - 2) * 128:(t - 1) * 128, :], [])
            return xbuf[t], [db("xbuf", t)]

        def rstd_from_ss(ss, Bss, tmp, Btmp, rstd, Brstd, n):
            op("act", ACT(tmp, ss, AF.Ln, scale=1.0 / n, bias=eps_t), reads=[Bss, Beps], writes=[Btmp])
            op("act", ACT(rstd, tmp, AF.Exp, scale=-0.5), reads=[Btmp], writes=[Brstd])

        def ck(name):
            if cfg.stop_after == name:
                raise StopBuild()

        try:
          for l in range(DEPTH):
              last = (l == DEPTH - 1)
              if cfg.stop_after == "pre":
                  break
              lam_init = 0.8 - 0.6 * math.exp(-0.3 * l)
              A.mark()
              lam4b = A.alloc([4, 32], F32); Bl4 = Buf()
              dma("sp", lam4b, I["lam4"][l:l + 1, :].broadcast_to([128, 128]).rearrange("p (a b) -> p a b", a=4), writes=[Bl4])
              lsc = A.alloc([8], F32); Blsc = Buf()
              ljunk = A.alloc([32], F32); Blj = Buf()
              op("dve", TT(ljunk, lam4b[:, 0, :], lam4b[:, 1, :], ALU.mult), reads=[Bl4], writes=[Blj])
              op("dve", RSUM(lsc[:, 0:1], ljunk), reads=[Blj], writes=[Blsc])
              op("dve", TT(ljunk, lam4b[:, 2, :], lam4b[:, 3, :], ALU.mult), reads=[Bl4], writes=[Blj])
              op("dve", RSUM(lsc[:, 1:2], ljunk), reads=[Blj, Blsc], writes=[Blsc])
              op("act", ACT(lsc[:, 2:4], lsc[:, 0:2], AF.Exp), reads=[Blsc], writes=[Blsc])
              op("dve", TT(lsc[:, 4:5], lsc[:, 2:3], lsc[:, 3:4], ALU.subtract), reads=[Blsc], writes=[Blsc])
              neglam = lsc[:, 5:6]
              op("dve", TS(neglam, lsc[:, 4:5], -1.0, ALU.mult, -lam_init, ALU.add), reads=[Blsc], writes=[Blsc])
              esink = A.alloc([8], F32); Besink = Buf()
              dma("sp", esink, I["sink"][l:l + 1, :].broadcast_to([128, 8]), writes=[Besink])
              op("act", ACT(esink, esink, AF.Exp), reads=[Besink], writes=[Besink])
              gsub_s = A.alloc([64], F32); Bgsub = Buf()
              dma("sp", gsub_s, I["g_sub"][l:l + 1, :].broadcast_to([128, 64]), writes=[Bgsub])
              op("dve", TS(gsub_s, gsub_s, 1.0 - lam_init, ALU.mult), reads=[Bgsub], writes=[Bgsub])
              A.mark()
              KTA = A.alloc([(NT + 2) * 128], BF16)
              VA = A.alloc([NT + 2, 130], BF16)
              BKTA = [Buf() for _ in range(NT + 2)]
              BVA = [Buf() for _ in range(NT + 2)]
              op("pool", MEMSET(VA, 1.0), writes=BVA)
              op("pool", MEMSET(KTA[:, NT * 128:(NT + 2) * 128], 0.0), writes=BKTA[NT:NT + 2])

              A.mark()
              win = A.alloc([8, DIN], BF16); Bwin = Buf()
              for kc in range(8):
                  dma("pool", win[:, kc, :], I["w_in"][l, kc * 128:(kc + 1) * 128, :], writes=[Bwin])
              rope_ring = Ring(2, [96], F32)
              G1 = [A.alloc([D], F32) for _ in range(2)]; BG1 = [Buf(), Buf()]
              SH1 = [A.alloc([D], F32) for _ in range(2)]; BSH1 = [Buf(), Buf()]
              gtmp = A.alloc([D], F32); Bgtmp = Buf()
              for r in range(2):
                  load_mod(l, r, 1, G1[r], BG1[r])
                  load_gain(I["g_mix"][l:l + 1, :], gtmp, Bgtmp)
                  op("dve", STT(G1[r], G1[r], 1.0, gtmp, ALU.add, ALU.mult), reads=[BG1[r], Bgtmp], writes=[BG1[r]])
                  load_mod(l, r, 0, SH1[r], BSH1[r])
              ws_f = A.alloc([4, 128], F32); Bwsf = Buf()
              dma("sp", ws_f, I["ws_tok"][l].rearrange("g p q -> p g q"), writes=[Bwsf])
              wsT = A.alloc([4, 128], BF16); BwsT = Buf()
              for g in range(4):
                  op("pe", TR(ps[:, 7, g * 128:(g + 1) * 128], ws_f[:, g, :], ident_f), reads=[Bwsf, Bidf], writes=[PS[7]])
              op("dve", CP(wsT, ps[:, 7, :].rearrange("p (g q) -> p g q", g=4)), reads=[PS[7]], writes=[BwsT])
              bsT = A.alloc([4], F32); BbsT = Buf()
              dma("sp", bsT, I["bs_tok"][l].rearrange("g p -> p g"), writes=[BbsT], allow_slow_non_contiguous=True)
              gtokb = A.alloc([256], F32); Bgtok = Buf()
              dma("sp", gtokb, I["g_tok"][l:l + 1, :].broadcast_to([128, 256]), writes=[Bgtok])

              ck(('p1a', l))
              x_ring = Ring(2, [D], F32)
              junk_b = A.alloc([D], BF16); Bjunk = Buf()
              sc_ring = Ring(2, [8], F32)
              t1_ring = Ring(2, [D], F32)
              h_ring = Ring(2, [D], BF16)
              hT_ring = Ring(2, [8, 128], BF16)
              psb_ring = Ring(2, [DIN], F32)
              rt_ring = Ring(2, [4, 320], F32)
              qk_ring = Ring(2, [1152], BF16)
              qta_ring = Ring(2, [512], BF16)
              qtc_ring = Ring(2, [256], BF16)
              vt = A.alloc([4, 65], BF16); Bvt = Buf()
              op("pool", MEMSET(vt, 1.0), writes=[Bvt])
              NSTG = min(2, NLAT)
              stgK_ring = Ring(2, [RANKS, 2, NSTG * 128], BF16)
              stgV_ring = Ring(2, [RANKS, NSTG, 260], BF16)
              hal_st = A.alloc([RANKS, 2 * 258], BF16); Bhal = Buf()
              kct_ring = Ring(2, [256], BF16)
              gl_ring = Ring(2, [4, 512], F32)
              ln_ring = Ring(2, [16], F32)
              vn_ring = Ring(2, [256], BF16)
              yb_ring = Ring(2, [256], BF16)
              sq_ring = Ring(2, [256], F32)

              stg = {}

              def p1_tile(t):
                  r = 1 if t < 2 else 0
                  xt, Bx = x_ring.next()
                  src, sdeps = xsrc(l, t)
                  dma("sp", xt, src, reads=sdeps, writes=[Bx])
                  sc, Bsc = sc_ring.next()
                  t1, Bt1 = t1_ring.next()
                  op("dve", TT(t1, xt, xt, ALU.mult), reads=[Bx], writes=[Bt1])
                  op("dve", RSUM(sc[:, 0:1], t1), reads=[Bt1], writes=[Bsc])
                  rstd_from_ss(sc[:, 0:1], Bsc, sc[:, 1:2], Bsc, sc[:, 2:3], Bsc, D)
                  op("dve", STT(t1, xt, sc[:, 2:3], G1[r], ALU.mult, ALU.mult), reads=[Bx, Bsc, BG1[r]], writes=[Bt1])
                  h, Bh = h_ring.next()
                  op("dve", TT(h, t1, SH1[r], ALU.add), reads=[Bt1, BSH1[r]], writes=[Bh])
                  yield
                  for kc in range(8):
                      op("pe", TR(psb[0][:, kc * 128:(kc + 1) * 128], h[:, kc * 128:(kc + 1) * 128], ident_b), reads=[Bh, Bidb], writes=[PS[0]])
                  hT, BhT = hT_ring.next()
                  op("act", ACT(hT, psb[0].rearrange("p (k t) -> p k t", k=8), AF.Copy), reads=[PS[0]], writes=[BhT])
                  yield
                  for j in range(4):
                      for kc in range(8):
                          op("pe", MM(ps[:, 1 + j, :], hT[:, kc, :], win[:, kc, j * 512:(j + 1) * 512], start=(kc == 0), stop=(kc == 7)),
                             reads=[BhT, Bwin], writes=[PS[1 + j]])
                  ck(('p1b', l))
                  p_sb, Bp = psb_ring.next()
                  op("act", ACT(p_sb[:, 0:512], ps[:, 1, :], AF.Copy), reads=[PS[1]], writes=[Bp])
                  op("act", ACT(p_sb[:, 512:1024], ps[:, 2, :], AF.Copy), reads=[PS[2]], writes=[Bp])
                  op("dve", CP(p_sb[:, 1024:1536], ps[:, 3, :]), reads=[PS[3]], writes=[Bp])
                  op("dve", CP(p_sb[:, 1536:2048], ps[:, 4, :]), reads=[PS[4]], writes=[Bp])
                  yield
                  ropet, Brope = rope_ring.next()
                  dma("sp", ropet, I["rope"][:, t * 96:(t + 1) * 96], writes=[Brope])
                  qk, Bqk = qk_ring.next()
                  rt, Brt = rt_ring.next()
                  for (c0, nh, dd, tb0, o0) in ((0, 10, 16, 0, 0), (1280, 16, 8, 64, 640)):
                      w = nh * 4 * dd
                      xv = p_sb[:, c0:c0 + w].rearrange("p (h r t d) -> p h r t d", h=nh, r=2, t=2, d=dd)
                      ov = qk[:, o0:o0 + w].rearrange("p (h r t d) -> p h r t d", h=nh, r=2, t=2, d=dd)
                      cosv = ropet[:, tb0:tb0 + 2 * dd].rearrange("p (r d) -> p r d", r=2).unsqueeze(1).broadcast_to([128, nh, 2, dd])
                      sinv = ropet[:, tb0 + 2 * dd:tb0 + 4 * dd].rearrange("p (r d) -> p r d", r=2).unsqueeze(1).broadcast_to([128, nh, 2, dd])
                      tv = [rt[:, i, 0:nh * 2 * dd].rearrange("p (h r d) -> p h r d", h=nh, r=2, d=dd) for i in range(4)]
                      x1, x2 = xv[:, :, :, 0, :], xv[:, :, :, 1, :]
                      op("dve", TT(tv[0], x1, cosv, ALU.mult), reads=[Bp, Brope], writes=[Brt])
                      op("dve", TT(tv[1], x2, sinv, ALU.mult), reads=[Bp, Brope], writes=[Brt])
                      op("dve", TT(tv[2], x1, sinv, ALU.mult), reads=[Bp, Brope], writes=[Brt])
                      op("dve", TT(tv[3], x2, cosv, ALU.mult), reads=[Bp, Brope], writes=[Brt])
                      op("dve", TT(ov[:, :, :, 0, :], tv[0], tv[1], ALU.subtract), reads=[Brt], writes=[Bqk])
                      op("dve", TT(ov[:, :, :, 1, :], tv[2], tv[3], ALU.add), reads=[Brt], writes=[Bqk])
                  ck(('p1c', l))
                  op("pool", CP(VA[:, t, :].rearrange("p (g d) -> p g d", g=2)[:, :, 0:64],
                                p_sb[:, 640:768].rearrange("p (g d) -> p g d", g=2)), reads=[Bp], writes=[BVA[t]])
                  op("pool", CP(vt[:, :, 0:64], p_sb[:, 1792:2048].rearrange("p (h d) -> p h d", h=4)), reads=[Bp], writes=[Bvt])
                  if t < 2:
                      dma("sp", vctx_d[t], vt.rearrange("p h d -> p (h d)"), reads=[Bvt], writes=[db("vctx")])
                  else:
                      j = (t - 2) % NSTG
                      if j == 0:
                          stg["K"], stg["BK"] = stgK_ring.next()
                          stg["V"], stg["BV"] = stgV_ring.next()
                      stgK, BstK, stgV, BstV = stg["K"], stg["BK"], stg["V"], stg["BV"]
                      for rr in range(RANKS):
                          op("pool", TS(stgV[:, rr, j, :], vt.rearrange("p h d -> p (h d)"), sel[:, rr:rr + 1], ALU.mult, 1.0, ALU.mult),
                             reads=[Bvt, Bsel], writes=[BstV])
                  yield
                  ck(('p1d', l))
                  for jj in range(4):
                      op("pe", TR(psb[5][:, jj * 128:(jj + 1) * 128], qk[:, jj * 128:(jj + 1) * 128], ident_b),
                         reads=[Bqk, Bidb], writes=[PS[5]])
                  op("pe", TR(psb[5][:, 512:640], qk[:, 512:640], ident_b), reads=[Bqk, Bidb], writes=[PS[5]])
                  for c in range(4):
                      op("pe", TR(psb[6][:, c * 128:(c + 1) * 128], qk[:, 640 + c * 128:640 + (c + 1) * 128], ident_b),
                         reads=[Bqk, Bidb], writes=[PS[6]])
                  ck(('p1d1', l))
                  qta_t, Bqta = qta_ring.next()
                  op("dve", CP(qta_t, psb[5][:, 0:512]), reads=[PS[5]], writes=[Bqta])
                  dma("sp", qta_d[t], qta_t, reads=[Bqta], writes=[db("qta", t)])
                  ck(('p1d2', l))
                  op("act", ACT(KTA[:, t * 128:(t + 1) * 128], psb[5][:, 512:640], AF.Copy), reads=[PS[5]], writes=[BKTA[t]])
                  ck(('p1d3', l))
                  qtc_t, Bqtc = qtc_ring.next()
                  op("dve", CP(qtc_t, psb[6][:, 0:256]), reads=[PS[6]], writes=[Bqtc])
                  dma("sp", qtc_d[t], qtc_t, reads=[Bqtc], writes=[db("qtc", t)])
                  ck(('p1d4', l))
                  if t < 2:
                      kct, Bkct = kct_ring.next()
                      op("act", ACT(kct, psb[6][:, 256:512], AF.Copy), reads=[PS[6]], writes=[Bkct])
                      dma("sp", kctx_d[:, :, t * 128:(t + 1) * 128], kct.rearrange("p (c k) -> p c k", c=2), reads=[Bkct], writes=[db("kctx")])
                  else:
                      j = (t - 2) % NSTG
                      for rr in range(RANKS):
                          op("act", ACT(stgK[:, rr, :, j * 128:(j + 1) * 128], psb[6][:, 256:512].rearrange("p (c k) -> p c k", c=2), AF.Copy,
                                        scale=sel[:, rr:rr + 1]), reads=[PS[6], Bsel], writes=[BstK])
                      if j == NSTG - 1 or t == NT - 1:
                          g0 = (t - 2) - j
                          ng = j + 1
                          for rr in range(RANKS):
                              dma("sp", xk_s[rr, :, :, g0 * 128:(g0 + ng) * 128], stgK[:, rr, :, 0:ng * 128], reads=[BstK], writes=[db("xk_s", rr)])
                              dma("sp", xv_s[rr, g0:g0 + ng].rearrange("j p f -> p j f"), stgV[:, rr, 0:ng, :], reads=[BstV], writes=[db("xv_s", rr)])
                      if t == 2 or t == NT - 1:
                          w = 0 if t == 2 else 1
                          for rr in range(RANKS):
                              op("pool", TS(hal_st[:, rr, w * 258:w * 258 + 128], KTA[:, t * 128:(t + 1) * 128], sel[:, rr:rr + 1], ALU.mult, 1.0, ALU.mult),
                                 reads=[BKTA[t], Bsel], writes=[Bhal])
                              op("pool", TS(hal_st[:, rr, w * 258 + 128:(w + 1) * 258], VA[:, t, :], sel[:, rr:rr + 1], ALU.mult, 1.0, ALU.mult),
                                 reads=[BVA[t], Bsel], writes=[Bhal])
                  yield
                  ck(('p1e', l))
                  gl, Bgl = gl_ring.next()
                  xg, x2, th, gg = gl[:, 0, :], gl[:, 1, :], gl[:, 2, :], gl[:, 3, :]
                  op("dve", CP(xg, p_sb[:, 768:1280]), reads=[Bp], writes=[Bgl])
                  op("dve", TT(x2, xg, xg, ALU.mult), reads=[Bgl], writes=[Bgl])
                  op("dve", TS(x2, x2, 0.044715, ALU.mult, 1.0, ALU.add), reads=[Bgl], writes=[Bgl])
                  op("dve", TT(x2, x2, xg, ALU.mult), reads=[Bgl], writes=[Bgl])
                  op("act", ACT(th, x2, AF.Tanh, scale=math.sqrt(2.0 / math.pi)), reads=[Bgl], writes=[Bgl])
                  op("dve", TS(th, th, 0.5, ALU.mult, 0.5, ALU.add), reads=[Bgl], writes=[Bgl])
                  op("dve", TT(gg, th, xg, ALU.mult), reads=[Bgl], writes=[Bgl])
                  ug = gg[:, 0:256]
                  vg = gg[:, 256:512].rearrange("p (g d) -> p g d", g=4)
                  ln, Bln = ln_ring.next()
                  sq, Bsq = sq_ring.next()
                  op("dve", RSUM(ln[:, 0:4], vg), reads=[Bgl], writes=[Bln])
                  op("dve", TT(sq, gg[:, 256:512], gg[:, 256:512], ALU.mult), reads=[Bgl], writes=[Bsq])
                  op("dve", RSUM(ln[:, 4:8], sq.rearrange("p (g d) -> p g d", g=4)), reads=[Bsq, Bln], writes=[Bln])
                  op("dve", TS(ln[:, 0:4], ln[:, 0:4], 1.0 / 64, ALU.mult), reads=[Bln], writes=[Bln])
                  op("dve", TT(ln[:, 8:12], ln[:, 0:4], ln[:, 0:4], ALU.mult), reads=[Bln], writes=[Bln])
                  op("dve", STT(ln[:, 4:8], ln[:, 4:8], 1.0 / 64, ln[:, 8:12], ALU.mult, ALU.subtract), reads=[Bln], writes=[Bln])
                  op("act", ACT(ln[:, 8:12], ln[:, 4:8], AF.Ln, bias=eps_t), reads=[Bln, Beps], writes=[Bln])
                  op("act", ACT(ln[:, 12:16], ln[:, 8:12], AF.Exp, scale=-0.5), reads=[Bln], writes=[Bln])
                  for g in range(4):
                      op("dve", TS(sq[:, g * 64:(g + 1) * 64], vg[:, g, :], ln[:, g:g + 1], ALU.subtract, ln[:, 12 + g:13 + g], ALU.mult),
                         reads=[Bgl, Bln, Bsq], writes=[Bsq])
                  yield
                  vn, Bvn = vn_ring.next()
                  op("dve", TT(vn, sq, gtokb, ALU.mult), reads=[Bsq, Bgtok], writes=[Bvn])
                  for g in range(4):
                      op("pe", MM(ps[:, 7, g * 64:(g + 1) * 64], wsT[:, g, :], vn[:, g * 64:(g + 1) * 64], start=True, stop=True, skip_group_check=True),
                         reads=[BwsT, Bvn], writes=[PS[7]])
                  ybt, Byb = yb_ring.next()
                  for g in range(4):
                      op("dve", STT(ybt[:, g * 64:(g + 1) * 64], ps[:, 7, g * 64:(g + 1) * 64], bsT[:, g:g + 1], ug[:, g * 64:(g + 1) * 64], ALU.add, ALU.mult),
                         reads=[PS[7], BbsT, Bgl], writes=[Byb])
                  dma("sp", yb_d[t], ybt, reads=[Byb], writes=[db("yb", t)])

              def run_interleaved(gens, depth):
                  active = []
                  it = iter(gens)
                  done = False
                  while True:
                      while len(active) < depth and not done:
                          try:
                              active.append(next(it))
                          except StopIteration:
                              done = True
                      if not active:
                          break
                      for g_ in list(active):
                          try:
                              next(g_)
                          except StopIteration:
                              active.remove(g_)

              run_interleaved((p1_tile(t) for t in range(NT)), 2)
              ck(('p1f', l))
              for rr in range(RANKS):
                  dma("sp", xh_s[rr], hal_st[:, rr, :], reads=[Bhal], writes=[db("xh_s", rr)])
              A.release()
              fw.barrier()
              if cfg.stop_after == ("p1", l):
                  break

              if RANKS > 1:
                  groups = [[b * RANKS + r for r in range(RANKS)] for b in range(cfg.NB)]
                  fw.cc(lambda e: e.collective_compute("AllReduce", ALU.add, replica_groups=groups,
                                                       ins=[xh_s.rearrange("r p f -> (r p) f")], outs=[xh_r.rearrange("r p f -> (r p) f")]),
                        reads=[db("xh_s", rr) for rr in range(RANKS)], writes=[db("xh_r")])
                  for rr in range(RANKS):
                      fw.cc(lambda e, rr=rr: e.collective_compute("AllReduce", ALU.add, replica_groups=groups,
                                                                  ins=[xk_s[rr].rearrange("p c k -> p (c k)")],
                                                                  outs=[xk_r[rr].rearrange("p c k -> p (c k)")]),
                            reads=[db("xk_s", rr)], writes=[db("xk_r", rr)])
                      fw.cc(lambda e, rr=rr: e.collective_compute("AllReduce", ALU.add, replica_groups=groups,
                                                                  ins=[xv_s[rr].rearrange("j p f -> (j p) f")],
                                                                  outs=[xv_r[rr].rearrange("j p f -> (j p) f")]),
                            reads=[db("xv_s", rr)], writes=[db("xv_r", rr)])
                  kvr = lambda name, rr: db(name + "_r", rr)
              else:
                  kvr = lambda name, rr: db(name + "_s", rr)
              Bxh = db("xh_r") if RANKS > 1 else db("xh_s", 0)

              A.mark()
              hal = A.alloc([RANKS, 2 * 258], BF16); Bhl = Buf()
              dma("sp", hal, xh_r.rearrange("r p f -> p r f"), reads=[Bxh], writes=[Bhl])
              hacc = A.alloc([2, 258], F32); Bhacc = Buf()
              for w, so in ((0, 2 * RANKS), (1, RANKS)):
                  for rr in range(RANKS):
                      src = hal[:, rr, w * 258:(w + 1) * 258]
                      if rr == 0:
                          op("dve", TS(hacc[:, w, :], src, sel[:, so + rr:so + rr + 1], ALU.mult), reads=[Bhl, Bsel], writes=[Bhacc])
                      else:
                          op("dve", STT(hacc[:, w, :], src, sel[:, so + rr:so + rr + 1], hacc[:, w, :], ALU.mult, ALU.add),
                             reads=[Bhl, Bsel, Bhacc], writes=[Bhacc])
              op("dve", CP(KTA[:, NT * 128:(NT + 1) * 128], hacc[:, 1, 0:128]), reads=[Bhacc], writes=[BKTA[NT]])
              op("dve", CP(VA[:, NT, :], hacc[:, 1, 128:258]), reads=[Bhacc], writes=[BVA[NT]])
              op("dve", CP(KTA[:, (NT + 1) * 128:(NT + 2) * 128], hacc[:, 0, 0:128]), reads=[Bhacc], writes=[BKTA[NT + 1]])
              op("dve", CP(VA[:, NT + 1, :], hacc[:, 0, 128:258]), reads=[Bhacc], writes=[BVA[NT + 1]])

              wout = A.alloc([8, D], BF16); Bwout = Buf()
              for kc in range(8):
                  dma("pool", wout[:, kc, :], I["w_out"][l, kc * 128:(kc + 1) * 128, :], writes=[Bwout])
              wr = A.alloc([8, 20], F32); Bwr = Buf()
              dma("sp", wr, I["w_r"][l].rearrange("(k p) n -> p k n", p=128), writes=[Bwr])
              brb = A.alloc([20], F32); Bbr = Buf()
              dma("sp", brb, I["b_r"][l:l + 1, :].broadcast_to([128, 20]), writes=[Bbr])
              GT1 = A.alloc([D], F32); BGT1 = Buf()
              G2 = A.alloc([D], F32); BG2 = Buf()
              SH2 = A.alloc([D], F32); BSH2 = Buf()
              gtmp2 = A.alloc([D], F32); Bgtmp2 = Buf()

              def load_p2_mods(r):
                  load_mod(l, r, 2, GT1, BGT1)
                  load_mod(l, r, 4, G2, BG2)
                  load_gain(I["g_ffn"][l:l + 1, :], gtmp2, Bgtmp2)
                  op("dve", STT(G2, G2, 1.0, gtmp2, ALU.add, ALU.mult), reads=[BG2, Bgtmp2], writes=[BG2])
                  load_mod(l, r, 3, SH2, BSH2)

              kc_ring = Ring(3, [2, KCH * 128], BF16)
              vc_ring = Ring(3, [KCH, 260], BF16)
              qb_ring = Ring(2, [2, 256], BF16)
              NE = 3
              E_aps = [[A.alloc([1024], BF16) for _ in range(NE)] for _ in range(2)]
              E_buf = [[Buf() for _ in range(NE)] for _ in range(2)]
              usb = A.alloc([2, 512], F32); Busb = Buf()
              accS = [A.alloc([1024], F32) for _ in range(2)]; BaccS = [Buf(), Buf()]
              epair = [A.alloc([1024], BF16) for _ in range(2)]; Bepair = [Buf(), Buf()]
              uasb = A.alloc([2, 512], F32); Buasb = Buf()
              qa_ring = Ring(2, [512], BF16)
              eab_ring = Ring(2, [5, 512], BF16)
              fsc_ring = Ring(4, [48], F32)
              osb_ring = Ring(2, [256], F32)
              ojk = A.alloc([256], F32); Bojk = Buf()
              ymix_ring = Ring(2, [D], BF16)
              ymT_ring = Ring(2, [8, 128], BF16)
              x2_ring = Ring(2, [D], F32)
              yt_ring = Ring(2, [D], F32)
              xm_ring = Ring(2, [D], F32)
              h2_ring = Ring(2, [D], F32)
              h2Tf_ring = Ring(2, [8, 128], F32)
              h2Tb_ring = Ring(2, [8, 128], BF16)
              rsc_ring = Ring(2, [96], F32)
              junk2 = A.alloc([D], BF16); Bjunk2 = Buf()

              qblocks = []
              if not last:
                  qblocks.append(([0, 1], 1, "ctx"))
              for i in range(0, NLAT, 2):
                  qblocks.append(([2 + i, 3 + i] if i + 1 < NLAT else [2 + i], 0, "lat"))
              cur_r = None
              pending = []
              pend_ptr = [0]

              def step_pending():
                  if not pending:
                      return
                  k = pend_ptr[0] % len(pending)
                  try:
                      next(pending[k])
                      pend_ptr[0] += 1
                  except StopIteration:
                      pending.pop(k)

              def flush_pending():
                  while pending:
                      step_pending()

              for (tiles, r, kind) in qblocks:
                  if r != cur_r:
                      load_p2_mods(r)
                      cur_r = r
                  nq = len(tiles) * 128
                  chunks = [(kctx_d, vctx_d.rearrange("j p f -> p j f"), 2, [db("kctx"), db("vctx")])]
                  if kind == "lat":
                      for rr in range(RANKS):
                          for c0 in range(0, NLAT, KCH):
                              chunks.append((xk_r[rr, :, :, c0 * 128:(c0 + KCH) * 128], xv_r[rr, c0:c0 + KCH].rearrange("j p f -> p j f"), KCH,
                                             [kvr("xk", rr), kvr("xv", rr)]))
                  Qb, BQb = qb_ring.next()
                  for j, t in enumerate(tiles):
                      dma("sp", Qb[:, :, j * 128:(j + 1) * 128], qtc_d[t].rearrange("p (c k) -> p c k", c=2), reads=[db("qtc", t)], writes=[BQb])
                  ktl = []
                  loaded = {}

                  def load_chunk(ci):
                      ksrc, vsrc, n, deps = chunks[ci]
                      kc_t, Bkc = kc_ring.next()
                      vc_t, Bvc = vc_ring.next()
                      dma("sp", kc_t[:, :, 0:n * 128], ksrc, reads=deps, writes=[Bkc])
                      dma("sp", vc_t[:, 0:n, :], vsrc, reads=deps, writes=[Bvc])
                      loaded[ci] = (kc_t, Bkc, vc_t, Bvc)

                  for ci, ch in enumerate(chunks):
                      for kk in range(ch[2]):
                          ktl.append((ci, kk))
                  nk = len(ktl)
                  load_chunk(0)
                  if len(chunks) > 1:
                      load_chunk(1)

                  def qk(i, X):
                      ci, kk = ktl[i]
                      kc_t, Bkc, _, _ = loaded[ci]
                      for c in range(2):
                          for gg in range(2):
                              g = 2 * X + gg
                              op("pe", MM(ps[:, 2 * X + gg, c * 256:c * 256 + nq], kc_t[32 * g:32 * g + 32, c, kk * 128:(kk + 1) * 128],
                                          Qb[32 * g:32 * g + 32, c, 0:nq], tile_position=(32 * g, 0)),
                                 reads=[Bkc, BQb], writes=[PS[2 * X], PS[2 * X + 1]])

                  def ex(i, X):
                      Et, Eb = E_aps[X][i % NE], E_buf[X][i % NE]
                      op("act", ACT(Et.rearrange("p (c g q) -> p g c q", c=2, g=2)[:, :, :, 0:nq],
                                    ps[:, 2 * X:2 * X + 2, :].rearrange("p g (c q) -> p g c q", c=2)[:, :, :, 0:nq], AF.Exp, scale=SCALE_C),
                         reads=[PS[2 * X], PS[2 * X + 1]], writes=[Eb])

                  def pv(i, X):
                      ci, kk = ktl[i]
                      _, _, vc_t, Bvc = loaded[ci]
                      Et, Eb = E_aps[X][i % NE], E_buf[X][i % NE]
                      for c in range(2):
                          hh = 2 * c + X
                          rhs = Et[:, c * 512:(c + 1) * 512].rearrange("p (g q) -> p g q", g=2)[:, :, 0:nq]
                          o = ps[64 * c:64 * c + 64, 4 + X, :].rearrange("p (g q) -> p g q", g=2)[:, :, 0:nq]
                          op("pe", MM(o, vc_t[:, kk, hh * 65:hh * 65 + 64], rhs, start=(i == 0), stop=(i == nk - 1), tile_position=(0, 64 * c),
                                      skip_group_check=True), reads=[Bvc, Eb], writes=[PS[4 + X]])
                      if i % 2 == 1:
                          Ep = E_aps[X][(i - 1) % NE]
                          Epb = E_buf[X][(i - 1) % NE]
                          if i == 1:
                              op("dve", TT(accS[X], Ep, Et, ALU.add), reads=[Epb, Eb], writes=[BaccS[X]])
                          else:
                              op("dve", TT(epair[X], Ep, Et, ALU.add), reads=[Epb, Eb], writes=[Bepair[X]])
                              op("dve", TT(accS[X], accS[X], epair[X], ALU.add), reads=[Bepair[X], BaccS[X]], writes=[BaccS[X]])
                      elif i == nk - 1:
                          if i == 0:
                              op("dve", CP(accS[X], Et), reads=[Eb], writes=[BaccS[X]])
                          else:
                              op("dve", TT(accS[X], accS[X], Et, ALU.add), reads=[Eb, BaccS[X]], writes=[BaccS[X]])

                  adv = max(1, nk // 60)
                  qk(0, 0)
                  qk(0, 1)
                  for i in range(nk):
                      ci, kk = ktl[i]
                      if kk == 0 and ci + 2 < len(chunks) and (ci + 2) not in loaded:
                          load_chunk(ci + 2)
                      for X in range(2):
                          ex(i, X)
                          if i + 1 < nk:
                              qk(i + 1, X)
                          pv(i, X)
                      if pending and i % adv == adv // 2:
                          step_pending()
                  flush_pending()

                  op("act", ACT(usb[:, 0:2, :], ps[:, 4:6, :], AF.Copy), reads=[PS[4], PS[5]], writes=[Busb])
                  for j, t in enumerate(tiles):
                      for X in range(2):
                          for c in range(2):
                              for gg in range(2):
                                  hm = (2 * c + X) * 2 + gg
                                  col = j * 8 + hm
                                  op("pe", MM(ps[:, 6, col:col + 1], accS[X][:, (c * 2 + gg) * 256 + j * 128:(c * 2 + gg) * 256 + (j + 1) * 128], ones1,
                                              start=True, stop=True, skip_group_check=True), reads=[BaccS[X], Bones], writes=[PS[6]])
                  for j, t in enumerate(tiles):
                      for X in range(2):
                          for m in range(2):
                              op("pe", TR(ps[:, j, (X * 2 + m) * 128:(X * 2 + m + 1) * 128], usb[:, X, m * 256 + j * 128:m * 256 + (j + 1) * 128], ident_f),
                                 reads=[Busb, Bidf], writes=[PS[j]])
                  ymix_l = []
                  for j, t in enumerate(tiles):
                      PU = [PS[j]]
                      fs, Bfs = fsc_ring.next()
                      rcp = fs[:, 0:8]
                      op("dve", lambda e, rcp=rcp, j=j: e.reciprocal(out=rcp, in_=ps[:, 6, j * 8:(j + 1) * 8]), reads=[PS[6]], writes=[Bfs])
                      nrl = fs[:, 8:12]
                      op("dve", TS(nrl, rcp.rearrange("p (h m) -> p h m", m=2)[:, :, 1], neglam, ALU.mult), reads=[Bfs, Blsc], writes=[Bfs])
                      osb, Bosb = osb_ring.next()
                      for hh in range(4):
                          X_, c_ = hh % 2, hh // 2
                          u1 = ps[:, j, ((X_ * 2 + 0) * 2 + c_) * 64:((X_ * 2 + 0) * 2 + c_) * 64 + 64]
                          u2 = ps[:, j, ((X_ * 2 + 1) * 2 + c_) * 64:((X_ * 2 + 1) * 2 + c_) * 64 + 64]
                          op("dve", TS(osb[:, hh * 64:(hh + 1) * 64], u1, rcp[:, 2 * hh:2 * hh + 1], ALU.mult), reads=PU + [Bfs], writes=[Bosb])
                          op("dve", STT(osb[:, hh * 64:(hh + 1) * 64], u2, nrl[:, hh:hh + 1], osb[:, hh * 64:(hh + 1) * 64], ALU.mult, ALU.add),
                             reads=PU + [Bfs, Bosb], writes=[Bosb])
                      op("dve", TT(ojk, osb, osb, ALU.mult), reads=[Bosb], writes=[Bojk])
                      op("dve", RSUM(fs[:, 12:16], ojk.rearrange("p (h d) -> p h d", h=4)), reads=[Bojk, Bfs], writes=[Bfs])
                      op("act", ACT(fs[:, 16:20], fs[:, 12:16], AF.Ln, scale=1.0 / 64, bias=eps_t), reads=[Bfs, Beps], writes=[Bfs])
                      op("act", ACT(fs[:, 20:24], fs[:, 16:20], AF.Exp, scale=-0.5), reads=[Bfs], writes=[Bfs])
                      ymix, Bym = ymix_ring.next()
                      for hh in range(4):
                          op("dve", STT(ymix[:, 768 + hh * 64:768 + (hh + 1) * 64], osb[:, hh * 64:(hh + 1) * 64], fs[:, 20 + hh:21 + hh], gsub_s,
                                        ALU.mult, ALU.mult), reads=[Bosb, Bfs, Bgsub], writes=[Bym])
                      dma("sp", ymix[:, 512:768], yb_d[t], reads=[db("yb", t)], writes=[Bym])
                      ymix_l.append((ymix, Bym))
                  def tail_tile(j, t):
                      ymix, Bym = ymix_l[j]
                      fs, Bfs = fsc_ring.next()
                      qa, Bqa = qa_ring.next()
                      dma("sp", qa, qta_d[t], reads=[db("qta", t)], writes=[Bqa])
                      if kind == "ctx":
                          slots = [(0, None), (1, None)]
                      else:
                          pslot = (t - 1, 0) if t > 2 else (NT, 2)
                          nslot = (t + 1, 1) if t < NT - 1 else (NT + 1, 3)
                          slots = [(0, None), (1, None), pslot, (t, None), nslot]
                      ns = len(slots)
                      for g in range(2):
                          for k_i, (slot, mk) in enumerate(slots):
                              op("pe", MM(ps[:, k_i, :], KTA[64 * g:64 * g + 64, slot * 128:(slot + 1) * 128], qa[64 * g:64 * g + 64, :],
                                          start=True, stop=(mk is None), tile_position=(64 * g, 0)), reads=[BKTA[slot], Bqa], writes=[PS[k_i]])
                              if mk is not None:
                                  op("pe", MM(ps[:, k_i, :], ident_b, mneg[:, mk, :], start=False, stop=True), reads=[Bidb, Bmneg], writes=[PS[k_i]])
                          eab, Beab = eab_ring.next()
                          op("act", ACT(eab[:, 0:ns, :], ps[:, 0:ns, :], AF.Exp, scale=SCALE_A), reads=PS[0:ns], writes=[Beab])
                          for k_i, (slot, mk) in enumerate(slots):
                              op("pe", MM(ps[0:65, 6 + g, :], VA[:, slot, g * 65:(g + 1) * 65], eab[:, k_i, :], start=(k_i == 0), stop=(k_i == ns - 1)),
                                 reads=[BVA[slot], Beab], writes=[PS[6 + g]])
                      op("act", ACT(uasb[0:65, :, :], ps[0:65, 6:8, :], AF.Copy), reads=[PS[6], PS[7]], writes=[Buasb])
                      for g in range(2):
                          for jh in range(4):
                              op("pe", TR(ps[:, 4 + g, jh * 65:(jh + 1) * 65], uasb[0:65, g, jh * 128:(jh + 1) * 128], ident_f[0:65, 0:65]),
                                 reads=[Buasb, Bidf], writes=[PS[4 + g]])
                      UAT = ps[:, 4:6, 0:260].rearrange("p g (j d) -> p g j d", j=4)
                      den = fs[:, 24:32]
                      op("dve", TT(den.rearrange("p (g j) -> p g j", g=2), UAT[:, :, :, 64], esink.rearrange("p (g j) -> p g j", g=2), ALU.add),
                         reads=[PS[4], PS[5], Besink, Bfs], writes=[Bfs])
                      op("dve", lambda e, den=den: e.reciprocal(out=den, in_=den), reads=[Bfs], writes=[Bfs])
                      for g in range(2):
                          op("dve", TT(ymix[:, g * 256:(g + 1) * 256].rearrange("p (j d) -> p j d", j=4), UAT[:, g, :, 0:64],
                                       den[:, g * 4:(g + 1) * 4].unsqueeze(2).broadcast_to([128, 4, 64]), ALU.mult),
                             reads=[PS[4 + g], Bfs], writes=[Bym])
                      yield

                  def tail2(j, t, ymix, Bym):
                      pb = 6 + j
                      P_ = PS[pb]
                      for kc in range(8):
                          op("pe", TR(psb[pb][:, kc * 128:(kc + 1) * 128], ymix[:, kc * 128:(kc + 1) * 128], ident_b), reads=[Bym, Bidb], writes=[P_])
                      ymT, BymT = ymT_ring.next()
                      x2t, Bx2 = x2_ring.next()
                      src, sdeps = xsrc(l, t)
                      dma("sp", x2t, src, reads=sdeps, writes=[Bx2])
                      yield
                      op("dve", CP(ymT, psb[pb].rearrange("p (k t) -> p k t", k=8)), reads=[P_], writes=[BymT])
                      yield
                      ytm, Byt = yt_ring.next()
                      for hf in range(2):
                          for kc in range(8):
                              op("pe", MM(ps[:, pb, :], ymT[:, kc, :], wout[:, kc, hf * 512:(hf + 1) * 512], start=(kc == 0), stop=(kc == 7)),
                                 reads=[BymT, Bwout], writes=[P_])
                          yield
                          op("dve", TT(ytm[:, hf * 512:(hf + 1) * 512], ps[:, pb, :], GT1[:, hf * 512:(hf + 1) * 512], ALU.mult),
                             reads=[P_, BGT1], writes=[Byt])
                          yield
                      xm, Bxm = xm_ring.next()
                      op("dve", TT(xm, ytm, x2t, ALU.add), reads=[Byt, Bx2], writes=[Bxm])
                      dma("sp", xmid[t], xm, reads=[Bxm], writes=[db("xmid", t)])
                      rs, Brs = rsc_ring.next()
                      op("dve", TT(ytm, xm, xm, ALU.mult), reads=[Bxm], writes=[Byt])
                      op("dve", RSUM(rs[:, 0:1], ytm), reads=[Byt], writes=[Brs])
                      yield
                      rstd_from_ss(rs[:, 0:1], Brs, rs[:, 1:2], Brs, rs[:, 2:3], Brs, D)
                      yield
                      op("dve", STT(ytm, xm, rs[:, 2:3], G2, ALU.mult, ALU.mult), reads=[Bxm, Brs, BG2], writes=[Byt])
                      h2, Bh2 = h2_ring.next()
                      op("dve", TT(h2, ytm, SH2, ALU.add), reads=[Byt, BSH2], writes=[Bh2])
                      yield
                      h2Tf, Bh2Tf = h2Tf_ring.next()
                      h2Tb, Bh2Tb = h2Tb_ring.next()
                      for hb in range(2):
                          for kq in range(4):
                              kc = 4 * hb + kq
                              op("pe", TR(ps[:, pb, kq * 128:(kq + 1) * 128], h2[:, kc * 128:(kc + 1) * 128], ident_f), reads=[Bh2, Bidf], writes=[P_])
                          yield
                          h2ps = ps[:, pb, :].rearrange("p (k t) -> p k t", k=4)
                          op("dve", CP(h2Tf[:, 4 * hb:4 * hb + 4, :], h2ps), reads=[P_], writes=[Bh2Tf])
                          op("dve", CP(h2Tb[:, 4 * hb:4 * hb + 4, :], h2ps), reads=[P_], writes=[Bh2Tb])
                          yield
                      dma("sp", h2t_d[:, :, t * 128:(t + 1) * 128], h2Tb, reads=[Bh2Tb], writes=[db("h2t", t)])
                      for kc in range(8):
                          op("pe", MM(ps[:, pb, 0:20], h2Tf[:, kc, :], wr[:, kc, :], start=(kc == 0), stop=(kc == 7)), reads=[Bh2Tf, Bwr], writes=[P_])
                      yield
                      lg = rs[:, 4:24]
                      op("dve", TT(lg, ps[:, pb, 0:20], brb, ALU.add), reads=[P_, Bbr, Brs], writes=[Brs])
                      lgG = lg[:, 0:4]
                      le = lg[:, 4:20].rearrange("p (g j) -> p g j", g=4)
                      mx = rs[:, 24:25]
                      op("dve", RMAX(mx, lgG), reads=[Brs], writes=[Brs])
                      shf = rs[:, 25:29]
                      op("dve", TS(shf, lgG, mx, ALU.subtract), reads=[Brs], writes=[Brs])
                      yield
                      exg = rs[:, 29:33]
                      sume = rs[:, 33:34]
                      op("act", ACT(exg, shf, AF.Exp), reads=[Brs], writes=[Brs])
                      yield
                      op("dve", RSUM(sume, exg), reads=[Brs], writes=[Brs])
                      ptop = rs[:, 34:35]
                      op("dve", lambda e, ptop=ptop, sume=sume: e.reciprocal(out=ptop, in_=sume), reads=[Brs], writes=[Brs])
                      oh = rs[:, 35:39]
                      op("dve", TS(oh, lgG, mx, ALU.is_equal), reads=[Brs], writes=[Brs])
                      tmp16 = rs[:, 40:56]
                      op("dve", TT(tmp16.rearrange("p (g j) -> p g j", g=4), le, oh.unsqueeze(2).broadcast_to([128, 4, 4]), ALU.mult), reads=[Brs], writes=[Brs])
                      yield
                      leg = rs[:, 56:60]
                      op("dve", RSUM(leg, tmp16.rearrange("p (g j) -> p j g", g=4)), reads=[Brs], writes=[Brs])
                      m1 = rs[:, 60:61]
                      op("dve", RMAX(m1, leg), reads=[Brs], writes=[Brs])
                      mk1 = rs[:, 61:65]
                      op("dve", TS(mk1, leg, m1, ALU.is_equal), reads=[Brs], writes=[Brs])
                      le2 = rs[:, 65:69]
                      op("dve", STT(le2, mk1, -1e30, leg, ALU.mult, ALU.add), reads=[Brs], writes=[Brs])
                      yield
                      m2 = rs[:, 69:70]
                      op("dve", RMAX(m2, le2), reads=[Brs], writes=[Brs])
                      mk2 = rs[:, 70:74]
                      op("dve", TS(mk2, le2, m2, ALU.is_equal), reads=[Brs], writes=[Brs])
                      d21 = rs[:, 74:75]
                      op("dve", TT(d21, m2, m1, ALU.subtract), reads=[Brs], writes=[Brs])
                      yield
                      e21 = rs[:, 75:76]
                      op("act", ACT(e21, d21, AF.Exp), reads=[Brs], writes=[Brs])
                      yield
                      w1 = rs[:, 76:77]
                      op("dve", TS(w1, e21, 1.0, ALU.add), reads=[Brs], writes=[Brs])
                      op("dve", lambda e, w1=w1: e.reciprocal(out=w1, in_=w1), reads=[Brs], writes=[Brs])
                      op("dve", TT(w1, w1, ptop, ALU.mult), reads=[Brs], writes=[Brs])
                      w2 = rs[:, 77:78]
                      op("dve", TT(w2, w1, e21, ALU.mult), reads=[Brs], writes=[Brs])
                      yield
                      gj = rs[:, 78:82]
                      op("dve", TS(gj, mk1, w1, ALU.mult), reads=[Brs], writes=[Brs])
                      op("dve", STT(gj, mk2, w2, gj, ALU.mult, ALU.add), reads=[Brs], writes=[Brs])
                      op("dve", TT(gates[:, t, :].rearrange("p (g j) -> p g j", g=4), oh.unsqueeze(2).broadcast_to([128, 4, 4]),
                                   gj.unsqueeze(1).broadcast_to([128, 4, 4]), ALU.mult), reads=[Brs], writes=[Bgate[t]])

                  run_interleaved((tail_tile(j, t) for j, t in enumerate(tiles)), 2)
                  if kind == "ctx":
                      run_interleaved((tail2(j, t, *ymix_l[j]) for j, t in enumerate(tiles)), 2)
                  else:
                      pending.extend([tail2(j, t, *ymix_l[j]) for j, t in enumerate(tiles)])
              flush_pending()
              A.release()
              A.release()
              fw.barrier()
              if cfg.stop_after == ("p2", l):
                  break

              A.mark()
              p3tiles = list(range(NT)) if not last else list(range(2, NT))
              if len(p3tiles) <= 9:
                  sblocks = [p3tiles]
              else:
                  hsz = (len(p3tiles) + 1) // 2
                  sblocks = [p3tiles[:hsz], p3tiles[hsz:]]
              SBT = max(len(x) for x in sblocks)
              GT2 = [A.alloc([D], F32) for _ in range(2)]; BGT2 = [Buf(), Buf()]
              for r in ((0,) if last else (0, 1)):
                  load_mod(l, r, 5, GT2[r], BGT2[r])
              if last:
                  gfin = A.alloc([D], F32); Bgfin = Buf()
                  load_gain(I["g_final"], gfin, Bgfin)
              h2sb = A.alloc([8, SBT * 128], BF16); Bh2sb = Buf()
              yacc = A.alloc([SBT, D], F32); Byacc = [Buf() for _ in range(SBT)]
              wg_ring = Ring(2, [8, DEXP], BF16)
              wu_ring = Ring(2, [8, DEXP], BF16)
              wd_ring = Ring(2, [4, D], BF16)
              sg_ring = Ring(2, [512], F32)
              he_ring = Ring(2, [4, 512], BF16)
              xm3_ring = Ring(2, [D], F32)
              fo_ring = Ring(2, [D], F32)
              f3_ring = Ring(2, [8], F32)
              junk3 = A.alloc([D], BF16); Bjunk3 = Buf()
              ycnt = 0
              for tl in sblocks:
                  ntok = len(tl) * 128
                  dma("sp", h2sb[:, :, 0:ntok], h2t_d[:, :, tl[0] * 128:(tl[-1] + 1) * 128], reads=[db("h2t", t) for t in tl], writes=[Bh2sb])
                  for e_ in range(NEXP):
                      wg, Bwg = wg_ring.next()
                      wu, Bwu = wu_ring.next()
                      wd, Bwd = wd_ring.next()
                      dma("pool", wg, I["w_gate"][l, e_].rearrange("(k p) n -> p k n", p=128), writes=[Bwg])
                      dma("pool", wu, I["w_up"][l, e_].rearrange("(k p) n -> p k n", p=128), writes=[Bwu])
                      dma("pool", wd, I["w_down"][l, e_].rearrange("(k p) n -> p k n", p=128), writes=[Bwd])
                      for b0 in range(0, ntok, 512):
                          n = min(512, ntok - b0)
                          he, Bhe = he_ring.next()
                          for dc in range(4):
                              gb, ub = dc % 2, 2 + dc % 2
                              for kc in range(8):
                                  op("pe", MM(ps[:, gb, 0:n], wg[:, kc, dc * 128:(dc + 1) * 128], h2sb[:, kc, b0:b0 + n], start=(kc == 0), stop=(kc == 7)),
                                     reads=[Bwg, Bh2sb], writes=[PS[gb]])
                              for kc in range(8):
                                  op("pe", MM(ps[:, ub, 0:n], wu[:, kc, dc * 128:(dc + 1) * 128], h2sb[:, kc, b0:b0 + n], start=(kc == 0), stop=(kc == 7)),
                                     reads=[Bwu, Bh2sb], writes=[PS[ub]])
                              sg, Bsg = sg_ring.next()
                              op("act", ACT(sg[:, 0:n], ps[:, gb, 0:n], AF.Silu), reads=[PS[gb]], writes=[Bsg])
                              op("dve", TT(he[:, dc, 0:n], sg[:, 0:n], ps[:, ub, 0:n], ALU.mult), reads=[Bsg, PS[ub]], writes=[Bhe])
                          for j in range(n // 128):
                              ti = b0 // 128 + j
                              t = tl[ti]
                              for hf in range(2):
                                  yb_ = 4 + (ycnt % 4)
                                  ycnt += 1
                                  for dc in range(4):
                                      op("pe", MM(ps[:, yb_, :], he[:, dc, j * 128:(j + 1) * 128], wd[:, dc, hf * 512:(hf + 1) * 512], start=(dc == 0), stop=(dc == 3)),
                                         reads=[Bhe, Bwd], writes=[PS[yb_]])
                                  ya_ = yacc[:, ti, hf * 512:(hf + 1) * 512]
                                  if e_ == 0:
                                      op("dve", TS(ya_, ps[:, yb_, :], gates[:, t, e_:e_ + 1], ALU.mult), reads=[PS[yb_], Bgate[t]], writes=[Byacc[ti]])
                                  else:
                                      op("dve", STT(ya_, ps[:, yb_, :], gates[:, t, e_:e_ + 1], ya_, ALU.mult, ALU.add),
                                         reads=[PS[yb_], Bgate[t], Byacc[ti]], writes=[Byacc[ti]])
                  for ti, t in enumerate(tl):
                      r = 1 if t < 2 else 0
                      xm3, Bxm3 = xm3_ring.next()
                      dma("sp", xm3, xmid[t], reads=[db("xmid", t)], writes=[Bxm3])
                      op("dve", TT(yacc[:, ti, :], yacc[:, ti, :], GT2[r], ALU.mult), reads=[Byacc[ti], BGT2[r]], writes=[Byacc[ti]])
                      xo, Bxo = xm3, Bxm3
                      op("dve", TT(xo, yacc[:, ti, :], xm3, ALU.add), reads=[Byacc[ti], Bxm3], writes=[Bxo])
                      if not last:
                          dma("sp", xbuf[t], xo, reads=[Bxo], writes=[db("xbuf", t)])
                      else:
                          f3, Bf3 = f3_ring.next()
                          fo, Bfo = fo_ring.next()
                          op("dve", TT(fo, xo, xo, ALU.mult), reads=[Bxo], writes=[Bfo])
                          op("dve", RSUM(f3[:, 0:1], fo), reads=[Bfo], writes=[Bf3])
                          rstd_from_ss(f3[:, 0:1], Bf3, f3[:, 1:2], Bf3, f3[:, 2:3], Bf3, D)
                          op("dve", STT(fo, xo, f3[:, 2:3], gfin, ALU.mult, ALU.mult), reads=[Bxo, Bf3, Bgfin], writes=[Bfo])
                          dma("sp", out_d[(t - 2) * 128:(t - 1) * 128, :], fo, reads=[Bfo], writes=[db("out", t)])
              A.release()
              A.release()
              fw.barrier()

        except StopBuild:
            pass
        fw.barrier()
        blk = st.enter_context(nc.Block())
        stats = fw.finish(blk)
    return nc, stats


def _rope_table(cfg, r):
    NT, NLAT = cfg.NT, cfg.NLAT
    tab = np.zeros((128, NT, 96), np.float32)
    tab[:, :, 0:32] = 1.0
    tab[:, :, 64:80] = 1.0
    invA = (np.float32(10000.0) ** (-np.arange(16, dtype=np.float32) / np.float32(16))).astype(np.float32)
    invC = (np.float32(10000.0) ** (-np.arange(8, dtype=np.float32) / np.float32(8))).astype(np.float32)
    for t in range(2, NT):
        tok = r * NLAT * 128 + (t - 2) * 128 + np.arange(128)
        rows = (tok // GRID_W).astype(np.float32)
        cols = (tok % GRID_W).astype(np.float32)
        for pi, pos in enumerate((rows, cols)):
            angA = (pos[:, None] * invA[None, :]).astype(np.float32)
            angC = (pos[:, None] * invC[None, :]).astype(np.float32)
            tab[:, t, pi * 16:(pi + 1) * 16] = np.cos(angA)
            tab[:, t, 32 + pi * 16:32 + (pi + 1) * 16] = np.sin(angA)
            tab[:, t, 64 + pi * 8:64 + (pi + 1) * 8] = np.cos(angC)
            tab[:, t, 80 + pi * 8:80 + (pi + 1) * 8] = np.sin(angC)
    return tab.reshape(128, NT * 96)


_WIN_PERM = np.arange(DIN)
for _g in range(2):
    for _j in range(4):
        _WIN_PERM[(_j * 2 + _g) * 64:(_j * 2 + _g + 1) * 64] = np.arange((_g * 4 + _j) * 64, (_g * 4 + _j + 1) * 64)


def make_in_maps(cfg, inputs):
    NLAT, RANKS, NB = cfg.NLAT, cfg.RANKS, cfg.NB
    f = lambda a: np.ascontiguousarray(np.asarray(a, dtype=np.float32))
    x, c, ctx, c_ctx = f(inputs["x"]), f(inputs["c"]), f(inputs["ctx"]), f(inputs["c_ctx"])
    dp = cfg.DEPTH
    shared = {
        "w_ada": f(inputs["w_ada"]),
        "b_ada2": f(np.repeat(f(inputs["b_ada"])[:, None, :], 2, axis=1)),
        "g_mix": f(inputs["g_mix"]), "w_in": f(f(inputs["w_in"])[:, :, _WIN_PERM]), "sink": f(inputs["sink"]),
        "ws_tok": f(inputs["ws_tok"]), "bs_tok": f(inputs["bs_tok"]), "g_tok": f(inputs["g_tok"]).reshape(dp, 256),
        "lam4": f(np.stack([f(inputs["lam_q1"]), f(inputs["lam_k1"]), f(inputs["lam_q2"]), f(inputs["lam_k2"])], axis=1)).reshape(dp, 128),
        "g_sub": f(inputs["g_sub"]), "w_out": f(inputs["w_out"]), "g_ffn": f(inputs["g_ffn"]),
        "w_r": f(np.concatenate([f(inputs["w_rg"]), f(inputs["w_re"])], axis=-1)),
        "b_r": f(np.concatenate([f(inputs["b_rg"]), f(inputs["b_re"])], axis=-1)),
        "w_gate": f(inputs["w_gate"]), "w_up": f(inputs["w_up"]), "w_down": f(inputs["w_down"]),
        "g_final": f(inputs["g_final"]).reshape(1, D),
        "ident": np.eye(128, dtype=np.float32),
    }
    kk = np.arange(128)[:, None]
    qq = np.arange(128)[None, :]
    mL = (qq <= kk).astype(np.float32)
    mU = (kk <= qq).astype(np.float32)
    maps = []
    for b in range(NB):
        for r in range(RANKS):
            m = dict(shared)
            m["xl"] = f(x[b, r * NLAT * 128:(r + 1) * NLAT * 128, :])
            m["ctx"] = f(ctx[b])
            m["c2"] = f(np.stack([c[b], c_ctx], axis=0))
            mk = np.stack([mL, mU, mL if r > 0 else np.zeros_like(mL), mU if r < RANKS - 1 else np.zeros_like(mU)], axis=1)
            m["masks"] = f(mk.reshape(128, 4 * 128))
            m["rope"] = _rope_table(cfg, r)
            s = np.zeros((3, RANKS), np.float32)
            s[0, r] = 1.0
            if r > 0:
                s[1, r - 1] = 1.0
            if r < RANKS - 1:
                s[2, r + 1] = 1.0
            m["sel"] = f(np.broadcast_to(s.reshape(1, 3 * RANKS), (128, 3 * RANKS)))
            maps.append(m)
    return maps


_CACHE = {}


def kernel(**inputs):
    x = np.asarray(inputs["x"])
    NB, S, _ = x.shape
    RANKS = 8 // NB
    NLAT = S // (RANKS * 128)
    cfg = Cfg(NLAT=NLAT, RANKS=RANKS, NB=NB, DEPTH=int(np.asarray(inputs["w_in"]).shape[0]))
    key = (NLAT, RANKS, NB, cfg.DEPTH)
    if key not in _CACHE:
        _CACHE[key] = build_program(cfg)[0]
    nc = _CACHE[key]
    maps = make_in_maps(cfg, inputs)
    res = run_bass_kernel_spmd(nc, maps, core_ids=list(range(cfg.NCORES)))
    out = np.zeros((NB, S, D), np.float32)
    for b in range(NB):
        for r in range(RANKS):
            out[b, r * NLAT * 128:(r + 1) * NLAT * 128, :] = np.asarray(res.results[b * RANKS + r]["out"])
    return out
```

```python
import math
from contextlib import ExitStack

import numpy as np
import concourse.bass as bass
import concourse.mybir as mybir
from concourse.bass_utils import run_bass_kernel_spmd

F32 = mybir.dt.float32
BF16 = mybir.dt.bfloat16
U8 = mybir.dt.uint8
AF = mybir.ActivationFunctionType
ALU = mybir.AluOpType
AX = mybir.AxisListType

D = 1024
DIN = 2048
EPS = 1e-6
GRID_W = 64
NEXP = 16
DEXP = 512
SCALE_A = 64 ** -0.5
SCALE_C = 32 ** -0.5


class Rec:
    __slots__ = ("q", "fn", "waits", "need", "val", "sem", "inc", "key")

    def __init__(self, q, fn):
        self.q, self.fn = q, fn
        self.waits = []
        self.need = False
        self.val = None
        self.sem = None
        self.inc = 1
        self.key = q.name


class Buf:
    __slots__ = ("name", "w", "r", "excl")

    def __init__(self, name="", excl=False):
        self.name = name
        self.w = None
        self.r = {}
        self.excl = excl


class Queue:
    def __init__(self, name, eng, sem):
        self.name, self.eng, self.sem = name, eng, sem
        self.recs = []
        self.dsems = []
        self.dlast = []
        self.dnext = 0


class FW:
    def __init__(self, nc, stack, n_dsem_sp=24, n_dsem_pool=12, n_cc=10):
        self.nc = nc
        self.Q = {}
        for name, eng in (("pe", nc.tensor), ("act", nc.scalar), ("dve", nc.vector), ("pool", nc.gpsimd), ("sp", nc.sync)):
            sem = stack.enter_context(nc.semaphore("q_" + name))
            self.Q[name] = Queue(name, eng, sem)
        for qn, n in (("sp", n_dsem_sp), ("pool", n_dsem_pool), ("act", 8)):
            q = self.Q[qn]
            for i in range(n):
                q.dsems.append(stack.enter_context(nc.semaphore(f"d_{qn}{i}")))
                q.dlast.append(None)
        self.ccsems = [stack.enter_context(nc.semaphore(f"cc{i}")) for i in range(n_cc)]
        self.cclast = [None] * n_cc
        self.ccnext = 0
        self.all_dma = []

    def _deps(self, q, reads, writes):
        deps = []
        for b in reads:
            if b.w is not None:
                deps.append(b.w)
            if b.excl:
                deps.extend(x for x in b.r.values() if x.q is not q)
        for b in writes:
            if b.w is not None:
                deps.append(b.w)
            deps.extend(b.r.values())
        out = []
        seen = set()
        for d in deps:
            if id(d) in seen:
                continue
            seen.add(id(d))
            if d.q is q and q.name == "pe" and d.key == "pe":
                continue
            out.append(d)
        return out

    def _commit(self, rec, reads, writes):
        for d in rec.waits:
            d.need = True
        rec.q.recs.append(rec)
        for b in reads:
            b.r[rec.key] = rec
        for b in writes:
            b.w = rec
            b.r = {}

    def op(self, qn, fn, reads=(), writes=()):
        q = self.Q[qn]
        rec = Rec(q, fn)
        rec.waits = self._deps(q, reads, writes)
        self._commit(rec, reads, writes)
        return rec

    def dma(self, qn, out, in_, reads=(), writes=(), **kw):
        q = self.Q[qn]
        rec = Rec(q, lambda e: e.dma_start(out=out, in_=in_, **kw))
        i = q.dnext
        q.dnext = (q.dnext + 1) % len(q.dsems)
        rec.key = f"{qn}_d{i}"
        rec.sem = q.dsems[i]
        rec.inc = 16
        rec.need = True
        rec.waits = self._deps(q, reads, writes)
        if q.dlast[i] is not None:
            rec.waits.append(q.dlast[i])
        q.dlast[i] = rec
        self._commit(rec, reads, writes)
        self.all_dma.append(rec)
        return rec

    def cc(self, fn, reads=(), writes=()):
        q = self.Q["pool"]
        rec = Rec(q, fn)
        i = self.ccnext
        self.ccnext = (self.ccnext + 1) % len(self.ccsems)
        rec.key = f"cc{i}"
        rec.sem = self.ccsems[i]
        rec.inc = 1
        rec.need = True
        rec.waits = self._deps(q, reads, writes)
        if self.cclast[i] is not None:
            rec.waits.append(self.cclast[i])
        self.cclast[i] = rec
        self._commit(rec, reads, writes)
        return rec

    def barrier(self):
        lasts = []
        for q in self.Q.values():
            for r in reversed(q.recs):
                if r.key == q.name and r.fn is not None:
                    lasts.append(r)
                    break
            lasts.extend(x for x in q.dlast if x is not None)
        lasts.extend(x for x in self.cclast if x is not None)
        for q in self.Q.values():
            rec = Rec(q, None)
            rec.waits = [d for d in lasts if not (d.q is q and d.key == q.name)]
            for d in rec.waits:
                d.need = True
            q.recs.append(rec)

    def finish(self, block):
        for q in self.Q.values():
            cnt = 0
            dcnt = {}
            for r in q.recs:
                if r.fn is None:
                    continue
                if r.key != q.name:
                    dcnt[r.key] = dcnt.get(r.key, 0) + r.inc
                    r.val = dcnt[r.key]
                elif r.need:
                    cnt += 1
                    r.val = cnt
                    r.sem = q.sem
        stats = {}
        for q in self.Q.values():
            def run(eng, q=q):
                seen = {}
                nw = 0
                for r in q.recs:
                    for d in r.waits:
                        k = d.key
                        if seen.get(k, 0) >= d.val:
                            continue
                        eng.wait_ge(d.sem, d.val)
                        seen[k] = d.val
                        nw += 1
                    if r.fn is None:
                        continue
                    ins = r.fn(eng)
                    if r.need:
                        ins.then_inc(r.sem, r.inc)
                stats[q.name] = (len(q.recs), nw)
            getattr(block, {"pe": "tensor", "act": "scalar", "dve": "vector", "pool": "gpsimd", "sp": "sync"}[q.name])(run)
        return stats


class Arena:
    def __init__(self, handle, nbytes):
        self.h, self.n = handle, nbytes
        self.off = 0
        self.marks = []

    def alloc(self, shape, dtype, parts=128):
        esz = {F32: 4, BF16: 2, U8: 1}[dtype]
        n = int(np.prod(shape)) * esz
        n_al = (n + 63) // 64 * 64
        assert self.off + n_al <= self.n, f"SBUF arena overflow: {self.off}+{n_al} > {self.n}"
        ap = self.h[0:parts, self.off:self.off + n]
        self.off += n_al
        self.peak = max(getattr(self, 'peak', 0), self.off)
        if dtype != U8:
            ap = ap.bitcast(dtype)
        if len(shape) == 1:
            return ap
        names = [f"a{i}" for i in range(len(shape))]
        pat = "p (" + " ".join(names) + ") -> p " + " ".join(names)
        return ap.rearrange(pat, **{nm: s for nm, s in zip(names[:-1], shape[:-1])})

    def mark(self):
        self.marks.append(self.off)

    def release(self):
        print('arena scope peak', getattr(self, 'peak', 0), 'at release, off', self.off)
        self.off = self.marks.pop()


def bcast_rows(ap_row, n):
    return ap_row.partition_broadcast(n) if ap_row.shape[0] != 1 else ap_row.broadcast_to([n] + list(ap_row.shape[1:]))


def MM(out, lhsT, rhs, start=True, stop=True, **kw):
    return lambda e: e.matmul(out, lhsT=lhsT, rhs=rhs, start=start, stop=stop, **kw)


def TR(out, in_, ident):
    return lambda e: e.transpose(out, in_, ident)


def ACT(out, in_, func, **kw):
    return lambda e: e.activation(out=out, in_=in_, func=func, **kw)


def TT(out, a, b, op):
    return lambda e: e.tensor_tensor(out=out, in0=a, in1=b, op=op)


def TS(out, a, s1, op0, s2=None, op1=None, **kw):
    if op1 is None:
        return lambda e: e.tensor_scalar(out=out, in0=a, scalar1=s1, scalar2=None, op0=op0, **kw)
    return lambda e: e.tensor_scalar(out=out, in0=a, scalar1=s1, scalar2=s2, op0=op0, op1=op1, **kw)


def STT(out, a, s, b, op0, op1):
    return lambda e: e.scalar_tensor_tensor(out=out, in0=a, scalar=s, in1=b, op0=op0, op1=op1)


def CP(out, in_):
    return lambda e: e.tensor_copy(out=out, in_=in_)


def MEMSET(ap, v):
    return lambda e: e.memset(ap, v)


def RSUM(out, in_, axis=None):
    return lambda e: e.reduce_sum(out=out, in_=in_, axis=axis or AX.X)


def RMAX(out, in_, axis=None):
    return lambda e: e.reduce_max(out=out, in_=in_, axis=axis or AX.X)


class Cfg:
    def __init__(self, NLAT=32, RANKS=4, NB=2, DEPTH=2, stop_after=None):
        self.NLAT, self.RANKS, self.NB, self.DEPTH = NLAT, RANKS, NB, DEPTH
        self.NT = NLAT + 2
        self.NKC = 2 + RANKS * NLAT
        self.NCORES = NB * RANKS
        self.stop_after = stop_after


ARENA_BYTES = 207 * 1024


class StopBuild(Exception):
    pass


def build_program(cfg):
    NLAT, RANKS, NT, NKC, DEPTH = cfg.NLAT, cfg.RANKS, cfg.NT, cfg.NKC, cfg.DEPTH
    KCH = 8 if NLAT % 8 == 0 else (2 if NLAT % 2 == 0 else 1)
    nc = bass.Bass("TRN2", target_bir_lowering=False)
    dbg = set(getattr(cfg, "debug", ()) or ())
    I = {}

    def inp(name, shape):
        I[name] = nc.dram_tensor(name, shape, F32, kind="ExternalInput").ap()

    inp("xl", [NLAT * 128, D]); inp("ctx", [256, D]); inp("c2", [2, D])
    inp("w_ada", [DEPTH, D, 6 * D]); inp("b_ada2", [DEPTH, 2, 6 * D]); inp("g_mix", [DEPTH, D])
    inp("w_in", [DEPTH, D, DIN]); inp("sink", [DEPTH, 8]); inp("ws_tok", [DEPTH, 4, 128, 128])
    inp("bs_tok", [DEPTH, 4, 128]); inp("g_tok", [DEPTH, 256]); inp("lam4", [DEPTH, 128]); inp("g_sub", [DEPTH, 64])
    inp("w_out", [DEPTH, D, D]); inp("g_ffn", [DEPTH, D]); inp("w_r", [DEPTH, D, 20]); inp("b_r", [DEPTH, 20])
    inp("w_gate", [DEPTH, NEXP, D, DEXP]); inp("w_up", [DEPTH, NEXP, D, DEXP]); inp("w_down", [DEPTH, NEXP, DEXP, D])
    inp("g_final", [1, D]); inp("ident", [128, 128]); inp("masks", [128, 4 * 128]); inp("rope", [128, NT * 96])
    inp("sel", [128, 3 * RANKS])
    out_d = nc.dram_tensor("out", [NLAT * 128, D], F32, kind="ExternalOutput").ap()

    def scr(name, shape, dt):
        kind = "ExternalOutput" if name in dbg else "Internal"
        return nc.dram_tensor(name, shape, dt, kind=kind).ap()

    modd = scr("modd", [DEPTH, 2, 6 * D], F32)
    xbuf = scr("xbuf", [NT, 128, D], F32)
    xmid = scr("xmid", [NT, 128, D], F32)
    qta_d = scr("qta_d", [NT, 128, 512], BF16)
    qtc_d = scr("qtc_d", [NT, 128, 256], BF16)
    yb_d = scr("yb_d", [NT, 128, 256], BF16)
    h2t_d = scr("h2t_d", [128, 8, NT * 128], BF16)
    kctx_d = scr("kctx_d", [128, 2, 256], BF16)
    vctx_d = scr("vctx_d", [2, 128, 260], BF16)
    xk_s = scr("xk_s", [RANKS, 128, 2, NLAT * 128], BF16)
    xv_s = scr("xv_s", [RANKS, NLAT, 128, 260], BF16)
    xh_s = scr("xh_s", [RANKS, 128, 2 * 258], BF16)
    if RANKS > 1:
        xk_r = scr("xk_r", [RANKS, 128, 2, NLAT * 128], BF16)
        xv_r = scr("xv_r", [RANKS, NLAT, 128, 260], BF16)
        xh_r = scr("xh_r", [RANKS, 128, 2 * 258], BF16)
    else:
        xk_r, xv_r, xh_r = xk_s, xv_s, xh_s

    DB = {}

    def db(name, idx=0):
        k = (name, idx)
        if k not in DB:
            DB[k] = Buf(f"{name}{idx}")
        return DB[k]

    with ExitStack() as st:
        arena_h = st.enter_context(nc.sbuf_tensor("arena", [128, ARENA_BYTES], U8))
        ps = st.enter_context(nc.psum_tensor("ps", [128, 8, 512], F32))
        fw = FW(nc, st)
        A = Arena(arena_h, ARENA_BYTES)
        PS = [Buf(f"ps{i}", excl=True) for i in range(8)]
        psb = [ps[:, i, :].bitcast(BF16) for i in range(8)]

        class Ring:
            def __init__(self, n, shape, dt, parts=128):
                self.aps = [A.alloc(shape, dt, parts) for _ in range(n)]
                self.bufs = [Buf() for _ in range(n)]
                self.i = 0

            def next(self):
                k = self.i
                self.i = (self.i + 1) % len(self.aps)
                return self.aps[k], self.bufs[k]

        op, dma = fw.op, fw.dma

        ident_f = A.alloc([128], F32); Bidf = Buf()
        ident_b = A.alloc([128], BF16); Bidb = Buf()
        masks = A.alloc([4, 128], BF16); Bmask = Buf()
        sel = A.alloc([3 * RANKS], F32); Bsel = Buf()
        dma("sp", ident_f, I["ident"], writes=[Bidf])
        dma("pool", ident_b, I["ident"], writes=[Bidb])
        dma("pool", masks, I["masks"].rearrange("p (m q) -> p m q", m=4), writes=[Bmask])
        dma("sp", sel, I["sel"], writes=[Bsel])
        mneg = A.alloc([4, 512], BF16); Bmneg = Buf()
        for m_ in range(4):
            op("dve", TS(mneg[:, m_, :].rearrange("p (j q) -> p j q", j=4), masks[:, m_, :].unsqueeze(1).broadcast_to([128, 4, 128]),
                         -1.0, ALU.add, 30000.0, ALU.mult), reads=[Bmask], writes=[Bmneg])
        ones1 = A.alloc([1], F32); Bones = Buf()
        op("pool", MEMSET(ones1, 1.0), writes=[Bones])

        gates = A.alloc([NT, 16], F32)
        Bgate = [Buf() for _ in range(NT)]

        eps_t = A.alloc([1], F32); Beps = Buf()
        op("pool", MEMSET(eps_t, EPS), writes=[Beps])

        A.mark()
        c_raw = A.alloc([2, 8], F32); Bcraw = Buf()
        cS = A.alloc([8, 2], F32); BcS = Buf()
        dma("sp", c_raw, I["c2"].rearrange("r (p k) -> p r k", p=128), writes=[Bcraw])
        op("act", ACT(cS, c_raw.rearrange("p r k -> p k r"), AF.Silu), reads=[Bcraw], writes=[BcS])
        wa_ring = Ring(2, [8, 512], F32)
        bada = A.alloc([6 * D], F32, parts=2); Bbada = Buf()
        msb_ring = Ring(2, [512], F32, parts=2)
        import os as _os
        for l in range(DEPTH if not _os.environ.get("DBG_NOADA") else 0):
            dma("sp", bada, I["b_ada2"][l], writes=[Bbada])
            for cb in range(12):
                wa, Bwa = wa_ring.next()
                dma("sp", wa, I["w_ada"][l, :, cb * 512:(cb + 1) * 512].rearrange("(p k) n -> p k n", p=128), writes=[Bwa])
                for kc in range(8):
                    op("pe", MM(ps[0:2, 0, :], cS[:, kc, :], wa[:, kc, :], start=(kc == 0), stop=(kc == 7)),
                       reads=[BcS, Bwa], writes=[PS[0]])
                msb, Bmsb = msb_ring.next()
                op("dve", TT(msb, ps[0:2, 0, :], bada[:, cb * 512:(cb + 1) * 512], ALU.add), reads=[PS[0], Bbada], writes=[Bmsb])
                dma("sp", modd[l, :, cb * 512:(cb + 1) * 512], msb, reads=[Bmsb], writes=[db("modd", l)])
        A.release()
        fw.barrier()

        def load_mod(l, r, chunk, dst, buf, q="sp"):
            return dma(q, dst, modd[l, r:r + 1, chunk * D:(chunk + 1) * D].broadcast_to([128, D]),
                       reads=[db("modd", l)], writes=[buf])

        def load_gain(src_row, dst, buf, q="sp"):
            return dma(q, dst, src_row.broadcast_to([128] + [src_row.shape[1]]), writes=[buf])

        def xsrc(l, t):
            if l == 0:
                return (I["ctx"][t * 128:(t + 1) * 128, :], []) if t < 2 else (I["xl"][(t - 2) * 128:(t - 1) * 128, :], [])
            return xbuf[t], [db("xbuf", t)]

        def rstd_from_ss(ss, Bss, tmp, Btmp, rstd, Brstd, n):
            op("act", ACT(tmp, ss, AF.Ln, scale=1.0 / n, bias=eps_t), reads=[Bss, Beps], writes=[Btmp])
            op("act", ACT(rstd, tmp, AF.Exp, scale=-0.5), reads=[Btmp], writes=[Brstd])

        def ck(name):
            if cfg.stop_after == name:
                raise StopBuild()

        try:
          for l in range(DEPTH):
              last = (l == DEPTH - 1)
              if cfg.stop_after == "pre":
                  break
              lam_init = 0.8 - 0.6 * math.exp(-0.3 * l)
              A.mark()
              lam4b = A.alloc([4, 32], F32); Bl4 = Buf()
              dma("sp", lam4b, I["lam4"][l:l + 1, :].broadcast_to([128, 128]).rearrange("p (a b) -> p a b", a=4), writes=[Bl4])
              lsc = A.alloc([8], F32); Blsc = Buf()
              ljunk = A.alloc([32], F32); Blj = Buf()
              op("dve", TT(ljunk, lam4b[:, 0, :], lam4b[:, 1, :], ALU.mult), reads=[Bl4], writes=[Blj])
              op("dve", RSUM(lsc[:, 0:1], ljunk), reads=[Blj], writes=[Blsc])
              op("dve", TT(ljunk, lam4b[:, 2, :], lam4b[:, 3, :], ALU.mult), reads=[Bl4], writes=[Blj])
              op("dve", RSUM(lsc[:, 1:2], ljunk), reads=[Blj, Blsc], writes=[Blsc])
              op("act", ACT(lsc[:, 2:4], lsc[:, 0:2], AF.Exp), reads=[Blsc], writes=[Blsc])
              op("dve", TT(lsc[:, 4:5], lsc[:, 2:3], lsc[:, 3:4], ALU.subtract), reads=[Blsc], writes=[Blsc])
              neglam = lsc[:, 5:6]
              op("dve", TS(neglam, lsc[:, 4:5], -1.0, ALU.mult, -lam_init, ALU.add), reads=[Blsc], writes=[Blsc])
              esink = A.alloc([8], F32); Besink = Buf()
              dma("sp", esink, I["sink"][l:l + 1, :].broadcast_to([128, 8]), writes=[Besink])
              op("act", ACT(esink, esink, AF.Exp), reads=[Besink], writes=[Besink])
              gsub_s = A.alloc([64], F32); Bgsub = Buf()
              dma("sp", gsub_s, I["g_sub"][l:l + 1, :].broadcast_to([128, 64]), writes=[Bgsub])
              op("dve", TS(gsub_s, gsub_s, 1.0 - lam_init, ALU.mult), reads=[Bgsub], writes=[Bgsub])
              A.mark()
              KTA = A.alloc([(NT + 2) * 128], BF16)
              VA = A.alloc([NT + 2, 130], BF16)
              BKTA = [Buf() for _ in range(NT + 2)]
              BVA = [Buf() for _ in range(NT + 2)]
              op("pool", MEMSET(VA, 1.0), writes=BVA)
              op("pool", MEMSET(KTA[:, NT * 128:(NT + 2) * 128], 0.0), writes=BKTA[NT:NT + 2])

              A.mark()
              win = A.alloc([8, DIN], BF16); Bwin = Buf()
              for kc in range(8):
                  dma("pool", win[:, kc, :], I["w_in"][l, kc * 128:(kc + 1) * 128, :], writes=[Bwin])
              rope_ring = Ring(2, [96], F32)
              G1 = [A.alloc([D], F32) for _ in range(2)]; BG1 = [Buf(), Buf()]
              SH1 = [A.alloc([D], F32) for _ in range(2)]; BSH1 = [Buf(), Buf()]
              gtmp = A.alloc([D], F32); Bgtmp = Buf()
              for r in range(2):
                  load_mod(l, r, 1, G1[r], BG1[r])
                  load_gain(I["g_mix"][l:l + 1, :], gtmp, Bgtmp)
                  op("dve", STT(G1[r], G1[r], 1.0, gtmp, ALU.add, ALU.mult), reads=[BG1[r], Bgtmp], writes=[BG1[r]])
                  load_mod(l, r, 0, SH1[r], BSH1[r])
              ws_f = A.alloc([4, 128], F32); Bwsf = Buf()
              dma("sp", ws_f, I["ws_tok"][l].rearrange("g p q -> p g q"), writes=[Bwsf])
              wsT = A.alloc([4, 128], BF16); BwsT = Buf()
              for g in range(4):
                  op("pe", TR(ps[:, 7, g * 128:(g + 1) * 128], ws_f[:, g, :], ident_f), reads=[Bwsf, Bidf], writes=[PS[7]])
              op("dve", CP(wsT, ps[:, 7, :].rearrange("p (g q) -> p g q", g=4)), reads=[PS[7]], writes=[BwsT])
              bsT = A.alloc([4], F32); BbsT = Buf()
              dma("sp", bsT, I["bs_tok"][l].rearrange("g p -> p g"), writes=[BbsT], allow_slow_non_contiguous=True)
              gtokb = A.alloc([256], F32); Bgtok = Buf()
              dma("sp", gtokb, I["g_tok"][l:l + 1, :].broadcast_to([128, 256]), writes=[Bgtok])

              ck(('p1a', l))
              x_ring = Ring(2, [D], F32)
              junk_b = A.alloc([D], BF16); Bjunk = Buf()
              sc_ring = Ring(2, [8], F32)
              t1_ring = Ring(2, [D], F32)
              h_ring = Ring(2, [D], BF16)
              hT_ring = Ring(2, [8, 128], BF16)
              psb_ring = Ring(2, [DIN], F32)
              rt_ring = Ring(2, [4, 320], F32)
              qk_ring = Ring(2, [1152], BF16)
              qta_ring = Ring(2, [512], BF16)
              qtc_ring = Ring(2, [256], BF16)
              vt = A.alloc([4, 65], BF16); Bvt = Buf()
              op("pool", MEMSET(vt, 1.0), writes=[Bvt])
              NSTG = min(2, NLAT)
              stgK_ring = Ring(2, [RANKS, 2, NSTG * 128], BF16)
              stgV_ring = Ring(2, [RANKS, NSTG, 260], BF16)
              hal_st = A.alloc([RANKS, 2 * 258], BF16); Bhal = Buf()
              kct_ring = Ring(2, [256], BF16)
              gl_ring = Ring(2, [4, 512], F32)
              ln_ring = Ring(2, [16], F32)
              vn_ring = Ring(2, [256], BF16)
              yb_ring = Ring(2, [256], BF16)
              sq_ring = Ring(2, [256], F32)

              stg = {}

              def p1_tile(t):
                  r = 1 if t < 2 else 0
                  xt, Bx = x_ring.next()
                  src, sdeps = xsrc(l, t)
                  dma("act", xt, src, reads=sdeps, writes=[Bx])
                  sc, Bsc = sc_ring.next()
                  t1, Bt1 = t1_ring.next()
                  op("dve", TT(t1, xt, xt, ALU.mult), reads=[Bx], writes=[Bt1])
                  op("dve", RSUM(sc[:, 0:1], t1), reads=[Bt1], writes=[Bsc])
                  rstd_from_ss(sc[:, 0:1], Bsc, sc[:, 1:2], Bsc, sc[:, 2:3], Bsc, D)
                  op("dve", STT(t1, xt, sc[:, 2:3], G1[r], ALU.mult, ALU.mult), reads=[Bx, Bsc, BG1[r]], writes=[Bt1])
                  h, Bh = h_ring.next()
                  op("dve", TT(h, t1, SH1[r], ALU.add), reads=[Bt1, BSH1[r]], writes=[Bh])
                  yield
                  for kc in range(8):
                      op("pe", TR(psb[0][:, kc * 128:(kc + 1) * 128], h[:, kc * 128:(kc + 1) * 128], ident_b), reads=[Bh, Bidb], writes=[PS[0]])
                  hT, BhT = hT_ring.next()
                  op("act", ACT(hT, psb[0].rearrange("p (k t) -> p k t", k=8), AF.Copy), reads=[PS[0]], writes=[BhT])
                  yield
                  for j in range(4):
                      for kc in range(8):
                          op("pe", MM(ps[:, 1 + j, :], hT[:, kc, :], win[:, kc, j * 512:(j + 1) * 512], start=(kc == 0), stop=(kc == 7)),
                             reads=[BhT, Bwin], writes=[PS[1 + j]])
                  ck(('p1b', l))
                  p_sb, Bp = psb_ring.next()
                  op("act", ACT(p_sb[:, 0:512], ps[:, 1, :], AF.Copy), reads=[PS[1]], writes=[Bp])
                  op("act", ACT(p_sb[:, 512:1024], ps[:, 2, :], AF.Copy), reads=[PS[2]], writes=[Bp])
                  op("dve", CP(p_sb[:, 1024:1536], ps[:, 3, :]), reads=[PS[3]], writes=[Bp])
                  op("dve", CP(p_sb[:, 1536:2048], ps[:, 4, :]), reads=[PS[4]], writes=[Bp])
                  yield
                  ropet, Brope = rope_ring.next()
                  dma("act", ropet, I["rope"][:, t * 96:(t + 1) * 96], writes=[Brope])
                  qk, Bqk = qk_ring.next()
                  rt, Brt = rt_ring.next()
                  for (c0, nh, dd, tb0, o0) in ((0, 10, 16, 0, 0), (1280, 16, 8, 64, 640)):
                      w = nh * 4 * dd
                      xv = p_sb[:, c0:c0 + w].rearrange("p (h r t d) -> p h r t d", h=nh, r=2, t=2, d=dd)
                      ov = qk[:, o0:o0 + w].rearrange("p (h r t d) -> p h r t d", h=nh, r=2, t=2, d=dd)
                      cosv = ropet[:, tb0:tb0 + 2 * dd].rearrange("p (r d) -> p r d", r=2).unsqueeze(1).broadcast_to([128, nh, 2, dd])
                      sinv = ropet[:, tb0 + 2 * dd:tb0 + 4 * dd].rearrange("p (r d) -> p r d", r=2).unsqueeze(1).broadcast_to([128, nh, 2, dd])
                      tv = [rt[:, i, 0:nh * 2 * dd].rearrange("p (h r d) -> p h r d", h=nh, r=2, d=dd) for i in range(4)]
                      x1, x2 = xv[:, :, :, 0, :], xv[:, :, :, 1, :]
                      op("dve", TT(tv[0], x1, cosv, ALU.mult), reads=[Bp, Brope], writes=[Brt])
                      op("dve", TT(tv[1], x2, sinv, ALU.mult), reads=[Bp, Brope], writes=[Brt])
                      op("dve", TT(tv[2], x1, sinv, ALU.mult), reads=[Bp, Brope], writes=[Brt])
                      op("dve", TT(tv[3], x2, cosv, ALU.mult), reads=[Bp, Brope], writes=[Brt])
                      op("dve", TT(ov[:, :, :, 0, :], tv[0], tv[1], ALU.subtract), reads=[Brt], writes=[Bqk])
                      op("dve", TT(ov[:, :, :, 1, :], tv[2], tv[3], ALU.add), reads=[Brt], writes=[Bqk])
                  ck(('p1c', l))
                  op("pool", CP(VA[:, t, :].rearrange("p (g d) -> p g d", g=2)[:, :, 0:64],
                                p_sb[:, 640:768].rearrange("p (g d) -> p g d", g=2)), reads=[Bp], writes=[BVA[t]])
                  op("pool", CP(vt[:, :, 0:64], p_sb[:, 1792:2048].rearrange("p (h d) -> p h d", h=4)), reads=[Bp], writes=[Bvt])
                  if t < 2:
                      dma("sp", vctx_d[t], vt.rearrange("p h d -> p (h d)"), reads=[Bvt], writes=[db("vctx")])
                  else:
                      j = (t - 2) % NSTG
                      if j == 0:
                          stg["K"], stg["BK"] = stgK_ring.next()
                          stg["V"], stg["BV"] = stgV_ring.next()
                      stgK, BstK, stgV, BstV = stg["K"], stg["BK"], stg["V"], stg["BV"]
                      for rr in range(RANKS):
                          op("pool", TS(stgV[:, rr, j, :], vt.rearrange("p h d -> p (h d)"), sel[:, rr:rr + 1], ALU.mult, 1.0, ALU.mult),
                             reads=[Bvt, Bsel], writes=[BstV])
                  yield
                  ck(('p1d', l))
                  for jj in range(4):
                      op("pe", TR(psb[5][:, jj * 128:(jj + 1) * 128], qk[:, jj * 128:(jj + 1) * 128], ident_b),
                         reads=[Bqk, Bidb], writes=[PS[5]])
                  op("pe", TR(psb[5][:, 512:640], qk[:, 512:640], ident_b), reads=[Bqk, Bidb], writes=[PS[5]])
                  for c in range(4):
                      op("pe", TR(psb[6][:, c * 128:(c + 1) * 128], qk[:, 640 + c * 128:640 + (c + 1) * 128], ident_b),
                         reads=[Bqk, Bidb], writes=[PS[6]])
                  ck(('p1d1', l))
                  qta_t, Bqta = qta_ring.next()
                  op("dve", CP(qta_t, psb[5][:, 0:512]), reads=[PS[5]], writes=[Bqta])
                  dma("sp", qta_d[t], qta_t, reads=[Bqta], writes=[db("qta", t)])
                  ck(('p1d2', l))
                  op("act", ACT(KTA[:, t * 128:(t + 1) * 128], psb[5][:, 512:640], AF.Copy), reads=[PS[5]], writes=[BKTA[t]])
                  ck(('p1d3', l))
                  qtc_t, Bqtc = qtc_ring.next()
                  op("dve", CP(qtc_t, psb[6][:, 0:256]), reads=[PS[6]], writes=[Bqtc])
                  dma("sp", qtc_d[t], qtc_t, reads=[Bqtc], writes=[db("qtc", t)])
                  ck(('p1d4', l))
                  if t < 2:
                      kct, Bkct = kct_ring.next()
                      op("act", ACT(kct, psb[6][:, 256:512], AF.Copy), reads=[PS[6]], writes=[Bkct])
                      dma("sp", kctx_d[:, :, t * 128:(t + 1) * 128], kct.rearrange("p (c k) -> p c k", c=2), reads=[Bkct], writes=[db("kctx")])
                  else:
                      j = (t - 2) % NSTG
                      for rr in range(RANKS):
                          op("act", ACT(stgK[:, rr, :, j * 128:(j + 1) * 128], psb[6][:, 256:512].rearrange("p (c k) -> p c k", c=2), AF.Copy,
                                        scale=sel[:, rr:rr + 1]), reads=[PS[6], Bsel], writes=[BstK])
                      if j == NSTG - 1 or t == NT - 1:
                          g0 = (t - 2) - j
                          ng = j + 1
                          for rr in range(RANKS):
                              dma("sp", xk_s[rr, :, :, g0 * 128:(g0 + ng) * 128], stgK[:, rr, :, 0:ng * 128], reads=[BstK], writes=[db("xk_s", rr)])
                              dma("sp", xv_s[rr, g0:g0 + ng].rearrange("j p f -> p j f"), stgV[:, rr, 0:ng, :], reads=[BstV], writes=[db("xv_s", rr)])
                      if t == 2 or t == NT - 1:
                          w = 0 if t == 2 else 1
                          for rr in range(RANKS):
                              op("pool", TS(hal_st[:, rr, w * 258:w * 258 + 128], KTA[:, t * 128:(t + 1) * 128], sel[:, rr:rr + 1], ALU.mult, 1.0, ALU.mult),
                                 reads=[BKTA[t], Bsel], writes=[Bhal])
                              op("pool", TS(hal_st[:, rr, w * 258 + 128:(w + 1) * 258], VA[:, t, :], sel[:, rr:rr + 1], ALU.mult, 1.0, ALU.mult),
                                 reads=[BVA[t], Bsel], writes=[Bhal])
                  yield
                  ck(('p1e', l))
                  gl, Bgl = gl_ring.next()
                  xg, x2, th, gg = gl[:, 0, :], gl[:, 1, :], gl[:, 2, :], gl[:, 3, :]
                  op("dve", CP(xg, p_sb[:, 768:1280]), reads=[Bp], writes=[Bgl])
                  op("dve", TT(x2, xg, xg, ALU.mult), reads=[Bgl], writes=[Bgl])
                  op("dve", TS(x2, x2, 0.044715, ALU.mult, 1.0, ALU.add), reads=[Bgl], writes=[Bgl])
                  op("dve", TT(x2, x2, xg, ALU.mult), reads=[Bgl], writes=[Bgl])
                  op("act", ACT(th, x2, AF.Tanh, scale=math.sqrt(2.0 / math.pi)), reads=[Bgl], writes=[Bgl])
                  op("dve", TS(th, th, 0.5, ALU.mult, 0.5, ALU.add), reads=[Bgl], writes=[Bgl])
                  op("dve", TT(gg, th, xg, ALU.mult), reads=[Bgl], writes=[Bgl])
                  ug = gg[:, 0:256]
                  vg = gg[:, 256:512].rearrange("p (g d) -> p g d", g=4)
                  ln, Bln = ln_ring.next()
                  sq, Bsq = sq_ring.next()
                  op("dve", RSUM(ln[:, 0:4], vg), reads=[Bgl], writes=[Bln])
                  op("dve", TT(sq, gg[:, 256:512], gg[:, 256:512], ALU.mult), reads=[Bgl], writes=[Bsq])
                  op("dve", RSUM(ln[:, 4:8], sq.rearrange("p (g d) -> p g d", g=4)), reads=[Bsq, Bln], writes=[Bln])
                  op("dve", TS(ln[:, 0:4], ln[:, 0:4], 1.0 / 64, ALU.mult), reads=[Bln], writes=[Bln])
                  op("dve", TT(ln[:, 8:12], ln[:, 0:4], ln[:, 0:4], ALU.mult), reads=[Bln], writes=[Bln])
                  op("dve", STT(ln[:, 4:8], ln[:, 4:8], 1.0 / 64, ln[:, 8:12], ALU.mult, ALU.subtract), reads=[Bln], writes=[Bln])
                  op("act", ACT(ln[:, 8:12], ln[:, 4:8], AF.Ln, bias=eps_t), reads=[Bln, Beps], writes=[Bln])
                  op("act", ACT(ln[:, 12:16], ln[:, 8:12], AF.Exp, scale=-0.5), reads=[Bln], writes=[Bln])
                  for g in range(4):
                      op("dve", TS(sq[:, g * 64:(g + 1) * 64], vg[:, g, :], ln[:, g:g + 1], ALU.subtract, ln[:, 12 + g:13 + g], ALU.mult),
                         reads=[Bgl, Bln, Bsq], writes=[Bsq])
                  yield
                  vn, Bvn = vn_ring.next()
                  op("dve", TT(vn, sq, gtokb, ALU.mult), reads=[Bsq, Bgtok], writes=[Bvn])
                  for g in range(4):
                      op("pe", MM(ps[:, 7, g * 64:(g + 1) * 64], wsT[:, g, :], vn[:, g * 64:(g + 1) * 64], start=True, stop=True, skip_group_check=True),
                         reads=[BwsT, Bvn], writes=[PS[7]])
                  ybt, Byb = yb_ring.next()
                  for g in range(4):
                      op("dve", STT(ybt[:, g * 64:(g + 1) * 64], ps[:, 7, g * 64:(g + 1) * 64], bsT[:, g:g + 1], ug[:, g * 64:(g + 1) * 64], ALU.add, ALU.mult),
                         reads=[PS[7], BbsT, Bgl], writes=[Byb])
                  dma("sp", yb_d[t], ybt, reads=[Byb], writes=[db("yb", t)])

              def run_interleaved(gens, depth):
                  active = []
                  it = iter(gens)
                  done = False
                  while True:
                      while len(active) < depth and not done:
                          try:
                              active.append(next(it))
                          except StopIteration:
                              done = True
                      if not active:
                          break
                      for g_ in list(active):
                          try:
                              next(g_)
                          except StopIteration:
                              active.remove(g_)

              run_interleaved((p1_tile(t) for t in range(NT)), 2)
              ck(('p1f', l))
              for rr in range(RANKS):
                  dma("sp", xh_s[rr], hal_st[:, rr, :], reads=[Bhal], writes=[db("xh_s", rr)])
              A.release()
              fw.barrier()
              if cfg.stop_after == ("p1", l):
                  break

              if RANKS > 1:
                  groups = [[b * RANKS + r for r in range(RANKS)] for b in range(cfg.NB)]
                  fw.cc(lambda e: e.collective_compute("AllReduce", ALU.add, replica_groups=groups,
                                                       ins=[xh_s.rearrange("r p f -> (r p) f")], outs=[xh_r.rearrange("r p f -> (r p) f")]),
                        reads=[db("xh_s", rr) for rr in range(RANKS)], writes=[db("xh_r")])
                  for rr in range(RANKS):
                      fw.cc(lambda e, rr=rr: e.collective_compute("AllReduce", ALU.add, replica_groups=groups,
                                                                  ins=[xk_s[rr].rearrange("p c k -> p (c k)")],
                                                                  outs=[xk_r[rr].rearrange("p c k -> p (c k)")]),
                            reads=[db("xk_s", rr)], writes=[db("xk_r", rr)])
                      fw.cc(lambda e, rr=rr: e.collective_compute("AllReduce", ALU.add, replica_groups=groups,
                                                                  ins=[xv_s[rr].rearrange("j p f -> (j p) f")],
                                                                  outs=[xv_r[rr].rearrange("j p f -> (j p) f")]),
                            reads=[db("xv_s", rr)], writes=[db("xv_r", rr)])
                  kvr = lambda name, rr: db(name + "_r", rr)
              else:
                  kvr = lambda name, rr: db(name + "_s", rr)
              Bxh = db("xh_r") if RANKS > 1 else db("xh_s", 0)

              A.mark()
              hal = A.alloc([RANKS, 2 * 258], BF16); Bhl = Buf()
              dma("sp", hal, xh_r.rearrange("r p f -> p r f"), reads=[Bxh], writes=[Bhl])
              hacc = A.alloc([2, 258], F32); Bhacc = Buf()
              for w, so in ((0, 2 * RANKS), (1, RANKS)):
                  for rr in range(RANKS):
                      src = hal[:, rr, w * 258:(w + 1) * 258]
                      if rr == 0:
                          op("dve", TS(hacc[:, w, :], src, sel[:, so + rr:so + rr + 1], ALU.mult), reads=[Bhl, Bsel], writes=[Bhacc])
                      else:
                          op("dve", STT(hacc[:, w, :], src, sel[:, so + rr:so + rr + 1], hacc[:, w, :], ALU.mult, ALU.add),
                             reads=[Bhl, Bsel, Bhacc], writes=[Bhacc])
              op("dve", CP(KTA[:, NT * 128:(NT + 1) * 128], hacc[:, 1, 0:128]), reads=[Bhacc], writes=[BKTA[NT]])
              op("dve", CP(VA[:, NT, :], hacc[:, 1, 128:258]), reads=[Bhacc], writes=[BVA[NT]])
              op("dve", CP(KTA[:, (NT + 1) * 128:(NT + 2) * 128], hacc[:, 0, 0:128]), reads=[Bhacc], writes=[BKTA[NT + 1]])
              op("dve", CP(VA[:, NT + 1, :], hacc[:, 0, 128:258]), reads=[Bhacc], writes=[BVA[NT + 1]])

              wout = A.alloc([8, D], BF16); Bwout = Buf()
              for kc in range(8):
                  dma("pool", wout[:, kc, :], I["w_out"][l, kc * 128:(kc + 1) * 128, :], writes=[Bwout])
              wr = A.alloc([8, 20], F32); Bwr = Buf()
              dma("sp", wr, I["w_r"][l].rearrange("(k p) n -> p k n", p=128), writes=[Bwr])
              brb = A.alloc([20], F32); Bbr = Buf()
              dma("sp", brb, I["b_r"][l:l + 1, :].broadcast_to([128, 20]), writes=[Bbr])
              GT1 = A.alloc([D], F32); BGT1 = Buf()
              G2 = A.alloc([D], F32); BG2 = Buf()
              SH2 = A.alloc([D], F32); BSH2 = Buf()
              gtmp2 = A.alloc([D], F32); Bgtmp2 = Buf()

              def load_p2_mods(r):
                  load_mod(l, r, 2, GT1, BGT1)
                  load_mod(l, r, 4, G2, BG2)
                  load_gain(I["g_ffn"][l:l + 1, :], gtmp2, Bgtmp2)
                  op("dve", STT(G2, G2, 1.0, gtmp2, ALU.add, ALU.mult), reads=[BG2, Bgtmp2], writes=[BG2])
                  load_mod(l, r, 3, SH2, BSH2)

              kc_ring = Ring(3, [2, KCH * 128], BF16)
              vc_ring = Ring(3, [KCH, 260], BF16)
              qb_ring = Ring(2, [2, 256], BF16)
              NE = 3
              E_aps = [[A.alloc([1024], BF16) for _ in range(NE)] for _ in range(2)]
              E_buf = [[Buf() for _ in range(NE)] for _ in range(2)]
              usb = A.alloc([2, 512], F32); Busb = Buf()
              accS = [A.alloc([1024], F32) for _ in range(2)]; BaccS = [Buf(), Buf()]
              epair = [A.alloc([1024], BF16) for _ in range(2)]; Bepair = [Buf(), Buf()]
              uasb = A.alloc([2, 512], F32); Buasb = Buf()
              qa_ring = Ring(2, [512], BF16)
              eab_ring = Ring(2, [5, 512], BF16)
              fsc_ring = Ring(4, [48], F32)
              osb_ring = Ring(2, [256], F32)
              ojk = A.alloc([256], F32); Bojk = Buf()
              ymix_ring = Ring(2, [D], BF16)
              ymT_ring = Ring(2, [8, 128], BF16)
              x2_ring = Ring(2, [D], F32)
              yt_ring = Ring(2, [D], F32)
              xm_ring = Ring(2, [D], F32)
              h2_ring = Ring(2, [D], F32)
              h2Tf_ring = Ring(2, [8, 128], F32)
              h2Tb_ring = Ring(2, [8, 128], BF16)
              rsc_ring = Ring(2, [96], F32)
              junk2 = A.alloc([D], BF16); Bjunk2 = Buf()

              qblocks = []
              if not last:
                  qblocks.append(([0, 1], 1, "ctx"))
              for i in range(0, NLAT, 2):
                  qblocks.append(([2 + i, 3 + i] if i + 1 < NLAT else [2 + i], 0, "lat"))
              cur_r = None
              pending = []
              pend_ptr = [0]

              def step_pending():
                  if not pending:
                      return
                  k = pend_ptr[0] % len(pending)
                  try:
                      next(pending[k])
                      pend_ptr[0] += 1
                  except StopIteration:
                      pending.pop(k)

              def flush_pending():
                  while pending:
                      step_pending()

              for (tiles, r, kind) in qblocks:
                  if r != cur_r:
                      load_p2_mods(r)
                      cur_r = r
                  nq = len(tiles) * 128
                  chunks = [(kctx_d, vctx_d.rearrange("j p f -> p j f"), 2, [db("kctx"), db("vctx")])]
                  if kind == "lat":
                      for rr in range(RANKS):
                          for c0 in range(0, NLAT, KCH):
                              chunks.append((xk_r[rr, :, :, c0 * 128:(c0 + KCH) * 128], xv_r[rr, c0:c0 + KCH].rearrange("j p f -> p j f"), KCH,
                                             [kvr("xk", rr), kvr("xv", rr)]))
                  Qb, BQb = qb_ring.next()
                  for j, t in enumerate(tiles):
                      dma("sp", Qb[:, :, j * 128:(j + 1) * 128], qtc_d[t].rearrange("p (c k) -> p c k", c=2), reads=[db("qtc", t)], writes=[BQb])
                  ktl = []
                  loaded = {}

                  def load_chunk(ci):
                      ksrc, vsrc, n, deps = chunks[ci]
                      kc_t, Bkc = kc_ring.next()
                      vc_t, Bvc = vc_ring.next()
                      dma("sp", kc_t[:, :, 0:n * 128], ksrc, reads=deps, writes=[Bkc])
                      dma("sp", vc_t[:, 0:n, :], vsrc, reads=deps, writes=[Bvc])
                      loaded[ci] = (kc_t, Bkc, vc_t, Bvc)

                  for ci, ch in enumerate(chunks):
                      for kk in range(ch[2]):
                          ktl.append((ci, kk))
                  nk = len(ktl)
                  load_chunk(0)
                  if len(chunks) > 1:
                      load_chunk(1)

                  def qk(i, X):
                      ci, kk = ktl[i]
                      kc_t, Bkc, _, _ = loaded[ci]
                      for c in range(2):
                          for gg in range(2):
                              g = 2 * X + gg
                              op("pe", MM(ps[:, 2 * X + gg, c * 256:c * 256 + nq], kc_t[32 * g:32 * g + 32, c, kk * 128:(kk + 1) * 128],
                                          Qb[32 * g:32 * g + 32, c, 0:nq], tile_position=(32 * g, 0)),
                                 reads=[Bkc, BQb], writes=[PS[2 * X], PS[2 * X + 1]])

                  def ex(i, X):
                      Et, Eb = E_aps[X][i % NE], E_buf[X][i % NE]
                      op("act", ACT(Et.rearrange("p (c g q) -> p g c q", c=2, g=2)[:, :, :, 0:nq],
                                    ps[:, 2 * X:2 * X + 2, :].rearrange("p g (c q) -> p g c q", c=2)[:, :, :, 0:nq], AF.Exp, scale=SCALE_C),
                         reads=[PS[2 * X], PS[2 * X + 1]], writes=[Eb])

                  def pv(i, X):
                      ci, kk = ktl[i]
                      _, _, vc_t, Bvc = loaded[ci]
                      Et, Eb = E_aps[X][i % NE], E_buf[X][i % NE]
                      for c in range(2):
                          hh = 2 * c + X
                          rhs = Et[:, c * 512:(c + 1) * 512].rearrange("p (g q) -> p g q", g=2)[:, :, 0:nq]
                          o = ps[64 * c:64 * c + 64, 4 + X, :].rearrange("p (g q) -> p g q", g=2)[:, :, 0:nq]
                          op("pe", MM(o, vc_t[:, kk, hh * 65:hh * 65 + 64], rhs, start=(i == 0), stop=(i == nk - 1), tile_position=(0, 64 * c),
                                      skip_group_check=True), reads=[Bvc, Eb], writes=[PS[4 + X]])
                      if i % 2 == 1:
                          Ep = E_aps[X][(i - 1) % NE]
                          Epb = E_buf[X][(i - 1) % NE]
                          if i == 1:
                              op("dve", TT(accS[X], Ep, Et, ALU.add), reads=[Epb, Eb], writes=[BaccS[X]])
                          else:
                              op("dve", TT(epair[X], Ep, Et, ALU.add), reads=[Epb, Eb], writes=[Bepair[X]])
                              op("dve", TT(accS[X], accS[X], epair[X], ALU.add), reads=[Bepair[X], BaccS[X]], writes=[BaccS[X]])
                      elif i == nk - 1:
                          if i == 0:
                              op("dve", CP(accS[X], Et), reads=[Eb], writes=[BaccS[X]])
                          else:
                              op("dve", TT(accS[X], accS[X], Et, ALU.add), reads=[Eb, BaccS[X]], writes=[BaccS[X]])

                  adv = max(1, nk // 60)
                  qk(0, 0)
                  qk(0, 1)
                  for i in range(nk):
                      ci, kk = ktl[i]
                      if kk == 0 and ci + 2 < len(chunks) and (ci + 2) not in loaded:
                          load_chunk(ci + 2)
                      for X in range(2):
                          ex(i, X)
                          if i + 1 < nk:
                              qk(i + 1, X)
                          pv(i, X)
                      if pending and i % adv == adv // 2:
                          step_pending()
                  flush_pending()

                  op("act", ACT(usb[:, 0:2, :], ps[:, 4:6, :], AF.Copy), reads=[PS[4], PS[5]], writes=[Busb])
                  for j, t in enumerate(tiles):
                      for X in range(2):
                          for c in range(2):
                              for gg in range(2):
                                  hm = (2 * c + X) * 2 + gg
                                  col = j * 8 + hm
                                  op("pe", MM(ps[:, 6, col:col + 1], accS[X][:, (c * 2 + gg) * 256 + j * 128:(c * 2 + gg) * 256 + (j + 1) * 128], ones1,
                                              start=True, stop=True, skip_group_check=True), reads=[BaccS[X], Bones], writes=[PS[6]])
                  for j, t in enumerate(tiles):
                      for X in range(2):
                          for m in range(2):
                              op("pe", TR(ps[:, j, (X * 2 + m) * 128:(X * 2 + m + 1) * 128], usb[:, X, m * 256 + j * 128:m * 256 + (j + 1) * 128], ident_f),
                                 reads=[Busb, Bidf], writes=[PS[j]])
                  ymix_l = []
                  for j, t in enumerate(tiles):
                      PU = [PS[j]]
                      fs, Bfs = fsc_ring.next()
                      rcp = fs[:, 0:8]
                      op("dve", lambda e, rcp=rcp, j=j: e.reciprocal(out=rcp, in_=ps[:, 6, j * 8:(j + 1) * 8]), reads=[PS[6]], writes=[Bfs])
                      nrl = fs[:, 8:12]
                      op("dve", TS(nrl, rcp.rearrange("p (h m) -> p h m", m=2)[:, :, 1], neglam, ALU.mult), reads=[Bfs, Blsc], writes=[Bfs])
                      osb, Bosb = osb_ring.next()
                      for hh in range(4):
                          X_, c_ = hh % 2, hh // 2
                          u1 = ps[:, j, ((X_ * 2 + 0) * 2 + c_) * 64:((X_ * 2 + 0) * 2 + c_) * 64 + 64]
                          u2 = ps[:, j, ((X_ * 2 + 1) * 2 + c_) * 64:((X_ * 2 + 1) * 2 + c_) * 64 + 64]
                          op("dve", TS(osb[:, hh * 64:(hh + 1) * 64], u1, rcp[:, 2 * hh:2 * hh + 1], ALU.mult), reads=PU + [Bfs], writes=[Bosb])
                          op("dve", STT(osb[:, hh * 64:(hh + 1) * 64], u2, nrl[:, hh:hh + 1], osb[:, hh * 64:(hh + 1) * 64], ALU.mult, ALU.add),
                             reads=PU + [Bfs, Bosb], writes=[Bosb])
                      op("dve", TT(ojk, osb, osb, ALU.mult), reads=[Bosb], writes=[Bojk])
                      op("dve", RSUM(fs[:, 12:16], ojk.rearrange("p (h d) -> p h d", h=4)), reads=[Bojk, Bfs], writes=[Bfs])
                      op("act", ACT(fs[:, 16:20], fs[:, 12:16], AF.Ln, scale=1.0 / 64, bias=eps_t), reads=[Bfs, Beps], writes=[Bfs])
                      op("act", ACT(fs[:, 20:24], fs[:, 16:20], AF.Exp, scale=-0.5), reads=[Bfs], writes=[Bfs])
                      ymix, Bym = ymix_ring.next()
                      for hh in range(4):
                          op("dve", STT(ymix[:, 768 + hh * 64:768 + (hh + 1) * 64], osb[:, hh * 64:(hh + 1) * 64], fs[:, 20 + hh:21 + hh], gsub_s,
                                        ALU.mult, ALU.mult), reads=[Bosb, Bfs, Bgsub], writes=[Bym])
                      dma("sp", ymix[:, 512:768], yb_d[t], reads=[db("yb", t)], writes=[Bym])
                      ymix_l.append((ymix, Bym))
                  def tail_tile(j, t):
                      ymix, Bym = ymix_l[j]
                      fs, Bfs = fsc_ring.next()
                      qa, Bqa = qa_ring.next()
                      dma("sp", qa, qta_d[t], reads=[db("qta", t)], writes=[Bqa])
                      if kind == "ctx":
                          slots = [(0, None), (1, None)]
                      else:
                          pslot = (t - 1, 0) if t > 2 else (NT, 2)
                          nslot = (t + 1, 1) if t < NT - 1 else (NT + 1, 3)
                          slots = [(0, None), (1, None), pslot, (t, None), nslot]
                      ns = len(slots)
                      for g in range(2):
                          for k_i, (slot, mk) in enumerate(slots):
                              op("pe", MM(ps[:, k_i, :], KTA[64 * g:64 * g + 64, slot * 128:(slot + 1) * 128], qa[64 * g:64 * g + 64, :],
                                          start=True, stop=(mk is None), tile_position=(64 * g, 0)), reads=[BKTA[slot], Bqa], writes=[PS[k_i]])
                              if mk is not None:
                                  op("pe", MM(ps[:, k_i, :], ident_b, mneg[:, mk, :], start=False, stop=True), reads=[Bidb, Bmneg], writes=[PS[k_i]])
                          eab, Beab = eab_ring.next()
                          op("act", ACT(eab[:, 0:ns, :], ps[:, 0:ns, :], AF.Exp, scale=SCALE_A), reads=PS[0:ns], writes=[Beab])
                          for k_i, (slot, mk) in enumerate(slots):
                              op("pe", MM(ps[0:65, 6 + g, :], VA[:, slot, g * 65:(g + 1) * 65], eab[:, k_i, :], start=(k_i == 0), stop=(k_i == ns - 1)),
                                 reads=[BVA[slot], Beab], writes=[PS[6 + g]])
                      op("act", ACT(uasb[0:65, :, :], ps[0:65, 6:8, :], AF.Copy), reads=[PS[6], PS[7]], writes=[Buasb])
                      for g in range(2):
                          for jh in range(4):
                              op("pe", TR(ps[:, 4 + g, jh * 65:(jh + 1) * 65], uasb[0:65, g, jh * 128:(jh + 1) * 128], ident_f[0:65, 0:65]),
                                 reads=[Buasb, Bidf], writes=[PS[4 + g]])
                      UAT = ps[:, 4:6, 0:260].rearrange("p g (j d) -> p g j d", j=4)
                      den = fs[:, 24:32]
                      op("dve", TT(den.rearrange("p (g j) -> p g j", g=2), UAT[:, :, :, 64], esink.rearrange("p (g j) -> p g j", g=2), ALU.add),
                         reads=[PS[4], PS[5], Besink, Bfs], writes=[Bfs])
                      op("dve", lambda e, den=den: e.reciprocal(out=den, in_=den), reads=[Bfs], writes=[Bfs])
                      for g in range(2):
                          op("dve", TT(ymix[:, g * 256:(g + 1) * 256].rearrange("p (j d) -> p j d", j=4), UAT[:, g, :, 0:64],
                                       den[:, g * 4:(g + 1) * 4].unsqueeze(2).broadcast_to([128, 4, 64]), ALU.mult),
                             reads=[PS[4 + g], Bfs], writes=[Bym])
                      yield

                  def tail2(j, t, ymix, Bym):
                      pb = 6 + j
                      P_ = PS[pb]
                      for kc in range(8):
                          op("pe", TR(psb[pb][:, kc * 128:(kc + 1) * 128], ymix[:, kc * 128:(kc + 1) * 128], ident_b), reads=[Bym, Bidb], writes=[P_])
                      ymT, BymT = ymT_ring.next()
                      x2t, Bx2 = x2_ring.next()
                      src, sdeps = xsrc(l, t)
                      dma("sp", x2t, src, reads=sdeps, writes=[Bx2])
                      yield
                      op("dve", CP(ymT, psb[pb].rearrange("p (k t) -> p k t", k=8)), reads=[P_], writes=[BymT])
                      yield
                      ytm, Byt = yt_ring.next()
                      for hf in range(2):
                          for kc in range(8):
                              op("pe", MM(ps[:, pb, :], ymT[:, kc, :], wout[:, kc, hf * 512:(hf + 1) * 512], start=(kc == 0), stop=(kc == 7)),
                                 reads=[BymT, Bwout], writes=[P_])
                          yield
                          op("dve", TT(ytm[:, hf * 512:(hf + 1) * 512], ps[:, pb, :], GT1[:, hf * 512:(hf + 1) * 512], ALU.mult),
                             reads=[P_, BGT1], writes=[Byt])
                          yield
                      xm, Bxm = xm_ring.next()
                      op("dve", TT(xm, ytm, x2t, ALU.add), reads=[Byt, Bx2], writes=[Bxm])
                      dma("sp", xmid[t], xm, reads=[Bxm], writes=[db("xmid", t)])
                      rs, Brs = rsc_ring.next()
                      op("dve", TT(ytm, xm, xm, ALU.mult), reads=[Bxm], writes=[Byt])
                      op("dve", RSUM(rs[:, 0:1], ytm), reads=[Byt], writes=[Brs])
                      yield
                      rstd_from_ss(rs[:, 0:1], Brs, rs[:, 1:2], Brs, rs[:, 2:3], Brs, D)
                      yield
                      op("dve", STT(ytm, xm, rs[:, 2:3], G2, ALU.mult, ALU.mult), reads=[Bxm, Brs, BG2], writes=[Byt])
                      h2, Bh2 = h2_ring.next()
                      op("dve", TT(h2, ytm, SH2, ALU.add), reads=[Byt, BSH2], writes=[Bh2])
                      yield
                      h2Tf, Bh2Tf = h2Tf_ring.next()
                      h2Tb, Bh2Tb = h2Tb_ring.next()
                      for hb in range(2):
                          for kq in range(4):
                              kc = 4 * hb + kq
                              op("pe", TR(ps[:, pb, kq * 128:(kq + 1) * 128], h2[:, kc * 128:(kc + 1) * 128], ident_f), reads=[Bh2, Bidf], writes=[P_])
                          yield
                          h2ps = ps[:, pb, :].rearrange("p (k t) -> p k t", k=4)
                          op("dve", CP(h2Tf[:, 4 * hb:4 * hb + 4, :], h2ps), reads=[P_], writes=[Bh2Tf])
                          op("dve", CP(h2Tb[:, 4 * hb:4 * hb + 4, :], h2ps), reads=[P_], writes=[Bh2Tb])
                          yield
                      dma("sp", h2t_d[:, :, t * 128:(t + 1) * 128], h2Tb, reads=[Bh2Tb], writes=[db("h2t", t)])
                      for kc in range(8):
                          op("pe", MM(ps[:, pb, 0:20], h2Tf[:, kc, :], wr[:, kc, :], start=(kc == 0), stop=(kc == 7)), reads=[Bh2Tf, Bwr], writes=[P_])
                      yield
                      lg = rs[:, 4:24]
                      op("dve", TT(lg, ps[:, pb, 0:20], brb, ALU.add), reads=[P_, Bbr, Brs], writes=[Brs])
                      lgG = lg[:, 0:4]
                      le = lg[:, 4:20].rearrange("p (g j) -> p g j", g=4)
                      mx = rs[:, 24:25]
                      op("dve", RMAX(mx, lgG), reads=[Brs], writes=[Brs])
                      shf = rs[:, 25:29]
                      op("dve", TS(shf, lgG, mx, ALU.subtract), reads=[Brs], writes=[Brs])
                      yield
                      exg = rs[:, 29:33]
                      sume = rs[:, 33:34]
                      op("act", ACT(exg, shf, AF.Exp), reads=[Brs], writes=[Brs])
                      yield
                      op("dve", RSUM(sume, exg), reads=[Brs], writes=[Brs])
                      ptop = rs[:, 34:35]
                      op("dve", lambda e, ptop=ptop, sume=sume: e.reciprocal(out=ptop, in_=sume), reads=[Brs], writes=[Brs])
                      oh = rs[:, 35:39]
                      op("dve", TS(oh, lgG, mx, ALU.is_equal), reads=[Brs], writes=[Brs])
                      tmp16 = rs[:, 40:56]
                      op("dve", TT(tmp16.rearrange("p (g j) -> p g j", g=4), le, oh.unsqueeze(2).broadcast_to([128, 4, 4]), ALU.mult), reads=[Brs], writes=[Brs])
                      yield
                      leg = rs[:, 56:60]
                      op("dve", RSUM(leg, tmp16.rearrange("p (g j) -> p j g", g=4)), reads=[Brs], writes=[Brs])
                      m1 = rs[:, 60:61]
                      op("dve", RMAX(m1, leg), reads=[Brs], writes=[Brs])
                      mk1 = rs[:, 61:65]
                      op("dve", TS(mk1, leg, m1, ALU.is_equal), reads=[Brs], writes=[Brs])
                      le2 = rs[:, 65:69]
                      op("dve", STT(le2, mk1, -1e30, leg, ALU.mult, ALU.add), reads=[Brs], writes=[Brs])
                      yield
                      m2 = rs[:, 69:70]
                      op("dve", RMAX(m2, le2), reads=[Brs], writes=[Brs])
                      mk2 = rs[:, 70:74]
                      op("dve", TS(mk2, le2, m2, ALU.is_equal), reads=[Brs], writes=[Brs])
                      d21 = rs[:, 74:75]
                      op("dve", TT(d21, m2, m1, ALU.subtract), reads=[Brs], writes=[Brs])
                      yield
                      e21 = rs[:, 75:76]
                      op("act", ACT(e21, d21, AF.Exp), reads=[Brs], writes=[Brs])
                      yield
                      w1 = rs[:, 76:77]
                      op("dve", TS(w1, e21, 1.0, ALU.add), reads=[Brs], writes=[Brs])
                      op("dve", lambda e, w1=w1: e.reciprocal(out=w1, in_=w1), reads=[Brs], writes=[Brs])
                      op("dve", TT(w1, w1, ptop, ALU.mult), reads=[Brs], writes=[Brs])
                      w2 = rs[:, 77:78]
                      op("dve", TT(w2, w1, e21, ALU.mult), reads=[Brs], writes=[Brs])
                      yield
                      gj = rs[:, 78:82]
                      op("dve", TS(gj, mk1, w1, ALU.mult), reads=[Brs], writes=[Brs])
                      op("dve", STT(gj, mk2, w2, gj, ALU.mult, ALU.add), reads=[Brs], writes=[Brs])
                      op("dve", TT(gates[:, t, :].rearrange("p (g j) -> p g j", g=4), oh.unsqueeze(2).broadcast_to([128, 4, 4]),
                                   gj.unsqueeze(1).broadcast_to([128, 4, 4]), ALU.mult), reads=[Brs], writes=[Bgate[t]])

                  run_interleaved((tail_tile(j, t) for j, t in enumerate(tiles)), 2)
                  if kind == "ctx":
                      run_interleaved((tail2(j, t, *ymix_l[j]) for j, t in enumerate(tiles)), 2)
                  else:
                      pending.extend([tail2(j, t, *ymix_l[j]) for j, t in enumerate(tiles)])
              flush_pending()
              A.release()
              A.release()
              fw.barrier()
              if cfg.stop_after == ("p2", l):
                  break

              A.mark()
              p3tiles = list(range(NT)) if not last else list(range(2, NT))
              if len(p3tiles) <= 9:
                  sblocks = [p3tiles]
              else:
                  hsz = (len(p3tiles) + 1) // 2
                  sblocks = [p3tiles[:hsz], p3tiles[hsz:]]
              SBT = max(len(x) for x in sblocks)
              GT2 = [A.alloc([D], F32) for _ in range(2)]; BGT2 = [Buf(), Buf()]
              for r in ((0,) if last else (0, 1)):
                  load_mod(l, r, 5, GT2[r], BGT2[r])
              if last:
                  gfin = A.alloc([D], F32); Bgfin = Buf()
                  load_gain(I["g_final"], gfin, Bgfin)
              h2sb = A.alloc([8, SBT * 128], BF16); Bh2sb = Buf()
              yacc = A.alloc([SBT, D], F32); Byacc = [Buf() for _ in range(SBT)]
              wg_ring = Ring(2, [8, DEXP], BF16)
              wu_ring = Ring(2, [8, DEXP], BF16)
              wd_ring = Ring(2, [4, D], BF16)
              sg_ring = Ring(2, [512], F32)
              he_ring = Ring(2, [4, 512], BF16)
              xm3_ring = Ring(2, [D], F32)
              fo_ring = Ring(2, [D], F32)
              f3_ring = Ring(2, [8], F32)
              junk3 = A.alloc([D], BF16); Bjunk3 = Buf()
              ycnt = 0
              for tl in sblocks:
                  ntok = len(tl) * 128
                  dma("sp", h2sb[:, :, 0:ntok], h2t_d[:, :, tl[0] * 128:(tl[-1] + 1) * 128], reads=[db("h2t", t) for t in tl], writes=[Bh2sb])
                  for e_ in range(NEXP):
                      wg, Bwg = wg_ring.next()
                      wu, Bwu = wu_ring.next()
                      wd, Bwd = wd_ring.next()
                      dma("pool", wg, I["w_gate"][l, e_].rearrange("(k p) n -> p k n", p=128), writes=[Bwg])
                      dma("pool", wu, I["w_up"][l, e_].rearrange("(k p) n -> p k n", p=128), writes=[Bwu])
                      dma("pool", wd, I["w_down"][l, e_].rearrange("(k p) n -> p k n", p=128), writes=[Bwd])
                      for b0 in range(0, ntok, 512):
                          n = min(512, ntok - b0)
                          he, Bhe = he_ring.next()
                          for dc in range(4):
                              gb, ub = dc % 2, 2 + dc % 2
                              for kc in range(8):
                                  op("pe", MM(ps[:, gb, 0:n], wg[:, kc, dc * 128:(dc + 1) * 128], h2sb[:, kc, b0:b0 + n], start=(kc == 0), stop=(kc == 7)),
                                     reads=[Bwg, Bh2sb], writes=[PS[gb]])
                              for kc in range(8):
                                  op("pe", MM(ps[:, ub, 0:n], wu[:, kc, dc * 128:(dc + 1) * 128], h2sb[:, kc, b0:b0 + n], start=(kc == 0), stop=(kc == 7)),
                                     reads=[Bwu, Bh2sb], writes=[PS[ub]])
                              sg, Bsg = sg_ring.next()
                              op("act", ACT(sg[:, 0:n], ps[:, gb, 0:n], AF.Silu), reads=[PS[gb]], writes=[Bsg])
                              op("dve", TT(he[:, dc, 0:n], sg[:, 0:n], ps[:, ub, 0:n], ALU.mult), reads=[Bsg, PS[ub]], writes=[Bhe])
                          for j in range(n // 128):
                              ti = b0 // 128 + j
                              t = tl[ti]
                              for hf in range(2):
                                  yb_ = 4 + (ycnt % 4)
                                  ycnt += 1
                                  for dc in range(4):
                                      op("pe", MM(ps[:, yb_, :], he[:, dc, j * 128:(j + 1) * 128], wd[:, dc, hf * 512:(hf + 1) * 512], start=(dc == 0), stop=(dc == 3)),
                                         reads=[Bhe, Bwd], writes=[PS[yb_]])
                                  ya_ = yacc[:, ti, hf * 512:(hf + 1) * 512]
                                  if e_ == 0:
                                      op("dve", TS(ya_, ps[:, yb_, :], gates[:, t, e_:e_ + 1], ALU.mult), reads=[PS[yb_], Bgate[t]], writes=[Byacc[ti]])
                                  else:
                                      op("dve", STT(ya_, ps[:, yb_, :], gates[:, t, e_:e_ + 1], ya_, ALU.mult, ALU.add),
                                         reads=[PS[yb_], Bgate[t], Byacc[ti]], writes=[Byacc[ti]])
                  for ti, t in enumerate(tl):
                      r = 1 if t < 2 else 0
                      xm3, Bxm3 = xm3_ring.next()
                      dma("act", xm3, xmid[t], reads=[db("xmid", t)], writes=[Bxm3])
                      op("dve", TT(yacc[:, ti, :], yacc[:, ti, :], GT2[r], ALU.mult), reads=[Byacc[ti], BGT2[r]], writes=[Byacc[ti]])
                      xo, Bxo = xm3, Bxm3
                      op("dve", TT(xo, yacc[:, ti, :], xm3, ALU.add), reads=[Byacc[ti], Bxm3], writes=[Bxo])
                      if not last:
                          dma("sp", xbuf[t], xo, reads=[Bxo], writes=[db("xbuf", t)])
                      else:
                          f3, Bf3 = f3_ring.next()
                          fo, Bfo = fo_ring.next()
                          op("dve", TT(fo, xo, xo, ALU.mult), reads=[Bxo], writes=[Bfo])
                          op("dve", RSUM(f3[:, 0:1], fo), reads=[Bfo], writes=[Bf3])
                          rstd_from_ss(f3[:, 0:1], Bf3, f3[:, 1:2], Bf3, f3[:, 2:3], Bf3, D)
                          op("dve", STT(fo, xo, f3[:, 2:3], gfin, ALU.mult, ALU.mult), reads=[Bxo, Bf3, Bgfin], writes=[Bfo])
                          dma("sp", out_d[(t - 2) * 128:(t - 1) * 128, :], fo, reads=[Bfo], writes=[db("out", t)])
              A.release()
              A.release()
              fw.barrier()

        except StopBuild:
            pass
        fw.barrier()
        blk = st.enter_context(nc.Block())
        stats = fw.finish(blk)
    return nc, stats


def _rope_table(cfg, r):
    NT, NLAT = cfg.NT, cfg.NLAT
    tab = np.zeros((128, NT, 96), np.float32)
    tab[:, :, 0:32] = 1.0
    tab[:, :, 64:80] = 1.0
    invA = (np.float32(10000.0) ** (-np.arange(16, dtype=np.float32) / np.float32(16))).astype(np.float32)
    invC = (np.float32(10000.0) ** (-np.arange(8, dtype=np.float32) / np.float32(8))).astype(np.float32)
    for t in range(2, NT):
        tok = r * NLAT * 128 + (t - 2) * 128 + np.arange(128)
        rows = (tok // GRID_W).astype(np.float32)
        cols = (tok % GRID_W).astype(np.float32)
        for pi, pos in enumerate((rows, cols)):
            angA = (pos[:, None] * invA[None, :]).astype(np.float32)
            angC = (pos[:, None] * invC[None, :]).astype(np.float32)
            tab[:, t, pi * 16:(pi + 1) * 16] = np.cos(angA)
            tab[:, t, 32 + pi * 16:32 + (pi + 1) * 16] = np.sin(angA)
            tab[:, t, 64 + pi * 8:64 + (pi + 1) * 8] = np.cos(angC)
            tab[:, t, 80 + pi * 8:80 + (pi + 1) * 8] = np.sin(angC)
    return tab.reshape(128, NT * 96)


_WIN_PERM = np.arange(DIN)
for _g in range(2):
    for _j in range(4):
        _WIN_PERM[(_j * 2 + _g) * 64:(_j * 2 + _g + 1) * 64] = np.arange((_g * 4 + _j) * 64, (_g * 4 + _j + 1) * 64)


def make_in_maps(cfg, inputs):
    NLAT, RANKS, NB = cfg.NLAT, cfg.RANKS, cfg.NB
    f = lambda a: np.ascontiguousarray(np.asarray(a, dtype=np.float32))
    x, c, ctx, c_ctx = f(inputs["x"]), f(inputs["c"]), f(inputs["ctx"]), f(inputs["c_ctx"])
    dp = cfg.DEPTH
    shared = {
        "w_ada": f(inputs["w_ada"]),
        "b_ada2": f(np.repeat(f(inputs["b_ada"])[:, None, :], 2, axis=1)),
        "g_mix": f(inputs["g_mix"]), "w_in": f(f(inputs["w_in"])[:, :, _WIN_PERM]), "sink": f(inputs["sink"]),
        "ws_tok": f(inputs["ws_tok"]), "bs_tok": f(inputs["bs_tok"]), "g_tok": f(inputs["g_tok"]).reshape(dp, 256),
        "lam4": f(np.stack([f(inputs["lam_q1"]), f(inputs["lam_k1"]), f(inputs["lam_q2"]), f(inputs["lam_k2"])], axis=1)).reshape(dp, 128),
        "g_sub": f(inputs["g_sub"]), "w_out": f(inputs["w_out"]), "g_ffn": f(inputs["g_ffn"]),
        "w_r": f(np.concatenate([f(inputs["w_rg"]), f(inputs["w_re"])], axis=-1)),
        "b_r": f(np.concatenate([f(inputs["b_rg"]), f(inputs["b_re"])], axis=-1)),
        "w_gate": f(inputs["w_gate"]), "w_up": f(inputs["w_up"]), "w_down": f(inputs["w_down"]),
        "g_final": f(inputs["g_final"]).reshape(1, D),
        "ident": np.eye(128, dtype=np.float32),
    }
    kk = np.arange(128)[:, None]
    qq = np.arange(128)[None, :]
    mL = (qq <= kk).astype(np.float32)
    mU = (kk <= qq).astype(np.float32)
    maps = []
    for b in range(NB):
        for r in range(RANKS):
            m = dict(shared)
            m["xl"] = f(x[b, r * NLAT * 128:(r + 1) * NLAT * 128, :])
            m["ctx"] = f(ctx[b])
            m["c2"] = f(np.stack([c[b], c_ctx], axis=0))
            mk = np.stack([mL, mU, mL if r > 0 else np.zeros_like(mL), mU if r < RANKS - 1 else np.zeros_like(mU)], axis=1)
            m["masks"] = f(mk.reshape(128, 4 * 128))
            m["rope"] = _rope_table(cfg, r)
            s = np.zeros((3, RANKS), np.float32)
            s[0, r] = 1.0
            if r > 0:
                s[1, r - 1] = 1.0
            if r < RANKS - 1:
                s[2, r + 1] = 1.0
            m["sel"] = f(np.broadcast_to(s.reshape(1, 3 * RANKS), (128, 3 * RANKS)))
            maps.append(m)
    return maps


_CACHE = {}


def kernel(**inputs):
    x = np.asarray(inputs["x"])
    NB, S, _ = x.shape
    RANKS = 8 // NB
    NLAT = S // (RANKS * 128)
    cfg = Cfg(NLAT=NLAT, RANKS=RANKS, NB=NB, DEPTH=int(np.asarray(inputs["w_in"]).shape[0]))
    key = (NLAT, RANKS, NB, cfg.DEPTH)
    if key not in _CACHE:
        _CACHE[key] = build_program(cfg)[0]
    nc = _CACHE[key]
    maps = make_in_maps(cfg, inputs)
    res = run_bass_kernel_spmd(nc, maps, core_ids=list(range(cfg.NCORES)))
    out = np.zeros((NB, S, D), np.float32)
    for b in range(NB):
        for r in range(RANKS):
            out[b, r * NLAT * 128:(r + 1) * NLAT * 128, :] = np.asarray(res.results[b * RANKS + r]["out"])
    return out
```
